# Optimizing a Trainium2 kernel written in Bass

```python
import jax, jax.numpy as jnp
from jax import lax
import numpy as np

D_MODEL = 1024
BATCH = 8
SEQ = 2048
DEPTH = 2

CHUNK = 64
N_LEFT_CHUNKS = 8
BAND_CHUNKS = N_LEFT_CHUNKS + 1
HEAD_DIM = 64
RWKV_WIDTH = D_MODEL // 2
ATTN_WIDTH = D_MODEL - RWKV_WIDTH
RWKV_HEADS = RWKV_WIDTH // HEAD_DIM
ATTN_HEADS = ATTN_WIDTH // HEAD_DIM
DECAY_LORA = 64
AAA_LORA = 64
GATE_LORA = 128
REL_CLIP = 128
D_FF = 2816
N_EXPERTS = 8
TOP_K = 2
D_FF_EXPERT = D_FF // 2
N_DENSE = (DEPTH + 1) // 2
N_MOE = DEPTH // 2
RMS_EPS = 1e-6
GN_EPS = 64e-5
MASK_VALUE = -1e30
SHIFT_SIZES = (RWKV_WIDTH, RWKV_WIDTH, RWKV_WIDTH, DECAY_LORA, AAA_LORA, GATE_LORA)
ATTN_SIZES = (ATTN_WIDTH, ATTN_WIDTH, ATTN_WIDTH)
SHIFT_COLS = 3 * RWKV_WIDTH + DECAY_LORA + AAA_LORA + GATE_LORA
IN_COLS = SHIFT_COLS + 3 * ATTN_WIDTH

kernel_name = 'hybrid_rwkv7_chunkattn_moe_encoder'


def _split(t, sizes):
    out, start = [], 0
    for s in sizes:
        out.append(t[..., start:start + s])
        start += s
    return out


def _rmsnorm(x, g):
    xf = x.astype(jnp.float32)
    y = xf * lax.rsqrt(jnp.mean(xf * xf, axis=-1, keepdims=True) + RMS_EPS)
    return (y * g.astype(jnp.float32)).astype(x.dtype)


def _token_shift(h, mu):
    prev = jnp.pad(h, ((0, 0), (1, 0), (0, 0)))[:, :-1]
    return h + mu.astype(h.dtype) * (prev - h)


def _rwkv7(r, k, v, wd, ad, gd, w0, w2, a0, a2, g2, k_k, k_a, r_k, ln_w, ln_b):
    B, S, _ = r.shape
    dt = r.dtype
    f = lambda t: t.astype(jnp.float32)
    r, k, v = f(r), f(k), f(v)
    w = f(w0) + jnp.tanh(f(wd)) @ f(w2)
    w = -jax.nn.softplus(-w) - 0.5
    decay = jnp.exp(-jnp.exp(w))
    a = jax.nn.sigmoid(f(a0) + f(ad) @ f(a2))
    g = jax.nn.sigmoid(f(gd)) @ f(g2)
    heads = lambda t: t.reshape(B, S, RWKV_HEADS, HEAD_DIM)
    kk = heads(k * f(k_k))
    kk = kk / jnp.maximum(jnp.linalg.norm(kk, axis=-1, keepdims=True), 1e-12)
    k = k * (1.0 + (a - 1.0) * f(k_a))
    rh, kh, vh, wh, ah = heads(r), heads(k), heads(v), heads(decay), heads(a)

    def step(state, inp):
        r_t, w_t, k_t, v_t, kk_t, a_t = inp
        sa = jnp.einsum('bhvk,bhk->bhv', state, -kk_t)
        state = (state * w_t[:, :, None, :]
                 + sa[..., None] * (kk_t * a_t)[:, :, None, :]
                 + v_t[..., None] * k_t[:, :, None, :])
        y_t = jnp.einsum('bhvk,bhk->bhv', state, r_t)
        return state, y_t

    xs = tuple(jnp.swapaxes(t, 0, 1) for t in (rh, wh, kh, vh, kk, ah))
    s0 = jnp.zeros((B, RWKV_HEADS, HEAD_DIM, HEAD_DIM), jnp.float32)
    _, y = lax.scan(step, s0, xs)
    y = jnp.swapaxes(y, 0, 1)
    mu = jnp.mean(y, axis=-1, keepdims=True)
    var = jnp.mean(jnp.square(y - mu), axis=-1, keepdims=True)
    y = ((y - mu) * lax.rsqrt(var + GN_EPS)).reshape(B, S, RWKV_WIDTH) * f(ln_w) + f(ln_b)
    r_k_h = f(r_k).reshape(RWKV_HEADS, HEAD_DIM)
    bonus = jnp.sum(rh * kh * r_k_h, axis=-1, keepdims=True) * vh
    y = (y + bonus.reshape(B, S, RWKV_WIDTH)) * g
    return y.astype(dt)


def _chunk_attention(q, k, v, rel_bias, norm_g):
    B, S, _ = q.shape
    NC = S // CHUNK
    shp = lambda t: t.reshape(B, NC, CHUNK, ATTN_HEADS, HEAD_DIM)
    q, k, v = shp(q), shp(k), shp(v)
    pad = ((0, 0), (N_LEFT_CHUNKS, 0), (0, 0), (0, 0), (0, 0))
    kp, vp = jnp.pad(k, pad), jnp.pad(v, pad)
    band = jnp.arange(NC)[:, None] + jnp.arange(BAND_CHUNKS)[None, :]
    kb = kp[:, band].reshape(B, NC, BAND_CHUNKS * CHUNK, ATTN_HEADS, HEAD_DIM)
    vb = vp[:, band].reshape(B, NC, BAND_CHUNKS * CHUNK, ATTN_HEADS, HEAD_DIM)
    scores = jnp.einsum('bnqhd,bnkhd->bnhqk', q, kb,
                        preferred_element_type=jnp.float32) * (HEAD_DIM ** -0.5)
    qi = jnp.arange(CHUNK)[:, None]
    kj = jnp.arange(BAND_CHUNKS * CHUNK)[None, :]
    rel = qi - kj + N_LEFT_CHUNKS * CHUNK
    idx = jnp.clip(rel, -REL_CLIP, REL_CLIP) + REL_CLIP
    bias = jnp.transpose(rel_bias.astype(jnp.float32)[idx], (2, 0, 1))
    valid = jnp.repeat(band >= N_LEFT_CHUNKS, CHUNK, axis=1)
    scores = jnp.where(valid[None, :, None, None, :], scores + bias[None, None], MASK_VALUE)
    p = jax.nn.softmax(scores, axis=-1)
    o = jnp.einsum('bnhqk,bnkhd->bnqhd', p.astype(vb.dtype), vb)
    o = o.reshape(B, S, ATTN_WIDTH)
    return _rmsnorm(o, norm_g)


def _swiglu(h, wg, wu, wd):
    return (jax.nn.silu(h @ wg) * (h @ wu)) @ wd


def _moe(h, router, wg, wu, wd):
    logits = (h @ router).astype(jnp.float32)
    top_val, top_idx = lax.top_k(logits, TOP_K)
    gates = jax.nn.softmax(top_val, axis=-1)
    combine = jnp.sum(jax.nn.one_hot(top_idx, N_EXPERTS, dtype=jnp.float32) * gates[..., None], axis=-2)
    combine = combine.astype(h.dtype)
    y = jnp.zeros_like(h)
    for e in range(N_EXPERTS):
        y = y + combine[..., e:e + 1] * _swiglu(h, wg[e], wu[e], wd[e])
    return y


def setup_inputs(seed: int = 0) -> dict:
    key = jax.random.key(seed)
    keys = jax.random.split(key, 40)
    counter = [0]

    def nxt():
        kk = keys[counter[0]]
        counter[0] += 1
        return kk

    nrm = lambda shape, scale: scale * jax.random.normal(nxt(), shape, jnp.float32)
    L = DEPTH
    x = nrm((BATCH, SEQ, D_MODEL), 1.0)
    norm_mix_g = 1.0 + nrm((L, D_MODEL), 0.02)
    w_in = nrm((L, D_MODEL, IN_COLS), D_MODEL ** -0.5)
    shift_mu = jax.random.uniform(nxt(), (L, SHIFT_COLS), jnp.float32, 0.2, 0.8)
    ramp = jnp.linspace(0.0, 1.0, RWKV_WIDTH) ** 0.85
    rwkv_w0 = (-6.5 + 5.0 * ramp)[None, :] + nrm((L, RWKV_WIDTH), 0.1)
    rwkv_w2 = nrm((L, DECAY_LORA, RWKV_WIDTH), 0.1 * DECAY_LORA ** -0.5)
    rwkv_a0 = nrm((L, RWKV_WIDTH), 0.2)
    rwkv_a2 = nrm((L, AAA_LORA, RWKV_WIDTH), 0.5 * AAA_LORA ** -0.5)
    rwkv_g2 = nrm((L, GATE_LORA, RWKV_WIDTH), GATE_LORA ** -0.5)
    rwkv_k_k = 0.85 + nrm((L, RWKV_WIDTH), 0.05)
    rwkv_k_a = 1.0 + nrm((L, RWKV_WIDTH), 0.05)
    rwkv_r_k = nrm((L, RWKV_WIDTH), 0.1)
    rwkv_ln_w = 1.0 + nrm((L, RWKV_WIDTH), 0.02)
    rwkv_ln_b = nrm((L, RWKV_WIDTH), 0.02)
    attn_rel_bias = nrm((L, 2 * REL_CLIP + 1, ATTN_HEADS), 0.5)
    attn_norm_g = 1.0 + nrm((L, ATTN_WIDTH), 0.02)
    w_out = nrm((L, D_MODEL, D_MODEL), D_MODEL ** -0.5)
    norm_ffn_g = 1.0 + nrm((L, D_MODEL), 0.02)
    ffn_w_gate = nrm((N_DENSE, D_MODEL, D_FF), D_MODEL ** -0.5)
    ffn_w_up = nrm((N_DENSE, D_MODEL, D_FF), D_MODEL ** -0.5)
    ffn_w_down = nrm((N_DENSE, D_FF, D_MODEL), D_FF ** -0.5)
    moe_router = nrm((N_MOE, D_MODEL, N_EXPERTS), D_MODEL ** -0.5)
    moe_w_gate = nrm((N_MOE, N_EXPERTS, D_MODEL, D_FF_EXPERT), D_MODEL ** -0.5)
    moe_w_up = nrm((N_MOE, N_EXPERTS, D_MODEL, D_FF_EXPERT), D_MODEL ** -0.5)
    moe_w_down = nrm((N_MOE, N_EXPERTS, D_FF_EXPERT, D_MODEL), D_FF_EXPERT ** -0.5)
    norm_final_g = 1.0 + nrm((D_MODEL,), 0.02)
    return {'x': x, 'norm_mix_g': norm_mix_g, 'w_in': w_in, 'shift_mu': shift_mu,
            'rwkv_w0': rwkv_w0, 'rwkv_w2': rwkv_w2, 'rwkv_a0': rwkv_a0, 'rwkv_a2': rwkv_a2,
            'rwkv_g2': rwkv_g2, 'rwkv_k_k': rwkv_k_k, 'rwkv_k_a': rwkv_k_a, 'rwkv_r_k': rwkv_r_k,
            'rwkv_ln_w': rwkv_ln_w, 'rwkv_ln_b': rwkv_ln_b, 'attn_rel_bias': attn_rel_bias,
            'attn_norm_g': attn_norm_g, 'w_out': w_out, 'norm_ffn_g': norm_ffn_g,
            'ffn_w_gate': ffn_w_gate, 'ffn_w_up': ffn_w_up, 'ffn_w_down': ffn_w_down,
            'moe_router': moe_router, 'moe_w_gate': moe_w_gate, 'moe_w_up': moe_w_up,
            'moe_w_down': moe_w_down, 'norm_final_g': norm_final_g}


def reference(x, norm_mix_g, w_in, shift_mu, rwkv_w0, rwkv_w2, rwkv_a0, rwkv_a2, rwkv_g2,
              rwkv_k_k, rwkv_k_a, rwkv_r_k, rwkv_ln_w, rwkv_ln_b, attn_rel_bias, attn_norm_g,
              w_out, norm_ffn_g, ffn_w_gate, ffn_w_up, ffn_w_down, moe_router, moe_w_gate,
              moe_w_up, moe_w_down, norm_final_g):
    for l in range(DEPTH):
        h = _rmsnorm(x, norm_mix_g[l])
        proj = h @ w_in[l]
        r, k, v, wd, ad, gd = _split(_token_shift(proj[..., :SHIFT_COLS], shift_mu[l]), SHIFT_SIZES)
        qa, ka, va = _split(proj[..., SHIFT_COLS:], ATTN_SIZES)
        y_rwkv = _rwkv7(r, k, v, wd, ad, gd, rwkv_w0[l], rwkv_w2[l], rwkv_a0[l], rwkv_a2[l],
                        rwkv_g2[l], rwkv_k_k[l], rwkv_k_a[l], rwkv_r_k[l], rwkv_ln_w[l], rwkv_ln_b[l])
        y_attn = _chunk_attention(qa, ka, va, attn_rel_bias[l], attn_norm_g[l])
        x = x + jnp.concatenate([y_rwkv, y_attn], axis=-1) @ w_out[l]
        h = _rmsnorm(x, norm_ffn_g[l])
        li = l // 2
        if l % 2 == 0:
            x = x + _swiglu(h, ffn_w_gate[li], ffn_w_up[li], ffn_w_down[li])
        else:
            x = x + _moe(h, moe_router[li], moe_w_gate[li], moe_w_up[li], moe_w_down[li])
    return _rmsnorm(x, norm_final_g)
```

```python
import numpy as np
from contextlib import ExitStack
import concourse.bass as bass
import concourse.mybir as mybir
from concourse.bass_utils import run_bass_kernel_spmd

F32 = mybir.dt.float32
BF16 = mybir.dt.bfloat16
AF = mybir.ActivationFunctionType
ALU = mybir.AluOpType
AX = mybir.AxisListType

T = 2048
NB = 4
NT = 16
SEM_LIMIT = 60000
NPV = 80
DECAY_C = 0.6065306597126334


class Trk:
    __slots__ = ("w", "r")

    def __init__(self):
        self.w = None
        self.r = {}


class KB:
    def __init__(self, n_dma_sems=12):
        self.nc = bass.Bass("TRN2", target_bir_lowering=False)
        self.es = ExitStack()
        nc = self.nc
        self.eh = {"pe": nc.tensor, "act": nc.scalar, "dve": nc.vector, "pool": nc.gpsimd, "sp": nc.sync}
        self.sem = {}
        self.cnt = {}
        self.cur = {}
        self.eng_of = {}
        self.gen = {}
        self.known = {e: {} for e in self.eh}
        for e in self.eh:
            self._newgen(e, e)
        self.dma_pools = {"sp": ["d%d" % j for j in range(6)], "act": ["d%d" % j for j in range(6)], "pool": ["g%d" % j for j in range(8)]}
        self.dma_names = self.dma_pools["sp"] + self.dma_pools["pool"]
        for d in self.dma_names:
            self._newgen(d, None)
        self.dma_rr = {"sp": 0, "act": 0, "pool": 0}
        self.nwaits = 0
        self.nins = 0
        self.total_ins = {e: 0 for e in self.eh}

    def _newgen(self, name, eng):
        g = self.gen.get(name, -1) + 1
        self.gen[name] = g
        key = "%s_%d" % (name, g)
        self.sem[key] = self.es.enter_context(self.nc.semaphore("s_" + key))
        self.cnt[key] = 0
        self.cur[name] = key
        self.eng_of[key] = eng
        return key

    def sbuf(self, name, shape, dt, stack=None):
        self.uid = getattr(self, "uid", 0) + 1
        return (stack or self.es).enter_context(self.nc.sbuf_tensor("%s_%d" % (name, self.uid), list(shape), dt))

    def psum(self, name, shape, dt=F32):
        return self.es.enter_context(self.nc.psum_tensor(name, list(shape), dt))

    def _wait(self, eng, key, val):
        if val <= 0:
            return
        kn = self.known[eng]
        if kn.get(key, 0) >= val:
            return
        self.eh[eng].wait_ge(self.sem[key], val)
        kn[key] = val
        self.nwaits += 1

    def _deps(self, eng, R, W):
        deps = {}
        cur = self.cur[eng]
        for t in R:
            if t.w is not None:
                k, v = t.w
                if k == cur:
                    if eng != "pe":
                        self._wait(eng, k, v)
                elif self.eng_of[k] != eng:
                    if deps.get(k, 0) < v:
                        deps[k] = v
        for t in W:
            if t.w is not None:
                k, v = t.w
                if self.eng_of[k] != eng and deps.get(k, 0) < v:
                    deps[k] = v
            for k, v in t.r.items():
                if self.eng_of[k] != eng and deps.get(k, 0) < v:
                    deps[k] = v
        for k, v in deps.items():
            self._wait(eng, k, v)

    def _mark(self, key, c, R, W):
        for t in W:
            t.w = (key, c)
            t.r = {}
        for t in R:
            if t.r.get(key, 0) < c:
                t.r[key] = c

    def ins(self, eng, fn, R=(), W=()):
        cur = self.cur[eng]
        if self.cnt[cur] >= SEM_LIMIT:
            self.eh[eng].wait_ge(self.sem[cur], self.cnt[cur])
            cur = self._newgen(eng, eng)
        self._deps(eng, R, W)
        inst = fn()
        inst.then_inc(self.sem[cur], 1)
        self.cnt[cur] += 1
        self._mark(cur, self.cnt[cur], R, W)
        self.nins += 1
        self.total_ins[eng] += 1
        return inst

    def dma(self, q, out, in_, R=(), W=(), **kw):
        pool_ = self.dma_pools[q]
        name = pool_[self.dma_rr[q] % len(pool_)]
        self.dma_rr[q] += 1
        key = self.cur[name]
        self._wait(q, key, self.cnt[key])
        if self.cnt[key] >= SEM_LIMIT:
            key = self._newgen(name, None)
        self._deps(q, R, W)
        for t in list(R) + list(W):
            if t.w is not None and self.eng_of[t.w[0]] == q:
                self.eh[q].wait_ge(self.sem[t.w[0]], t.w[1])
        for t in W:
            for k, v in t.r.items():
                if self.eng_of[k] == q:
                    self.eh[q].wait_ge(self.sem[k], v)
        inst = self.eh[q].dma_start(out=out, in_=in_, **kw)
        inst.then_inc(self.sem[key], 16)
        self.cnt[key] += 16
        self._mark(key, self.cnt[key], R, W)
        self.nins += 1
        return inst

    def barrier(self):
        keys = [self.cur[n] for n in self.dma_names] + [self.cur[e] for e in self.eh]
        for e in self.eh:
            for k in keys:
                if self.eng_of[k] != e:
                    self._wait(e, k, self.cnt[k])

    def finish(self, eng="sp"):
        for name in self.dma_names:
            key = self.cur[name]
            self._wait(eng, key, self.cnt[key])
        for e in self.eh:
            if e != eng:
                key = self.cur[e]
                self._wait(eng, key, self.cnt[key])

    def close(self):
        self.es.close()


def build_program(stop_after="final", n_layers=2, dump=None, dbg=None):
    kb = KB()
    nc = kb.nc
    D = {}

    def din(name, shape):
        D[name] = nc.dram_tensor(name, list(shape), F32, kind="ExternalInput")
        return D[name]

    xT_d = din("xT", [1024, T])
    pv_d = din("pv", [2, 128, NPV])
    agbc_d = din("attn_g_bc", [2, 128, 512])
    biasT_d = din("biasT", [2, 8, 128, 640])
    win_d = din("win", [2, 26, 128, 1024])
    wv_d = din("wv", [2, 128, 4096])
    wout_d = din("wout", [2, 8, 128, 1024])
    loraw_d = din("loraw", [2, 128, 512])
    g2_d = din("g2", [2, 128, 512])
    ew_d = []
    for l, ne in enumerate((2, 8)):
        ew_d.append((din("wg%d" % l, [ne, 11, 128, 1024]), din("wu%d" % l, [ne, 11, 128, 1024]),
                     din("wd%d" % l, [ne, 11, 128, 1024])))
    router_d = din("router", [128, 64])
    c_ident_d = din("c_ident", [128, 128])
    c_onesm_d = din("c_onesm", [128, 128])
    c_blk_d = din("c_blk", [128, 128])
    c_tri_i_d = din("c_tri_i", [128, 128])
    c_tri_e_d = din("c_tri_e", [128, 128])
    c_maskA_d = din("c_maskA", [64, 512])
    c_maskB_d = din("c_maskB", [64, 512])
    c_sel_d = din("c_sel", [8, 1024])
    out_d = nc.dram_tensor("outT", [1024, T], F32, kind="ExternalOutput")

    sb = kb.sbuf
    x = sb("x", [128, 8, T], F32)
    xt = [[Trk() for _ in range(NB)] for _ in range(8)]
    h = sb("h", [128, 8, T], BF16)
    ht = [Trk() for _ in range(NB)]
    pv = sb("pvs", [128, 2, NPV], F32); pvt = Trk()
    pvd = sb("pvd", [128, 2, 24], F32); pvdt = Trk()
    ident_f = sb("ident_f", [128, 128], F32)
    ident_b = sb("ident_b", [128, 128], BF16)
    onesm = sb("onesm", [128, 128], BF16)
    blk = sb("blk", [128, 128], BF16)
    tri_i = sb("tri_i", [128, 128], F32)
    tri_e = sb("tri_e", [128, 128], F32)
    maskA = sb("maskA", [64, 512], BF16)
    maskB = sb("maskB", [64, 512], BF16)
    sel = sb("sel", [8, 1024], BF16)
    ct = Trk()
    wbuf = sb("wbuf", [128, 4, 8, 128], BF16)
    wbt = [Trk() for _ in range(4)]
    wb_rr = [0]
    sq = [sb("sq%d" % i, [128, 512], BF16) for i in range(2)]
    sqt = [Trk() for _ in range(2)]
    rstd = sb("rstd", [128, 512], F32); rstdt = Trk()

    PP = [kb.psum("pp%d" % i, [128, 1024]) for i in range(4)]
    PT = [Trk() for _ in range(8)]

    def PS(i):
        return PP[i // 2][:, (i % 2) * 512:(i % 2) * 512 + 512]

    ps_rr = [0]

    def next_ps(lo=0, hi=8):
        i = lo + (ps_rr[0] % (hi - lo))
        ps_rr[0] += 1
        return i

    for tb in range(NB):
        for c in range(8):
            kb.dma("sp", x[:, c, tb * 512:(tb + 1) * 512], xT_d.ap()[c * 128:(c + 1) * 128, tb * 512:(tb + 1) * 512], W=[xt[c][tb]])
    kb.dma("sp", pv[:, 0, :], pv_d.ap()[0], W=[pvt])
    kb.dma("sp", pv[:, 1, :], pv_d.ap()[1], W=[pvt])
    kb.dma("sp", ident_f[:], c_ident_d.ap(), W=[ct])
    kb.dma("sp", tri_i[:], c_tri_i_d.ap(), W=[ct])
    kb.dma("sp", tri_e[:], c_tri_e_d.ap(), W=[ct])
    kb.dma("pool", sel[:], c_sel_d.ap(), W=[ct])
    kb.dma("pool", ident_b[:], c_ident_d.ap(), W=[ct])
    kb.dma("pool", onesm[:], c_onesm_d.ap(), W=[ct])
    kb.dma("pool", blk[:], c_blk_d.ap(), W=[ct])
    kb.dma("pool", maskA[:], c_maskA_d.ap(), W=[ct])
    kb.dma("pool", maskB[:], c_maskB_d.ap(), W=[ct])
    for l in range(2):
        kb.ins("dve", lambda l=l: nc.vector.tensor_scalar(pvd[:, l, 0:14], pv[:, l, 24:38], -1.0, 1.0, op0=ALU.mult, op1=ALU.add),
               R=[pvt], W=[pvdt])
        kb.ins("dve", lambda l=l: nc.vector.tensor_scalar(pvd[:, l, 14:18], pv[:, l, 64:68], -1.0, 1.0, op0=ALU.mult, op1=ALU.add),
               R=[pvt], W=[pvdt])

    def load_w(src_ap, slot=None, shape4=None):
        if slot is None:
            slot = wb_rr[0] % 4
            wb_rr[0] += 1
        kb.dma("pool", wbuf[:, slot, :, :].rearrange("p k j -> p (k j)"), src_ap, W=[wbt[slot]])
        return slot

    evac_rr = [0]

    def evac_copy(out_ap, in_ap, R, W, eng=None):
        if eng is None:
            eng = "act" if (evac_rr[0] % 2 == 0) else "dve"
            evac_rr[0] += 1
        if eng == "act":
            kb.ins("act", lambda: nc.scalar.copy(out_ap, in_ap), R=R, W=W)
        else:
            kb.ins("dve", lambda: nc.vector.tensor_copy(out_ap, in_ap), R=R, W=W)

    def mm(out_ap, lhsT, rhs, start, stop, R, W):
        kb.ins("pe", lambda: nc.tensor.matmul(out_ap, lhsT, rhs, start=start, stop=stop), R=R, W=W)

    def tbs(tb):
        return slice(tb * 512, (tb + 1) * 512)

    def rmsnorm_to_h(l, gcol, after_tb=None):
        for tb in range(NB):
            pi = next_ps(0, 7)
            for c in range(8):
                s = c % 2
                kb.ins("act", lambda c=c, s=s: nc.scalar.activation(sq[s][:], x[:, c, tbs(tb)], AF.Square),
                       R=[xt[c][tb]], W=[sqt[s]])
                mm(PS(pi), onesm[:], sq[s][:], c == 0, c == 7, R=[ct, sqt[s]], W=[PT[pi]])
            kb.ins("act", lambda: nc.scalar.activation(rstd[:], PS(pi), AF.Sqrt, bias=1e-6, scale=1.0), R=[PT[pi]], W=[rstdt])
            kb.ins("dve", lambda: nc.vector.reciprocal(rstd[:], rstd[:]), R=[rstdt], W=[rstdt])
            for c in range(8):
                kb.ins("dve", lambda c=c: nc.vector.scalar_tensor_tensor(
                    out=h[:, c, tbs(tb)], in0=x[:, c, tbs(tb)], scalar=pv[:, l, gcol + c:gcol + c + 1], in1=rstd[:],
                    op0=ALU.mult, op1=ALU.mult), R=[xt[c][tb], pvt, rstdt], W=[ht[tb]])
            if after_tb is not None:
                after_tb(tb)

    def attention_phase(l):
        st = ExitStack()
        qk = sb("qk", [128, 8, T], BF16, st)
        qkt = [[Trk() for _ in range(NT)] for _ in range(8)]
        Vp = sb("Vp", [128, NT, 8, 65], BF16, st); vpt = [Trk() for _ in range(NT)]
        Mh = sb("Mh", [128, 8, 640], BF16, st); mht = Trk()
        bst = sb("bst", [128, 640], F32, st); bstt = Trk()
        E = [sb("E%d" % i, [128, 640], BF16, st) for i in range(2)]; et = [Trk() for _ in range(2)]
        o = sb("o_att", [128, 8, 64], F32, st); ot = Trk()
        yo = sb("yo_att", [128, 512], F32, st); yot = Trk()
        junk = sb("junk_att", [128, 512], BF16, st); junkt = Trk()
        gbc = sb("gbc", [128, 512], F32, st); gbct = Trk()
        rec = sb("rec", [128, 8], F32, st); rect = Trk()
        ssq = sb("ssq", [128, 2], F32, st); ssqt = Trk()

        kb.dma("sp", gbc[:], agbc_d.ap()[l], W=[gbct])
        kb.ins("pool", lambda: nc.gpsimd.memset(Vp[:, :, :, 64:65], 1.0), W=vpt)
        for hh in range(8):
            kb.dma("sp", bst[:], biasT_d.ap()[l, hh], W=[bstt])
            kb.ins("act", lambda hh=hh: nc.scalar.activation(Mh[:, hh, :], bst[:], AF.Exp), R=[bstt], W=[mht])
        for ci in range(8):
            slot = load_w(win_d.ap()[l, 14 + ci])
            for tb in range(NB):
                pi = next_ps()
                for k in range(8):
                    mm(PS(pi), wbuf[:, slot, k, :], h[:, k, tbs(tb)], k == 0, k == 7, R=[wbt[slot], ht[tb]], W=[PT[pi]])
                evac_copy(qk[:, ci, tbs(tb)], PS(pi), R=[PT[pi]], W=qkt[ci][tb * 4:tb * 4 + 4])
        kb.dma("pool", wbuf[:].rearrange("p j k c -> p (j k c)"), wv_d.ap()[l], W=wbt)
        for tt in range(NT):
            pi = next_ps()
            for k in range(8):
                mm(PS(pi), h[:, k, tt * 128:(tt + 1) * 128], wbuf[:, :, k, :], k == 0, k == 7, R=wbt + [ht[tt // 4]], W=[PT[pi]])
            evac_copy(Vp[:, tt, :, 0:64], PS(pi).rearrange("p (h d) -> p h d", h=8), R=[PT[pi]], W=[vpt[tt]])
        items = [(m, hh) for m in range(NT) for hh in range(8)]

        def jlist(m):
            js = [j for j in range(m - 4, m + 1) if j >= 0]
            return js, 5 - len(js)

        def stageA(i):
            m, hh = items[i]
            js, s0 = jlist(m)
            hp, r0 = hh // 2, (hh % 2) * 64
            sp_ = i % 2
            Sps = PP[sp_]
            St = [PT[2 * sp_], PT[2 * sp_ + 1]]
            for idx, j in enumerate(js):
                sl = s0 + idx
                mm(Sps[:, sl * 128:(sl + 1) * 128], qk[r0:r0 + 64, 4 + hp, j * 128:(j + 1) * 128],
                   qk[r0:r0 + 64, hp, m * 128:(m + 1) * 128], True, True,
                   R=[qkt[4 + hp][j], qkt[hp][m]], W=St)
            eb = i % 2
            kb.ins("act", lambda: nc.scalar.activation(E[eb][:, s0 * 128:640], Sps[:, s0 * 128:640], AF.Exp, scale=0.125),
                   R=St, W=[et[eb]])
            kb.ins("dve", lambda: nc.vector.tensor_tensor(out=E[eb][:, s0 * 128:640], in0=E[eb][:, s0 * 128:640],
                                                          in1=Mh[:, hh, s0 * 128:640], op=ALU.mult),
                   R=[et[eb], mht], W=[et[eb]])

        def stageB(i):
            m, hh = items[i]
            js, s0 = jlist(m)
            eb = i % 2
            og = 4 + hh // 4
            for idx, j in enumerate(js):
                sl = s0 + idx
                mm(PS(og)[:, (hh % 4) * 65:(hh % 4) * 65 + 65], E[eb][:, sl * 128:(sl + 1) * 128], Vp[:, j, hh, :],
                   idx == 0, idx == len(js) - 1, R=[et[eb], vpt[j]], W=[PT[og]])

        def epilogue(m):
            for g in range(2):
                Og = PS(4 + g)[:, 0:260].rearrange("p (h d) -> p h d", h=4)
                kb.ins("dve", lambda: nc.vector.reciprocal(rec[:, g * 4:g * 4 + 4], Og[:, :, 64]), R=[PT[4 + g]], W=[rect])
                kb.ins("dve", lambda: nc.vector.tensor_tensor(out=o[:, g * 4:g * 4 + 4, :], in0=Og[:, :, 0:64],
                                                              in1=rec[:, g * 4:g * 4 + 4].unsqueeze(2).to_broadcast([128, 4, 64]),
                                                              op=ALU.mult), R=[PT[4 + g], rect], W=[ot])
            of = o[:].rearrange("p h d -> p (h d)")
            kb.ins("pool", lambda: nc.gpsimd.memset(ssq[:, 0:1], 0.0), W=[ssqt])
            kb.ins("act", lambda: nc.scalar.activation(junk[:], of, AF.Square, accum_out=ssq[:, 0:1]), R=[ot, ssqt], W=[junkt, ssqt])
            kb.ins("act", lambda: nc.scalar.activation(ssq[:, 1:2], ssq[:, 0:1], AF.Sqrt, bias=1e-6, scale=1.0 / 512), R=[ssqt], W=[ssqt])
            kb.ins("dve", lambda: nc.vector.reciprocal(ssq[:, 1:2], ssq[:, 1:2]), R=[ssqt], W=[ssqt])
            kb.ins("dve", lambda: nc.vector.scalar_tensor_tensor(out=yo[:], in0=of, scalar=ssq[:, 1:2], in1=gbc[:],
                                                                 op0=ALU.mult, op1=ALU.mult), R=[ot, ssqt, gbct], W=[yot])
            pi = 6 + (m % 2)
            for c in range(4):
                mm(PS(pi)[:, c * 128:(c + 1) * 128], yo[:, c * 128:(c + 1) * 128], ident_f[:], True, True, R=[yot, ct], W=[PT[pi]])
            evac_copy(qk[:, 0:4, m * 128:(m + 1) * 128], PS(pi).rearrange("p (c t) -> p c t", c=4), R=[PT[pi]],
                      W=[qkt[c][m] for c in range(4)])

        stageA(0)
        for i in range(len(items)):
            if i + 1 < len(items):
                stageA(i + 1)
            stageB(i)
            if items[i][1] == 7:
                epilogue(items[i][0])
        for oc in range(8):
            slot = load_w(wout_d.ap()[l, oc])
            for tb in range(NB):
                pi = next_ps(0, 4)
                for k in range(4):
                    mm(PS(pi), wbuf[:, slot, 4 + k, :], qk[:, k, tbs(tb)], k == 0, k == 3,
                       R=[wbt[slot]] + qkt[k][tb * 4:tb * 4 + 4], W=[PT[pi]])
                kb.ins("dve", lambda: nc.vector.tensor_tensor(out=x[:, oc, tbs(tb)], in0=PS(pi), in1=x[:, oc, tbs(tb)], op=ALU.add),
                       R=[PT[pi], xt[oc][tb]], W=[xt[oc][tb]])
        kb.barrier()
        st.close()

    def rwkv_phase(l):
        BS = 256
        NBLK = T // BS
        NCH = BS // 64
        NQ = BS // 128
        NSTREAM = 2
        st = ExitStack()
        lora1 = sb("lora1", [128, T], BF16, st); l1t = [Trk() for _ in range(NB)]
        lora2 = sb("lora2", [128, T], BF16, st); l2t = [Trk() for _ in range(NB)]
        loraw = sb("loraw_s", [128, 512], BF16, st); lwt = Trk()
        g2s = sb("g2_s", [128, 512], BF16, st); g2t = Trk()
        wx = sb("r_wx", [128, 2, 8, 128], BF16, st); wxt = [Trk(), Trk()]
        GNB = sb("r_GNB", [64, 1], F32, st); GNBt = Trk()
        kb.ins("pool", lambda: nc.gpsimd.memset(GNB[:], 64e-5), W=[GNBt])
        kb.dma("pool", loraw[:], loraw_d.ap()[l], W=[lwt])
        kb.dma("pool", g2s[:], g2_d.ap()[l], W=[g2t])

        ps_free = [0, 1, 2, 3, 4, 5]

        def aps():
            return ps_free.pop(0)

        def fps(i):
            ps_free.append(i)

        def bsl(b):
            return slice(b * BS, (b + 1) * BS)

        shl = sb("shbuf_l", [128, 516], F32, st); shlt = Trk()
        carl = sb("carry_l", [128, 2], F32, st); carlt = Trk()
        tl = rstd; tlt = rstdt
        for ci, cc in enumerate((12, 13)):
            slot = load_w(win_d.ap()[l, cc])
            for tb in range(NB):
                pi = aps()
                for k in range(8):
                    mm(PS(pi), wbuf[:, slot, k, :], h[:, k, tbs(tb)], k == 0, k == 7, R=[wbt[slot], ht[tb]], W=[PT[pi]])
                mu = pv[:, l, 24 + cc:25 + cc]
                omm = pvd[:, l, cc:cc + 1]
                if tb == 0:
                    kb.ins("pool", lambda: nc.gpsimd.memset(shl[:, 0:1], 0.0), W=[shlt])
                else:
                    kb.ins("act", lambda: nc.scalar.copy(shl[:, 0:1], carl[:, ci:ci + 1]), R=[carlt], W=[shlt])
                kb.ins("act", lambda: nc.scalar.activation(shl[:, 1:513], PS(pi), AF.Identity, scale=mu), R=[PT[pi], pvt, shlt], W=[shlt])
                kb.ins("act", lambda: nc.scalar.copy(carl[:, ci:ci + 1], shl[:, 512:513]), R=[shlt], W=[carlt])
                kb.ins("dve", lambda: nc.vector.scalar_tensor_tensor(out=tl[:], in0=PS(pi), scalar=omm, in1=shl[:, 0:512],
                                                                     op0=ALU.mult, op1=ALU.add), R=[PT[pi], pvdt, shlt], W=[tlt])
                fps(pi)
                if cc == 12:
                    kb.ins("act", lambda: nc.scalar.activation(lora1[0:64, tbs(tb)], tl[0:64, :], AF.Tanh), R=[tlt], W=[l1t[tb]])
                    kb.ins("act", lambda: nc.scalar.copy(lora1[64:128, tbs(tb)], tl[64:128, :]), R=[tlt], W=[l1t[tb]])
                else:
                    kb.ins("act", lambda: nc.scalar.activation(lora2[:, tbs(tb)], tl[:], AF.Sigmoid), R=[tlt], W=[l2t[tb]])

        def stream(s, hps):
            def f32t(name):
                return sb(name, [128, BS], F32, st), Trk()

            def b16t(name):
                return sb(name, [128, BS], BF16, st), Trk()
            RKV = sb("r_RKV", [128, 3, BS], F32, st)
            Rt, Rtt = RKV[:, 0, :], Trk(); Kt, Ktt = RKV[:, 1, :], Trk(); Vt, Vtt = RKV[:, 2, :], Trk()
            SA = sb("r_SA", [128, 2, BS], F32, st)
            SG, SGt = SA[:, 0, :], Trk(); Aa, Aat = SA[:, 1, :], Trk(); Gt, Gtt = b16t("r_G")
            Yraw = RKV[0:64, 0:2, :].rearrange("p a (b v) -> p (a b) v", v=64)
            Ycb = SA[0:64, :, :].rearrange("p a (b v) -> p (a b) v", v=64)
            Ynb = sb("r_Ynb", [64, 2 * NCH, 64], BF16, st); Ynbt = Trk()
            kkn, kknt = f32t("r_kkn"); t1, t1t = f32t("r_t1"); t2, t2t = f32t("r_t2")
            eG, eGt = SG, SGt; eGx, eGxt = f32t("r_eGx"); eGn, eGnt = f32t("r_eGn")
            bv, bvt = f32t("r_bv")
            kk2, kk2t = b16t("r_kk2")
            ART = sb("r_ART", [128, NCH, 128], BF16, st); ARTt = Trk()
            bT, bTt = b16t("r_bT"); kT, kTt = b16t("r_kT"); vT, vTt = b16t("r_vT")
            sgT = eGn[:].rearrange("p (q f) -> p q f", q=NQ); sgTt = eGnt
            CA = sb("r_CA", [64, NCH, 512], BF16, st); CAt = Trk()
            CB = sb("r_CB", [64, NCH, 512], BF16, st); CBt = Trk()
            NI = 2 * NCH
            Xb = [sb("r_X%d" % i, [64, NI, 64], BF16, st) for i in range(2)]; Xbt = [Trk(), Trk()]
            Nb = [sb("r_N%d" % i, [64, NI, 64], BF16, st) for i in range(2)]; Nbt = [Trk(), Trk()]
            TTf = sb("r_TT", [64, NI, 64], BF16, st); TTft = Trk()
            ST = sb("r_ST", [64, 2, 64], F32, st); STt = Trk()
            STb = sb("r_STb", [64, 2, 64], BF16, st); STbt = Trk()
            WC = sb("r_WC", [64, 2, NCH], F32, st); WCt = Trk()
            WCs = sb("r_WCs", [128, NCH], F32, st); WCst = Trk()
            P1 = sb("r_P1", [64, 2, 64], BF16, st); P1t = Trk()
            U = sb("r_U", [64, 2, 64], BF16, st); Ut = Trk()
            Yc = sb("r_Yc", [64, 2, 64], F32, st); Yct = Trk()
            Ysq = sb("r_Ysq", [64, 2, 64], F32, st); Ysqt = Trk()
            Yn = sb("r_Yn", [64, 2, 64], BF16, st); Ynt = Trk()
            stat = sb("r_stat", [64, 8 * NCH], F32, st); statt = Trk()
            sh = sb("shbuf", [128, BS + 4], F32, st); sht = Trk()
            carry = sb("carry", [128, 4], F32, st); carryt = Trk()
            yrs = sb("yrs", [128, T], BF16, st); yrst = [Trk() for _ in range(NBLK)]
            if s == 0:
                wsl = [(wbuf[:, i, :, :], wbt[i]) for i in range(3)]
            else:
                wsl = [(wbuf[:, 3, :, :], wbt[3]), (wx[:, 0, :, :], wxt[0]), (wx[:, 1, :, :], wxt[1])]
            pyo = 6 + s

            def shifted_proj(wi, cc, b, out_ap, out_trk, ci):
                wap, wtr = wsl[wi]
                pi = aps()
                for k in range(8):
                    mm(PS(pi)[:, 0:BS], wap[:, k, :], h[:, k, bsl(b)], k == 0, k == 7, R=[wtr, ht[(b * BS) // 512]], W=[PT[pi]])
                mu = pv[:, l, 24 + cc:25 + cc]
                omm = pvd[:, l, cc:cc + 1]
                if b == 0:
                    kb.ins("pool", lambda: nc.gpsimd.memset(sh[:, 0:1], 0.0), W=[sht])
                else:
                    kb.ins("act", lambda: nc.scalar.copy(sh[:, 0:1], carry[:, ci:ci + 1]), R=[carryt], W=[sht])
                kb.ins("act", lambda: nc.scalar.activation(sh[:, 1:BS + 1], PS(pi)[:, 0:BS], AF.Identity, scale=mu), R=[PT[pi], pvt, sht], W=[sht])
                kb.ins("act", lambda: nc.scalar.copy(carry[:, ci:ci + 1], sh[:, BS:BS + 1]), R=[sht], W=[carryt])
                kb.ins("dve", lambda: nc.vector.scalar_tensor_tensor(out=out_ap, in0=PS(pi)[:, 0:BS], scalar=omm, in1=sh[:, 0:BS],
                                                                     op0=ALU.mult, op1=ALU.add), R=[PT[pi], pvdt, sht], W=[out_trk])
                fps(pi)

            for hp in hps:
                for wi, cc in enumerate((hp, 4 + hp, 8 + hp)):
                    kb.dma("pool", wsl[wi][0].rearrange("p k j -> p (k j)"), win_d.ap()[l, cc], W=[wsl[wi][1]])
                kb.ins("pool", lambda: nc.gpsimd.memset(ST[:], 0.0), W=[STt])
                kb.ins("pool", lambda: nc.gpsimd.memset(STb[:], 0.0), W=[STbt])
                w0 = pv[:, l, 52 + hp:53 + hp]; a0 = pv[:, l, 56 + hp:57 + hp]; k_k = pv[:, l, 60 + hp:61 + hp]
                k_a = pv[:, l, 64 + hp:65 + hp]; r_k = pv[:, l, 68 + hp:69 + hp]
                ln_w = pv[:, l, 72 + hp:73 + hp]; ln_b = pv[:, l, 76 + hp:77 + hp]
                omka = pvd[:, l, 14 + hp:15 + hp]
                cs = slice(hp * 128, (hp + 1) * 128)
                yield
                for b in range(NBLK):
                    tb = (b * BS) // 512
                    shifted_proj(0, hp, b, Rt[:], Rtt, 0)
                    yield
                    shifted_proj(1, 4 + hp, b, Kt[:], Ktt, 1)
                    yield
                    shifted_proj(2, 8 + hp, b, Vt[:], Vtt, 2)
                    yield
                    pi = aps()
                    mm(PS(pi)[:, 0:BS], loraw[0:64, cs], lora1[0:64, bsl(b)], True, True, R=[lwt, l1t[tb]], W=[PT[pi]])
                    kb.ins("act", lambda: nc.scalar.activation(SG[:], PS(pi)[:, 0:BS], AF.Sigmoid, bias=w0, scale=1.0), R=[PT[pi], pvt], W=[SGt])
                    fps(pi)
                    pi = aps()
                    mm(PS(pi)[:, 0:BS], loraw[64:128, cs], lora1[64:128, bsl(b)], True, True, R=[lwt, l1t[tb]], W=[PT[pi]])
                    kb.ins("act", lambda: nc.scalar.activation(Aa[:], PS(pi)[:, 0:BS], AF.Sigmoid, bias=a0, scale=1.0), R=[PT[pi], pvt], W=[Aat])
                    fps(pi)
                    pi = aps()
                    mm(PS(pi)[:, 0:BS], g2s[:, cs], lora2[:, bsl(b)], True, True, R=[g2t, l2t[tb]], W=[PT[pi]])
                    evac_copy(Gt[:], PS(pi)[:, 0:BS], R=[PT[pi]], W=[Gtt], eng="act")
                    fps(pi)
                    yield
                    kb.ins("act", lambda: nc.scalar.activation(kk2[:], Kt[:], AF.Square, scale=k_k), R=[Ktt, pvt], W=[kk2t])
                    pi = aps()
                    mm(PS(pi)[:, 0:BS], blk[:], kk2[:], True, True, R=[ct, kk2t], W=[PT[pi]])
                    kb.ins("act", lambda: nc.scalar.activation(t1[:], PS(pi)[:, 0:BS], AF.Sqrt, bias=1e-24, scale=1.0), R=[PT[pi]], W=[t1t])
                    fps(pi)
                    kb.ins("dve", lambda: nc.vector.reciprocal(t1[:], t1[:]), R=[t1t], W=[t1t])
                    kb.ins("dve", lambda: nc.vector.scalar_tensor_tensor(out=kkn[:], in0=Kt[:], scalar=k_k, in1=t1[:], op0=ALU.mult, op1=ALU.mult),
                           R=[Ktt, pvt, t1t], W=[kknt])
                    kb.ins("dve", lambda: nc.vector.tensor_scalar(t2[:], Aa[:], k_a, omka, op0=ALU.mult, op1=ALU.add), R=[Aat, pvt, pvdt], W=[t2t])
                    kb.ins("pool", lambda: nc.gpsimd.tensor_tensor(out=t2[:], in0=t2[:], in1=Kt[:], op=ALU.mult), R=[t2t, Ktt], W=[t2t])
                    kb.ins("dve", lambda: nc.vector.scalar_tensor_tensor(out=kk2[:], in0=Rt[:], scalar=r_k, in1=t2[:], op0=ALU.mult, op1=ALU.mult),
                           R=[Rtt, pvt, t2t], W=[kk2t])
                    pi = aps()
                    mm(PS(pi)[:, 0:BS], blk[:], kk2[:], True, True, R=[ct, kk2t], W=[PT[pi]])
                    kb.ins("dve", lambda: nc.vector.tensor_tensor(out=bv[:], in0=PS(pi)[:, 0:BS], in1=Vt[:], op=ALU.mult), R=[PT[pi], Vtt], W=[bvt])
                    fps(pi)
                    yield
                    pi = aps()
                    for q4 in range(NQ):
                        mm(PS(pi)[:, q4 * 128:(q4 + 1) * 128], SG[:, q4 * 128:(q4 + 1) * 128], ident_f[:], True, True, R=[SGt, ct], W=[PT[pi]])
                    evac_copy(sgT, PS(pi)[:, 0:BS].rearrange("p (q f) -> p q f", q=NQ), R=[PT[pi]], W=[sgTt], eng="act")
                    fps(pi)
                    pg = aps(); pgx = aps()
                    for q4 in range(NQ):
                        mm(PS(pg)[:, q4 * 128:(q4 + 1) * 128], sgT[:, q4, :], tri_i[:], True, True, R=[sgTt, ct], W=[PT[pg]])
                    for q4 in range(NQ):
                        mm(PS(pgx)[:, q4 * 128:(q4 + 1) * 128], sgT[:, q4, :], tri_e[:], True, True, R=[sgTt, ct], W=[PT[pgx]])
                    kb.ins("act", lambda: nc.scalar.activation(eG[:], PS(pg)[:, 0:BS], AF.Exp, scale=-DECAY_C), R=[PT[pg]], W=[eGt])
                    kb.ins("act", lambda: nc.scalar.activation(eGn[:], PS(pg)[:, 0:BS], AF.Exp, scale=DECAY_C), R=[PT[pg]], W=[eGnt])
                    kb.ins("act", lambda: nc.scalar.activation(eGx[:], PS(pgx)[:, 0:BS], AF.Exp, scale=-DECAY_C), R=[PT[pgx]], W=[eGxt])
                    fps(pg); fps(pgx)
                    yield
                    ARTv = ART[:]
                    kb.ins("dve", lambda: nc.vector.scalar_tensor_tensor(out=ARTv[:, :, 0:64], in0=kkn[:].rearrange("p (c t) -> p c t", c=NCH), scalar=-1.0,
                                                                         in1=eGx[:].rearrange("p (c t) -> p c t", c=NCH), op0=ALU.mult, op1=ALU.mult),
                           R=[kknt, eGxt], W=[ARTt])
                    kb.ins("pool", lambda: nc.gpsimd.tensor_tensor(out=ARTv[:, :, 64:128], in0=Rt[:].rearrange("p (c t) -> p c t", c=NCH),
                                                                   in1=eG[:].rearrange("p (c t) -> p c t", c=NCH), op=ALU.mult), R=[Rtt, eGt], W=[ARTt])
                    kb.ins("dve", lambda: nc.vector.tensor_tensor(out=Aa[:], in0=kkn[:], in1=Aa[:], op=ALU.mult), R=[kknt, Aat], W=[Aat])
                    kb.ins("dve", lambda: nc.vector.tensor_tensor(out=bT[:], in0=Aa[:], in1=eGn[:], op=ALU.mult), R=[Aat, eGnt], W=[bTt])
                    kb.ins("pool", lambda: nc.gpsimd.tensor_tensor(out=kT[:], in0=t2[:], in1=eGn[:], op=ALU.mult), R=[t2t, eGnt], W=[kTt])
                    kb.ins("act", lambda: nc.scalar.copy(vT[:], Vt[:]), R=[Vtt], W=[vTt])
                    yield
                    kkb = kkn[:].bitcast(BF16)
                    t2b = t2[:].bitcast(BF16)
                    exb = eGx[:].bitcast(BF16)
                    ART1 = kkb[0:64, :].rearrange("p (c t) -> p c t", c=NCH)
                    bT1 = t2b[0:64, 0:BS]; kT1 = t2b[0:64, BS:2 * BS]; vT1 = exb[0:64, 0:BS]
                    ARTf = ART[:].rearrange("p c t -> p (c t)")
                    for (src, srct, dst, dstt, n) in ((ARTf, ARTt, kkb[0:64, :], kknt, 2 * BS), (bT[:], bTt, bT1, t2t, BS),
                                                    (kT[:], kTt, kT1, t2t, BS), (vT[:], vTt, vT1, eGxt, BS)):
                        pi = aps()
                        mm(PS(pi)[0:64, 0:n], ident_b[64:128, 64:128], src[64:128, :], True, True, R=[ct, srct], W=[PT[pi]])
                        evac_copy(dst, PS(pi)[0:64, 0:n], R=[PT[pi]], W=[dstt])
                        fps(pi)
                    pi = aps()
                    eGl = eG[:].rearrange("p (c t) -> p c t", c=NCH)[:, :, 63]
                    kb.ins("dve", lambda: nc.vector.tensor_copy(WCs[:], eGl), R=[eGt], W=[WCst])
                    mm(PS(pi)[0:64, 0:NCH], ident_f[64:128, 64:128], WCs[64:128, :], True, True, R=[ct, WCst], W=[PT[pi]])
                    kb.ins("dve", lambda: nc.vector.tensor_copy(WC[:, 0, :], WCs[0:64, :]), R=[WCst], W=[WCt])
                    kb.ins("dve", lambda: nc.vector.tensor_copy(WC[:, 1, :], PS(pi)[0:64, 0:NCH]), R=[PT[pi]], W=[WCt])
                    fps(pi)
                    yield

                    def opnd(hh):
                        if hh == 0:
                            return ART[0:64], bT[0:64, :], kT[0:64, :], vT[0:64, :], [ARTt, bTt, kTt, vTt]
                        return ART1, bT1, kT1, vT1, [kknt, t2t, t2t, eGxt]
                    for c in range(NCH):
                        pa = aps(); pb = aps()
                        cs64 = slice(c * 64, c * 64 + 64)
                        for hh in range(2):
                            ARh, bh, kh, vh, trs = opnd(hh)
                            A_ = PS(pa)[0:64, hh * 256:(hh + 1) * 256]
                            B_ = PS(pb)[0:64, hh * 256:(hh + 1) * 256]
                            mm(A_[:, 0:128], bh[:, cs64], ARh[:, c, :], True, True, R=trs, W=[PT[pa]])
                            mm(A_[:, 128:256], kh[:, cs64], ARh[:, c, :], True, True, R=trs, W=[PT[pa]])
                            mm(B_[:, 0:64], ARh[:, c, 0:64], bh[:, cs64], True, True, R=trs, W=[PT[pb]])
                            mm(B_[:, 64:128], bh[:, cs64], ident_b[0:64, 0:64], True, True, R=trs + [ct], W=[PT[pb]])
                            mm(B_[:, 128:192], kh[:, cs64], ident_b[0:64, 0:64], True, True, R=trs + [ct], W=[PT[pb]])
                            mm(B_[:, 192:256], vh[:, cs64], ident_b[0:64, 0:64], True, True, R=trs + [ct], W=[PT[pb]])
                        kb.ins("dve", lambda: nc.vector.tensor_tensor(out=CA[:, c, :], in0=PS(pa)[0:64, :], in1=maskA[:], op=ALU.mult),
                               R=[PT[pa], ct], W=[CAt])
                        kb.ins("dve", lambda: nc.vector.tensor_tensor(out=CB[:, c, :], in0=PS(pb)[0:64, :], in1=maskB[:], op=ALU.mult),
                               R=[PT[pb], ct], W=[CBt])
                        fps(pa); fps(pb)
                        yield
                    CAv = CA[:].rearrange("p c (h f) -> p (c h) f", h=2)
                    CBv = CB[:].rearrange("p c (h f) -> p (c h) f", h=2)
                    kb.ins("pool", lambda: nc.gpsimd.tensor_copy(Xb[0][:], CAv[:, :, 0:64]), R=[CAt], W=[Xbt[0]])
                    kb.ins("pool", lambda: nc.gpsimd.tensor_copy(Nb[0][:], CBv[:, :, 0:64]), R=[CBt], W=[Nbt[0]])
                    kb.ins("dve", lambda: nc.vector.tensor_tensor(out=TTf[:], in0=CAv[:, :, 0:64],
                                                                  in1=ident_b[0:64, 0:64].unsqueeze(1).to_broadcast([64, NI, 64]), op=ALU.add),
                           R=[CAt, ct], W=[TTft])
                    cur = 0
                    for lev in range(5):
                        nx = 1 - cur
                        px = aps(); pn = aps()
                        if lev < 4:
                            for i in range(NI):
                                mm(PS(px)[0:64, i * 64:(i + 1) * 64], Nb[cur][:, i, :], Xb[cur][:, i, :], True, True,
                                   R=[Nbt[cur], Xbt[cur]], W=[PT[px]])
                        for i in range(NI):
                            mm(PS(pn)[0:64, i * 64:(i + 1) * 64], Xb[cur][:, i, :], Nb[cur][:, i, :], True, True,
                               R=[Nbt[cur], Xbt[cur]], W=[PT[pn]])
                        if lev < 4:
                            evac_copy(Xb[nx][:], PS(px)[0:64, 0:NI * 64].rearrange("p (i f) -> p i f", i=NI), R=[PT[px]], W=[Xbt[nx]], eng="act")
                        evac_copy(Nb[nx][:], PS(pn)[0:64, 0:NI * 64].rearrange("p (i f) -> p i f", i=NI), R=[PT[pn]], W=[Nbt[nx]], eng="dve")
                        fps(px); fps(pn)
                        yield
                        pt = aps()
                        for i in range(NI):
                            mm(PS(pt)[0:64, i * 64:(i + 1) * 64], Nb[nx][:, i, :], TTf[:, i, :], True, True,
                               R=[Nbt[nx], TTft], W=[PT[pt]])
                        kb.ins("dve", lambda: nc.vector.tensor_tensor(out=TTf[:], in0=PS(pt)[0:64, 0:NI * 64].rearrange("p (i f) -> p i f", i=NI),
                                                                      in1=TTf[:], op=ALU.add), R=[PT[pt], TTft], W=[TTft])
                        fps(pt)
                        cur = nx
                        yield
                    W1 = Xb[0]; W1t = Xbt[0]; atok = Xb[1]; atokt = Xbt[1]; Ub = Nb[0]; Ubt = Nbt[0]; Atok = Nb[1]; Atokt = Nbt[1]
                    PhiT = W1; PhiTt = W1t; RpT = atok; RpTt = atokt
                    psA = aps(); psB = aps()
                    for c in range(NCH):
                        for hh in range(2):
                            ARh, bh, kh, vh, trs = opnd(hh)
                            i = 2 * c + hh
                            mm(PS(psA)[0:64, i * 64:(i + 1) * 64], CA[:, c, hh * 256 + 128:hh * 256 + 192], CB[:, c, hh * 256 + 192:hh * 256 + 256], True, True,
                               R=[CAt, CBt], W=[PT[psA]])
                            mm(PS(psB)[0:64, i * 64:(i + 1) * 64], ARh[:, c, 0:64], ident_b[0:64, 0:64], True, True, R=trs + [ct], W=[PT[psB]])
                    evac_copy(W1[:], PS(psA)[0:64, 0:NI * 64].rearrange("p (i f) -> p i f", i=NI), R=[PT[psA]], W=[W1t], eng="act")
                    evac_copy(atok[:], PS(psB)[0:64, 0:NI * 64].rearrange("p (i f) -> p i f", i=NI), R=[PT[psB]], W=[atokt], eng="dve")
                    fps(psA); fps(psB)
                    yield
                    psA = aps(); psB = aps()
                    for i in range(NI):
                        mm(PS(psA)[0:64, i * 64:(i + 1) * 64], TTf[:, i, :], W1[:, i, :], True, True, R=[TTft, W1t], W=[PT[psA]])
                        mm(PS(psB)[0:64, i * 64:(i + 1) * 64], TTf[:, i, :], atok[:, i, :], True, True, R=[TTft, atokt], W=[PT[psB]])
                    evac_copy(Ub[:], PS(psA)[0:64, 0:NI * 64].rearrange("p (i f) -> p i f", i=NI), R=[PT[psA]], W=[Ubt], eng="act")
                    evac_copy(Atok[:], PS(psB)[0:64, 0:NI * 64].rearrange("p (i f) -> p i f", i=NI), R=[PT[psB]], W=[Atokt], eng="dve")
                    fps(psA); fps(psB)
                    yield
                    psA = aps(); psB = aps()
                    for c in range(NCH):
                        for hh in range(2):
                            i = 2 * c + hh
                            mm(PS(psA)[0:64, i * 64:(i + 1) * 64], Atok[:, i, :], CB[:, c, hh * 256 + 64:hh * 256 + 128], True, True,
                               R=[Atokt, CBt], W=[PT[psA]])
                            mm(PS(psB)[0:64, i * 64:(i + 1) * 64], Atok[:, i, :], CA[:, c, hh * 256 + 64:hh * 256 + 128], True, True,
                               R=[Atokt, CAt], W=[PT[psB]])
                    evac_copy(PhiT[:], PS(psA)[0:64, 0:NI * 64].rearrange("p (i f) -> p i f", i=NI), R=[PT[psA]], W=[PhiTt], eng="act")
                    for hh in range(2):
                        ARh, bh, kh, vh, trs = opnd(hh)
                        kb.ins("dve", lambda: nc.vector.tensor_tensor(
                            out=RpT[:].rearrange("p (c h) f -> p c h f", h=2)[:, :, hh, :],
                            in0=PS(psB)[0:64, 0:NI * 64].rearrange("p (c h f) -> p c h f", h=2, f=64)[:, :, hh, :],
                            in1=ARh[:, :, 64:128], op=ALU.add), R=[PT[psB]] + trs, W=[RpTt])
                    fps(psA); fps(psB)
                    yield
                    for c in range(NCH):
                        pp = aps()
                        Yps = PS(pp)[0:64, 0:128]
                        pst = PS(pp)[0:64, 128:256]
                        for hh in range(2):
                            i = 2 * c + hh
                            fo = slice(hh * 64, hh * 64 + 64)
                            mm(pst[:, fo], CB[:, c, hh * 256 + 64:hh * 256 + 128], Ub[:, i, :], True, False, R=[CBt, Ubt], W=[PT[pp]])
                            mm(pst[:, fo], CB[:, c, hh * 256 + 128:hh * 256 + 192], CB[:, c, hh * 256 + 192:hh * 256 + 256], False, False,
                               R=[CBt], W=[PT[pp]])
                            mm(pst[:, fo], PhiT[:, i, :], STb[:, hh, :], False, True, R=[PhiTt, STbt], W=[PT[pp]])
                        for hh in range(2):
                            i = 2 * c + hh
                            fo = slice(hh * 64, hh * 64 + 64)
                            mm(Yps[:, fo], CA[:, c, hh * 256 + 64:hh * 256 + 128], Ub[:, i, :], True, False, R=[CAt, Ubt], W=[PT[pp]])
                            mm(Yps[:, fo], CA[:, c, hh * 256 + 192:hh * 256 + 256], CB[:, c, hh * 256 + 192:hh * 256 + 256], False, False,
                               R=[CAt, CBt], W=[PT[pp]])
                            mm(Yps[:, fo], RpT[:, i, :], STb[:, hh, :], False, True, R=[RpTt, STbt], W=[PT[pp]])
                        STf = ST[:].rearrange("p h v -> p (h v)")
                        kb.ins("dve", lambda: nc.vector.tensor_tensor(out=STf, in0=pst, in1=STf, op=ALU.add), R=[PT[pp], STt], W=[STt])
                        kb.ins("dve", lambda: nc.vector.tensor_tensor(out=ST[:], in0=ST[:], in1=WC[:, :, c:c + 1].to_broadcast([64, 2, 64]), op=ALU.mult),
                               R=[STt, WCt], W=[STt])
                        kb.ins("act", lambda: nc.scalar.copy(STb[:], ST[:]), R=[STt], W=[STbt])
                        kb.ins("act", lambda: nc.scalar.copy(Yraw[:, 2 * c:2 * c + 2, :], Yps.rearrange("p (h v) -> p h v", h=2)),
                               R=[PT[pp]], W=[Rtt, Ktt])
                        fps(pp)
                        yield
                    NI2 = 2 * NCH
                    kb.ins("dve", lambda: nc.vector.tensor_reduce(out=stat[:, 0:NI2], in_=Yraw, axis=AX.X, op=ALU.add), R=[Rtt, Ktt], W=[statt])
                    kb.ins("dve", lambda: nc.vector.tensor_scalar(stat[:, NI2:2 * NI2], stat[:, 0:NI2], -1.0 / 64, None, op0=ALU.mult), R=[statt], W=[statt])
                    kb.ins("pool", lambda: nc.gpsimd.tensor_tensor(out=Ycb, in0=Yraw, in1=stat[:, NI2:2 * NI2].unsqueeze(2).to_broadcast([64, NI2, 64]), op=ALU.add),
                           R=[Rtt, Ktt, statt], W=[SGt, Aat])
                    yield
                    kb.ins("pool", lambda: nc.gpsimd.tensor_tensor(out=Yraw, in0=Ycb, in1=Ycb, op=ALU.mult), R=[SGt, Aat], W=[Rtt, Ktt])
                    kb.ins("dve", lambda: nc.vector.tensor_reduce(out=stat[:, 2 * NI2:3 * NI2], in_=Yraw, axis=AX.X, op=ALU.add), R=[Rtt, Ktt], W=[statt])
                    kb.ins("act", lambda: nc.scalar.activation(stat[:, 3 * NI2:4 * NI2], stat[:, 2 * NI2:3 * NI2], AF.Sqrt, bias=GNB[:], scale=1.0 / 64),
                           R=[statt, GNBt], W=[statt])
                    yield
                    kb.ins("dve", lambda: nc.vector.reciprocal(stat[:, 3 * NI2:4 * NI2], stat[:, 3 * NI2:4 * NI2]), R=[statt], W=[statt])
                    kb.ins("pool", lambda: nc.gpsimd.tensor_tensor(out=Ynb[:], in0=Ycb, in1=stat[:, 3 * NI2:4 * NI2].unsqueeze(2).to_broadcast([64, NI2, 64]), op=ALU.mult),
                           R=[SGt, Aat, statt], W=[Ynbt])
                    for c in range(NCH):
                        mm(PS(pyo)[:, c * 64:(c + 1) * 64], Ynb[:, 2 * c:2 * c + 2, :].rearrange("p h v -> p (h v)"), ident_b[0:64, 0:64], True, True,
                           R=[Ynbt, ct], W=[PT[pyo]])
                    yield
                    kb.ins("act", lambda: nc.scalar.activation(t1[:], PS(pyo)[:, 0:BS], AF.Identity, bias=ln_b, scale=ln_w), R=[PT[pyo], pvt], W=[t1t])
                    kb.ins("dve", lambda: nc.vector.tensor_tensor(out=t1[:], in0=t1[:], in1=bv[:], op=ALU.add), R=[t1t, bvt], W=[t1t])
                    kb.ins("dve", lambda: nc.vector.tensor_tensor(out=yrs[:, bsl(b)], in0=t1[:], in1=Gt[:], op=ALU.mult), R=[t1t, Gtt], W=[yrst[b]])
                    yield
                wap, wtr = wsl[0]
                kb.dma("pool", wap, wout_d.ap()[l].rearrange("o p (k j) -> p o k j", k=8)[:, :, hp, :], W=[wtr])
                for oc in range(8):
                    for tb in range(NB):
                        pi = aps()
                        mm(PS(pi), wap[:, oc, :], yrs[:, tbs(tb)], True, True, R=[wtr] + yrst[tb * 2:tb * 2 + 2], W=[PT[pi]])
                        kb.ins("dve", lambda: nc.vector.tensor_tensor(out=x[:, oc, tbs(tb)], in0=PS(pi), in1=x[:, oc, tbs(tb)], op=ALU.add),
                               R=[PT[pi], xt[oc][tb]], W=[xt[oc][tb]])
                        fps(pi)
                    yield

        gens = [stream(0, [0, 1]), stream(1, [2, 3])]
        alive = list(gens)
        first = True
        for _ in range(0):
            next(gens[0])
        while alive:
            for g in list(alive):
                try:
                    next(g)
                except StopIteration:
                    alive.remove(g)
            if first:
                first = False
                kb.min_free = min(getattr(kb, "min_free", 1 << 30), nc.sbuf_bytes_remaining)
        kb.barrier()
        st.close()

    def ffn_phase(l, moe):
        st = ExitStack()
        ne = 8 if moe else 2
        wg_d, wu_d, wd_d = ew_d[1 if moe else 0]
        hid = sb("hid", [128, 11, T], BF16, st); hidt = [[Trk() for _ in range(NB)] for _ in range(11)]
        slu = [sb("slu%d" % i, [128, 512], F32, st) for i in range(2)]; slut = [Trk(), Trk()]
        wdn = sb("wdn", [128, 11, 8, 128], BF16, st); wdnt = [Trk() for _ in range(11)]
        if moe:
            cbc = sb("cbc", [128, T], F32, st); cbct = [Trk() for _ in range(NB)]
            wdn_dummy = None
            combT = sb("combT", [8, T], BF16, st); combTt = [Trk() for _ in range(NT)]
            rtr = sb("rtr", [128, 8, 8], F32, st); rtrt = Trk()
            lg = sb("lg", [128, 8], F32, st); lgt = Trk()
            top = sb("top8", [128, 8], F32, st); topt = Trk()
            gts = sb("gts", [128, 4], F32, st); gtst = Trk()
            eq1 = sb("eq1", [128, 8], F32, st); eq1t = Trk()
            eq2 = sb("eq2", [128, 8], F32, st); eq2t = Trk()
            comb = sb("comb", [128, 8], F32, st); combt = Trk()
            xsq = sb("xsq", [128, 128], BF16, st); xsqt = Trk()
            rs = sb("rs_tok", [128, 2], F32, st); rst = Trk()
            kb.dma("sp", rtr[:].rearrange("p k e -> p (k e)"), router_d.ap(), W=[rtrt])
            for k in range(8):
                kb.ins("dve", lambda k=k: nc.vector.tensor_scalar(rtr[:, k, :], rtr[:, k, :], pv[:, l, 8 + k:9 + k], None, op0=ALU.mult),
                       R=[rtrt, pvt], W=[rtrt])
            lg3 = sb("lg3", [128, NT, 8], F32, st); lg3t = Trk()
            lgb = sb("lgb", [128, NT, 8], F32, st); lgbt = Trk()
            e1 = sb("e1", [128, NT, 8], F32, st); e1t = Trk()
            e2 = sb("e2", [128, NT, 8], F32, st); e2t = Trk()
            tp = sb("tp", [128, 6, NT], F32, st); tpt = Trk()
            PSl = PS(7).rearrange("p (t e) -> p t e", e=16)[:, 0:NT, :]

            def router_tb(tb):
                for q in range(4):
                    tt = tb * 4 + q
                    tsl = slice(tt * 128, (tt + 1) * 128)
                    for k in range(8):
                        mm(PSl[:, tt, 0:8], x[:, k, tsl], rtr[:, k, :], k == 0, k == 7, R=[xt[k][tb], rtrt], W=[PT[7]])
                    mm(PSl[:, tt, 8:9], rstd[0:1, q * 128:(q + 1) * 128], ident_f[0:1, 0:1], True, True, R=[rstdt, ct], W=[PT[7]])
            rmsnorm_to_h(l, 8, after_tb=router_tb)
            kb.ins("act", lambda: nc.scalar.copy(tp[:, 0, :], PSl[:, :, 8]), R=[PT[7]], W=[tpt])
            kb.ins("dve", lambda: nc.vector.tensor_tensor(out=lg3[:], in0=PSl[:, :, 0:8], in1=tp[:, 0, :].unsqueeze(2).to_broadcast([128, NT, 8]), op=ALU.mult),
                   R=[PT[7], tpt], W=[lg3t])
            kb.ins("dve", lambda: nc.vector.tensor_reduce(out=tp[:, 1, :], in_=lg3[:], axis=AX.X, op=ALU.max), R=[lg3t], W=[tpt])
            kb.ins("dve", lambda: nc.vector.tensor_tensor(out=e1[:], in0=lg3[:], in1=tp[:, 1, :].unsqueeze(2).to_broadcast([128, NT, 8]), op=ALU.is_equal),
                   R=[lg3t, tpt], W=[e1t])
            kb.ins("dve", lambda: nc.vector.scalar_tensor_tensor(out=lgb[:], in0=e1[:], scalar=-1e30, in1=lg3[:], op0=ALU.mult, op1=ALU.add),
                   R=[e1t, lg3t], W=[lgbt])
            kb.ins("dve", lambda: nc.vector.tensor_reduce(out=tp[:, 2, :], in_=lgb[:], axis=AX.X, op=ALU.max), R=[lgbt], W=[tpt])
            kb.ins("dve", lambda: nc.vector.tensor_tensor(out=e2[:], in0=lgb[:], in1=tp[:, 2, :].unsqueeze(2).to_broadcast([128, NT, 8]), op=ALU.is_equal),
                   R=[lgbt, tpt], W=[e2t])
            kb.ins("dve", lambda: nc.vector.tensor_tensor(out=tp[:, 3, :], in0=tp[:, 2, :], in1=tp[:, 1, :], op=ALU.subtract), R=[tpt], W=[tpt])
            kb.ins("act", lambda: nc.scalar.activation(tp[:, 3, :], tp[:, 3, :], AF.Exp), R=[tpt], W=[tpt])
            kb.ins("dve", lambda: nc.vector.tensor_scalar(tp[:, 3, :], tp[:, 3, :], 1.0, None, op0=ALU.add), R=[tpt], W=[tpt])
            kb.ins("dve", lambda: nc.vector.reciprocal(tp[:, 4, :], tp[:, 3, :]), R=[tpt], W=[tpt])
            kb.ins("dve", lambda: nc.vector.tensor_scalar(tp[:, 5, :], tp[:, 4, :], -1.0, 1.0, op0=ALU.mult, op1=ALU.add), R=[tpt], W=[tpt])
            kb.ins("dve", lambda: nc.vector.tensor_tensor(out=e1[:], in0=e1[:], in1=tp[:, 4, :].unsqueeze(2).to_broadcast([128, NT, 8]), op=ALU.mult),
                   R=[e1t, tpt], W=[e1t])
            kb.ins("dve", lambda: nc.vector.tensor_tensor(out=e2[:], in0=e2[:], in1=tp[:, 5, :].unsqueeze(2).to_broadcast([128, NT, 8]), op=ALU.mult),
                   R=[e2t, tpt], W=[e2t])
            kb.ins("dve", lambda: nc.vector.tensor_tensor(out=e1[:], in0=e1[:], in1=e2[:], op=ALU.add), R=[e1t, e2t], W=[e1t])
            for tt in range(NT):
                bnk = tt // 4
                mm(PS(bnk)[0:8, (tt % 4) * 128:(tt % 4 + 1) * 128], e1[:, tt, :], ident_f[:], True, True, R=[e1t, ct], W=[PT[bnk]])
            for bnk in range(4):
                evac_copy(combT[:, bnk * 512:(bnk + 1) * 512], PS(bnk)[0:8, :], R=[PT[bnk]], W=combTt[bnk * 4:bnk * 4 + 4])
        else:
            rmsnorm_to_h(l, 8)
        steps = [(e, hc) for e in range(ne) for hc in range(11)]

        def issue_loads(i):
            e_, hc_ = steps[i]
            base = 2 * (i % 2)
            load_w(wg_d.ap()[e_, hc_], slot=base)
            load_w(wu_d.ap()[e_, hc_], slot=base + 1)
        issue_loads(0)
        for i, (e, hc) in enumerate(steps):
            if hc == 0 and moe:
                for tb in range(NB):
                    pi = next_ps()
                    mm(PS(pi), sel[:, e * 128:(e + 1) * 128], combT[:, tbs(tb)], True, True, R=[ct] + combTt[tb * 4:tb * 4 + 4], W=[PT[pi]])
                    evac_copy(cbc[:, tbs(tb)], PS(pi), R=[PT[pi]], W=[cbct[tb]], eng="act")
            if i + 1 < len(steps):
                issue_loads(i + 1)
            if hc >= 1:
                for hq in ((0, 1) if hc == 1 else (hc,)):
                    kb.dma("pool", wdn[:, hq, :, :].rearrange("p o j -> p (o j)"), wd_d.ap()[e, hq], W=[wdnt[hq]])
            sg_ = 2 * (i % 2); su_ = sg_ + 1
            for tb in range(NB):
                pg = next_ps(); pu = next_ps()
                while pu == pg:
                    pu = next_ps()
                for k in range(8):
                    mm(PS(pg), wbuf[:, sg_, k, :], h[:, k, tbs(tb)], k == 0, k == 7, R=[wbt[sg_], ht[tb]], W=[PT[pg]])
                for k in range(8):
                    mm(PS(pu), wbuf[:, su_, k, :], h[:, k, tbs(tb)], k == 0, k == 7, R=[wbt[su_], ht[tb]], W=[PT[pu]])
                s = (hc * NB + tb) % 2
                kb.ins("act", lambda: nc.scalar.activation(slu[s][:], PS(pg), AF.Silu), R=[PT[pg]], W=[slut[s]])
                if moe:
                    kb.ins("dve", lambda: nc.vector.tensor_tensor(out=slu[s][:], in0=slu[s][:], in1=cbc[:, tbs(tb)], op=ALU.mult),
                           R=[slut[s], cbct[tb]], W=[slut[s]])
                kb.ins("dve", lambda: nc.vector.tensor_tensor(out=hid[:, hc, tbs(tb)], in0=PS(pu), in1=slu[s][:], op=ALU.mult),
                       R=[PT[pu], slut[s]], W=[hidt[hc][tb]])
            if hc == 10:
                down_proj(e, wd_d, hid, hidt, wdn, wdnt)
        kb.barrier()
        st.close()

    def down_proj(e, wd_d, hid, hidt, wdn, wdnt):
        for ocg in range(2):
            for tbg in range(2):
                for hc in range(11):
                    for oi in range(4):
                        oc = ocg * 4 + oi
                        for ti in range(2):
                            tb = tbg * 2 + ti
                            pi = oi * 2 + ti
                            mm(PS(pi), wdn[:, hc, oc, :], hid[:, hc, tbs(tb)], hc == 0, hc == 10,
                               R=[wdnt[hc], hidt[hc][tb]], W=[PT[pi]])
                for oi in range(4):
                    oc = ocg * 4 + oi
                    for ti in range(2):
                        tb = tbg * 2 + ti
                        pi = oi * 2 + ti
                        kb.ins("dve", lambda: nc.vector.tensor_tensor(out=x[:, oc, tbs(tb)], in0=PS(pi), in1=x[:, oc, tbs(tb)], op=ALU.add),
                               R=[PT[pi], xt[oc][tb]], W=[xt[oc][tb]])

    def final_phase():
        for tb in range(NB):
            pi = next_ps()
            for c in range(8):
                s = c % 2
                kb.ins("act", lambda c=c, s=s: nc.scalar.activation(sq[s][:], x[:, c, tbs(tb)], AF.Square), R=[xt[c][tb]], W=[sqt[s]])
                mm(PS(pi), onesm[:], sq[s][:], c == 0, c == 7, R=[ct, sqt[s]], W=[PT[pi]])
            kb.ins("act", lambda: nc.scalar.activation(rstd[:], PS(pi), AF.Sqrt, bias=1e-6, scale=1.0), R=[PT[pi]], W=[rstdt])
            kb.ins("dve", lambda: nc.vector.reciprocal(rstd[:], rstd[:]), R=[rstdt], W=[rstdt])
            for c in range(8):
                kb.ins("dve", lambda c=c: nc.vector.scalar_tensor_tensor(
                    out=x[:, c, tbs(tb)], in0=x[:, c, tbs(tb)], scalar=pv[:, 0, 16 + c:17 + c], in1=rstd[:],
                    op0=ALU.mult, op1=ALU.mult), R=[xt[c][tb], pvt, rstdt], W=[xt[c][tb]])
                kb.dma("sp", out_d.ap()[c * 128:(c + 1) * 128, tbs(tb)], x[:, c, tbs(tb)], R=[xt[c][tb]])

    def store_x():
        for c in range(8):
            kb.dma("sp", out_d.ap()[c * 128:(c + 1) * 128, :], x[:, c, :], R=xt[c])

    done = False
    for l in range(n_layers):
        rmsnorm_to_h(l, 0)
        attention_phase(l)
        if stop_after == "attn%d" % l:
            done = True
            break
        rwkv_phase(l)
        if stop_after == "mix%d" % l:
            done = True
            break
        ffn_phase(l, moe=(l % 2 == 1))
        if stop_after == "ffn%d" % l:
            done = True
            break
    if not done:
        final_phase()
    else:
        store_x()
    kb.finish()
    kb.close()
    return kb


def host_inputs(inp):
    f = lambda a: np.ascontiguousarray(a, dtype=np.float32)
    L = 2
    w_in = inp["w_in"]
    win = f(w_in.reshape(L, 8, 128, 26, 128).transpose(0, 3, 2, 1, 4).reshape(L, 26, 128, 1024))
    wv = f(win[:, 22:26].reshape(L, 4, 128, 1024).transpose(0, 2, 1, 3).reshape(L, 128, 4096))
    wout = f(inp["w_out"].reshape(L, 8, 128, 8, 128).transpose(0, 3, 2, 1, 4).reshape(L, 8, 128, 1024))
    loraw = f(np.concatenate([inp["rwkv_w2"], inp["rwkv_a2"]], axis=1))
    g2 = f(inp["rwkv_g2"])

    def experts(wg, wu, wd, ne):
        a = f(wg.reshape(ne, 8, 128, 11, 128).transpose(0, 3, 2, 1, 4).reshape(ne, 11, 128, 1024))
        b = f(wu.reshape(ne, 8, 128, 11, 128).transpose(0, 3, 2, 1, 4).reshape(ne, 11, 128, 1024))
        c = f(wd.reshape(ne, 11, 128, 1024))
        return a, b, c
    dg = inp["ffn_w_gate"][0].reshape(1024, 2, 1408).transpose(1, 0, 2)
    du = inp["ffn_w_up"][0].reshape(1024, 2, 1408).transpose(1, 0, 2)
    dd = inp["ffn_w_down"][0].reshape(2, 1408, 1024)
    wg0, wu0, wd0 = experts(dg, du, dd, 2)
    wg1, wu1, wd1 = experts(inp["moe_w_gate"][0], inp["moe_w_up"][0], inp["moe_w_down"][0], 8)
    router = f(inp["moe_router"][0].reshape(8, 128, 8).transpose(1, 0, 2).reshape(128, 64))

    pv = np.zeros((L, 128, NPV), np.float32)
    col = lambda v: v.reshape(-1, 128).T
    for l in range(L):
        pv[l, :, 0:8] = col(inp["norm_mix_g"][l])
        pv[l, :, 8:16] = col(inp["norm_ffn_g"][l])
        pv[l, :, 16:24] = col(inp["norm_final_g"])
        pv[l, :, 24:38] = col(inp["shift_mu"][l])
        pv[l, :, 52:56] = col(inp["rwkv_w0"][l])
        pv[l, :, 56:60] = col(inp["rwkv_a0"][l])
        pv[l, :, 60:64] = col(inp["rwkv_k_k"][l])
        pv[l, :, 64:68] = col(inp["rwkv_k_a"][l])
        pv[l, :, 68:72] = col(inp["rwkv_r_k"][l])
        pv[l, :, 72:76] = col(inp["rwkv_ln_w"][l])
        pv[l, :, 76:80] = col(inp["rwkv_ln_b"][l])
    attn_g_bc = f(np.broadcast_to(inp["attn_norm_g"][:, None, :], (L, 128, 512)))
    ki = np.arange(128)[:, None]
    qi = np.arange(128)[None, :]
    biasT = np.zeros((L, 8, 128, 5, 128), np.float32)
    for s in range(5):
        rel = 128 * (4 - s) + qi - ki
        idx = np.clip(rel, -128, 128) + 128
        valid = np.ones((128, 128), bool)
        if s == 4:
            valid = ~((ki >= 64) & (qi < 64))
        if s == 0:
            valid = ~((ki < 64) & (qi >= 64))
        for l in range(L):
            g = inp["attn_rel_bias"][l][idx]
            g = np.where(valid[:, :, None], g, np.float32(-1e30))
            biasT[l, :, :, s, :] = g.transpose(2, 0, 1)
    biasT = f(biasT.reshape(L, 8, 128, 640))
    ident = np.eye(128, dtype=np.float32)
    onesm = np.full((128, 128), 1.0 / 1024, np.float32)
    blk = np.kron(np.eye(2, dtype=np.float32), np.ones((64, 64), np.float32))
    s_ = np.arange(128)[:, None]; t_ = np.arange(128)[None, :]
    same = (s_ // 64) == (t_ // 64)
    tri_i = (same & (s_ <= t_)).astype(np.float32)
    tri_e = (same & (s_ < t_)).astype(np.float32)
    s6 = np.arange(64)[:, None]; t6 = np.arange(64)[None, :]
    strict = (s6 < t6).astype(np.float32); incl = (s6 <= t6).astype(np.float32)
    lower = (t6 < s6).astype(np.float32)
    mA = np.concatenate([strict, incl, strict, incl], axis=1)
    maskA = np.concatenate([mA, mA], axis=1)
    mB = np.concatenate([lower, np.ones((64, 192), np.float32)], axis=1)
    maskB = np.concatenate([mB, mB], axis=1)
    sel = np.zeros((8, 8, 128), np.float32)
    for e in range(8):
        sel[e, e, :] = 1.0
    shared = {"pv": pv, "attn_g_bc": attn_g_bc, "biasT": biasT, "win": win, "wv": wv, "wout": wout, "loraw": loraw, "g2": g2,
              "wg0": wg0, "wu0": wu0, "wd0": wd0, "wg1": wg1, "wu1": wu1, "wd1": wd1, "router": router,
              "c_ident": ident, "c_onesm": onesm, "c_blk": blk, "c_tri_i": tri_i, "c_tri_e": tri_e,
              "c_maskA": f(maskA), "c_maskB": f(maskB), "c_sel": f(sel.reshape(8, 1024))}
    return shared


_PROG = {}


def kernel(**inputs):
    inp = {k: np.asarray(v) for k, v in inputs.items()}
    shared = host_inputs(inp)
    if "kb" not in _PROG:
        _PROG["kb"] = build_program()
    kb = _PROG["kb"]
    x = inp["x"]
    in_maps = []
    for b in range(8):
        m = dict(shared)
        m["xT"] = np.ascontiguousarray(x[b].T)
        in_maps.append(m)
    res = run_bass_kernel_spmd(kb.nc, in_maps, core_ids=list(range(8)))
    out = np.stack([np.ascontiguousarray(res.results[b]["outT"].T) for b in range(8)], axis=0)
    return out.astype(np.float32)
```

```python
import numpy as np
from contextlib import ExitStack
import concourse.bass as bass
import concourse.mybir as mybir
from concourse.bass_utils import run_bass_kernel_spmd

F32 = mybir.dt.float32
BF16 = mybir.dt.bfloat16
AF = mybir.ActivationFunctionType
ALU = mybir.AluOpType
AX = mybir.AxisListType

T = 2048
NB = 4
NT = 16
SEM_LIMIT = 60000
NPV = 80
DECAY_C = 0.6065306597126334


class Trk:
    __slots__ = ("w", "r")

    def __init__(self):
        self.w = None
        self.r = {}


class KB:
    def __init__(self, n_dma_sems=12):
        self.nc = bass.Bass("TRN2", target_bir_lowering=False)
        self.es = ExitStack()
        nc = self.nc
        self.eh = {"pe": nc.tensor, "act": nc.scalar, "dve": nc.vector, "pool": nc.gpsimd, "sp": nc.sync}
        self.sem = {}
        self.cnt = {}
        self.cur = {}
        self.eng_of = {}
        self.gen = {}
        self.known = {e: {} for e in self.eh}
        for e in self.eh:
            self._newgen(e, e)
        self.dma_pools = {"sp": ["d%d" % j for j in range(6)], "act": ["d%d" % j for j in range(6)], "pool": ["g%d" % j for j in range(8)]}
        self.dma_names = self.dma_pools["sp"] + self.dma_pools["pool"]
        for d in self.dma_names:
            self._newgen(d, None)
        self.dma_rr = {"sp": 0, "act": 0, "pool": 0}
        self.nwaits = 0
        self.nins = 0
        self.total_ins = {e: 0 for e in self.eh}

    def _newgen(self, name, eng):
        g = self.gen.get(name, -1) + 1
        self.gen[name] = g
        key = "%s_%d" % (name, g)
        self.sem[key] = self.es.enter_context(self.nc.semaphore("s_" + key))
        self.cnt[key] = 0
        self.cur[name] = key
        self.eng_of[key] = eng
        return key

    def sbuf(self, name, shape, dt, stack=None):
        self.uid = getattr(self, "uid", 0) + 1
        return (stack or self.es).enter_context(self.nc.sbuf_tensor("%s_%d" % (name, self.uid), list(shape), dt))

    def psum(self, name, shape, dt=F32):
        return self.es.enter_context(self.nc.psum_tensor(name, list(shape), dt))

    def _wait(self, eng, key, val):
        if val <= 0:
            return
        kn = self.known[eng]
        if kn.get(key, 0) >= val:
            return
        self.eh[eng].wait_ge(self.sem[key], val)
        kn[key] = val
        self.nwaits += 1

    def _deps(self, eng, R, W):
        deps = {}
        cur = self.cur[eng]
        for t in R:
            if t.w is not None:
                k, v = t.w
                if k == cur:
                    if eng != "pe":
                        self._wait(eng, k, v)
                elif self.eng_of[k] != eng:
                    if deps.get(k, 0) < v:
                        deps[k] = v
        for t in W:
            if t.w is not None:
                k, v = t.w
                if self.eng_of[k] != eng and deps.get(k, 0) < v:
                    deps[k] = v
            for k, v in t.r.items():
                if self.eng_of[k] != eng and deps.get(k, 0) < v:
                    deps[k] = v
        for k, v in deps.items():
            self._wait(eng, k, v)

    def _mark(self, key, c, R, W):
        for t in W:
            t.w = (key, c)
            t.r = {}
        for t in R:
            if t.r.get(key, 0) < c:
                t.r[key] = c

    def ins(self, eng, fn, R=(), W=()):
        cur = self.cur[eng]
        if self.cnt[cur] >= SEM_LIMIT:
            self.eh[eng].wait_ge(self.sem[cur], self.cnt[cur])
            cur = self._newgen(eng, eng)
        self._deps(eng, R, W)
        inst = fn()
        inst.then_inc(self.sem[cur], 1)
        self.cnt[cur] += 1
        self._mark(cur, self.cnt[cur], R, W)
        self.nins += 1
        self.total_ins[eng] += 1
        return inst

    def dma(self, q, out, in_, R=(), W=(), **kw):
        pool_ = self.dma_pools[q]
        name = pool_[self.dma_rr[q] % len(pool_)]
        self.dma_rr[q] += 1
        key = self.cur[name]
        self._wait(q, key, self.cnt[key])
        if self.cnt[key] >= SEM_LIMIT:
            key = self._newgen(name, None)
        self._deps(q, R, W)
        for t in list(R) + list(W):
            if t.w is not None and self.eng_of[t.w[0]] == q:
                self.eh[q].wait_ge(self.sem[t.w[0]], t.w[1])
        for t in W:
            for k, v in t.r.items():
                if self.eng_of[k] == q:
                    self.eh[q].wait_ge(self.sem[k], v)
        inst = self.eh[q].dma_start(out=out, in_=in_, **kw)
        inst.then_inc(self.sem[key], 16)
        self.cnt[key] += 16
        self._mark(key, self.cnt[key], R, W)
        self.nins += 1
        return inst

    def barrier(self):
        keys = [self.cur[n] for n in self.dma_names] + [self.cur[e] for e in self.eh]
        for e in self.eh:
            for k in keys:
                if self.eng_of[k] != e:
                    self._wait(e, k, self.cnt[k])

    def finish(self, eng="sp"):
        for name in self.dma_names:
            key = self.cur[name]
            self._wait(eng, key, self.cnt[key])
        for e in self.eh:
            if e != eng:
                key = self.cur[e]
                self._wait(eng, key, self.cnt[key])

    def close(self):
        self.es.close()


def build_program(stop_after="final", n_layers=2, dump=None, dbg=None):
    kb = KB()
    nc = kb.nc
    D = {}

    def din(name, shape):
        D[name] = nc.dram_tensor(name, list(shape), F32, kind="ExternalInput")
        return D[name]

    xT_d = din("xT", [1024, T])
    pv_d = din("pv", [2, 128, NPV])
    agbc_d = din("attn_g_bc", [2, 128, 512])
    biasT_d = din("biasT", [2, 8, 128, 640])
    win_d = din("win", [2, 26, 128, 1024])
    wv_d = din("wv", [2, 128, 4096])
    wout_d = din("wout", [2, 8, 128, 1024])
    loraw_d = din("loraw", [2, 128, 512])
    g2_d = din("g2", [2, 128, 512])
    ew_d = []
    for l, ne in enumerate((2, 8)):
        ew_d.append((din("wg%d" % l, [ne, 11, 128, 1024]), din("wu%d" % l, [ne, 11, 128, 1024]),
                     din("wd%d" % l, [ne, 11, 128, 1024])))
    router_d = din("router", [128, 64])
    c_ident_d = din("c_ident", [128, 128])
    c_onesm_d = din("c_onesm", [128, 128])
    c_blk_d = din("c_blk", [128, 128])
    c_tri_i_d = din("c_tri_i", [128, 128])
    c_tri_e_d = din("c_tri_e", [128, 128])
    c_maskA_d = din("c_maskA", [64, 512])
    c_maskB_d = din("c_maskB", [64, 512])
    c_sel_d = din("c_sel", [8, 1024])
    out_d = nc.dram_tensor("outT", [1024, T], F32, kind="ExternalOutput")

    sb = kb.sbuf
    x = sb("x", [128, 8, T], F32)
    xt = [[Trk() for _ in range(NB)] for _ in range(8)]
    h = sb("h", [128, 8, T], BF16)
    ht = [Trk() for _ in range(NB)]
    pv = sb("pvs", [128, 2, NPV], F32); pvt = Trk()
    pvd = sb("pvd", [128, 2, 24], F32); pvdt = Trk()
    ident_f = sb("ident_f", [128, 128], F32)
    ident_b = sb("ident_b", [128, 128], BF16)
    onesm = sb("onesm", [128, 128], BF16)
    blk = sb("blk", [128, 128], BF16)
    tri_i = sb("tri_i", [128, 128], F32)
    tri_e = sb("tri_e", [128, 128], F32)
    maskA = sb("maskA", [64, 512], BF16)
    maskB = sb("maskB", [64, 512], BF16)
    sel = sb("sel", [8, 1024], BF16)
    ct = Trk()
    wbuf = sb("wbuf", [128, 4, 8, 128], BF16)
    wbt = [Trk() for _ in range(4)]
    wb_rr = [0]
    sq = [sb("sq%d" % i, [128, 512], BF16) for i in range(2)]
    sqt = [Trk() for _ in range(2)]
    rstd = sb("rstd", [128, 512], F32); rstdt = Trk()

    PP = [kb.psum("pp%d" % i, [128, 1024]) for i in range(4)]
    PT = [Trk() for _ in range(8)]

    def PS(i):
        return PP[i // 2][:, (i % 2) * 512:(i % 2) * 512 + 512]

    ps_rr = [0]

    def next_ps(lo=0, hi=8):
        i = lo + (ps_rr[0] % (hi - lo))
        ps_rr[0] += 1
        return i

    for tb in range(NB):
        for c in range(8):
            kb.dma("sp", x[:, c, tb * 512:(tb + 1) * 512], xT_d.ap()[c * 128:(c + 1) * 128, tb * 512:(tb + 1) * 512], W=[xt[c][tb]])
    kb.dma("sp", pv[:, 0, :], pv_d.ap()[0], W=[pvt])
    kb.dma("sp", pv[:, 1, :], pv_d.ap()[1], W=[pvt])
    kb.dma("sp", ident_f[:], c_ident_d.ap(), W=[ct])
    kb.dma("sp", tri_i[:], c_tri_i_d.ap(), W=[ct])
    kb.dma("sp", tri_e[:], c_tri_e_d.ap(), W=[ct])
    kb.dma("pool", sel[:], c_sel_d.ap(), W=[ct])
    kb.dma("pool", ident_b[:], c_ident_d.ap(), W=[ct])
    kb.dma("pool", onesm[:], c_onesm_d.ap(), W=[ct])
    kb.dma("pool", blk[:], c_blk_d.ap(), W=[ct])
    kb.dma("pool", maskA[:], c_maskA_d.ap(), W=[ct])
    kb.dma("pool", maskB[:], c_maskB_d.ap(), W=[ct])
    for l in range(2):
        kb.ins("dve", lambda l=l: nc.vector.tensor_scalar(pvd[:, l, 0:14], pv[:, l, 24:38], -1.0, 1.0, op0=ALU.mult, op1=ALU.add),
               R=[pvt], W=[pvdt])
        kb.ins("dve", lambda l=l: nc.vector.tensor_scalar(pvd[:, l, 14:18], pv[:, l, 64:68], -1.0, 1.0, op0=ALU.mult, op1=ALU.add),
               R=[pvt], W=[pvdt])

    def load_w(src_ap, slot=None, shape4=None):
        if slot is None:
            slot = wb_rr[0] % 4
            wb_rr[0] += 1
        kb.dma("pool", wbuf[:, slot, :, :].rearrange("p k j -> p (k j)"), src_ap, W=[wbt[slot]])
        return slot

    evac_rr = [0]

    def evac_copy(out_ap, in_ap, R, W, eng=None):
        if eng is None:
            eng = "act" if (evac_rr[0] % 2 == 0) else "dve"
            evac_rr[0] += 1
        if eng == "act":
            kb.ins("act", lambda: nc.scalar.copy(out_ap, in_ap), R=R, W=W)
        else:
            kb.ins("dve", lambda: nc.vector.tensor_copy(out_ap, in_ap), R=R, W=W)

    def mm(out_ap, lhsT, rhs, start, stop, R, W):
        kb.ins("pe", lambda: nc.tensor.matmul(out_ap, lhsT, rhs, start=start, stop=stop), R=R, W=W)

    def tbs(tb):
        return slice(tb * 512, (tb + 1) * 512)

    def rmsnorm_to_h(l, gcol, after_tb=None):
        for tb in range(NB):
            pi = next_ps(0, 7)
            for c in range(8):
                s = c % 2
                kb.ins("act", lambda c=c, s=s: nc.scalar.activation(sq[s][:], x[:, c, tbs(tb)], AF.Square),
                       R=[xt[c][tb]], W=[sqt[s]])
                mm(PS(pi), onesm[:], sq[s][:], c == 0, c == 7, R=[ct, sqt[s]], W=[PT[pi]])
            kb.ins("act", lambda: nc.scalar.activation(rstd[:], PS(pi), AF.Ln, bias=1e-6, scale=1.0), R=[PT[pi]], W=[rstdt])
            kb.ins("act", lambda: nc.scalar.activation(rstd[:], rstd[:], AF.Exp, scale=-0.5), R=[rstdt], W=[rstdt])
            for c in range(8):
                kb.ins("dve", lambda c=c: nc.vector.scalar_tensor_tensor(
                    out=h[:, c, tbs(tb)], in0=x[:, c, tbs(tb)], scalar=pv[:, l, gcol + c:gcol + c + 1], in1=rstd[:],
                    op0=ALU.mult, op1=ALU.mult), R=[xt[c][tb], pvt, rstdt], W=[ht[tb]])
            if after_tb is not None:
                after_tb(tb)

    def attention_phase(l):
        st = ExitStack()
        qk = sb("qk", [128, 8, T], BF16, st)
        qkt = [[Trk() for _ in range(NT)] for _ in range(8)]
        Vp = sb("Vp", [128, NT, 8, 65], BF16, st); vpt = [Trk() for _ in range(NT)]
        Mh = sb("Mh", [128, 8, 640], BF16, st); mht = Trk()
        bst = sb("bst", [128, 640], F32, st); bstt = Trk()
        E = [sb("E%d" % i, [128, 640], BF16, st) for i in range(2)]; et = [Trk() for _ in range(2)]
        o = sb("o_att", [128, 8, 64], F32, st); ot = Trk()
        yo = sb("yo_att", [128, 512], F32, st); yot = Trk()
        junk = sb("junk_att", [128, 512], BF16, st); junkt = Trk()
        gbc = sb("gbc", [128, 512], F32, st); gbct = Trk()
        rec = sb("rec", [128, 8], F32, st); rect = Trk()
        ssq = sb("ssq", [128, 2], F32, st); ssqt = Trk()

        kb.dma("sp", gbc[:], agbc_d.ap()[l], W=[gbct])
        kb.ins("pool", lambda: nc.gpsimd.memset(Vp[:, :, :, 64:65], 1.0), W=vpt)
        for hh in range(8):
            kb.dma("sp", bst[:], biasT_d.ap()[l, hh], W=[bstt])
            kb.ins("act", lambda hh=hh: nc.scalar.activation(Mh[:, hh, :], bst[:], AF.Exp), R=[bstt], W=[mht])
        for ci in range(8):
            slot = load_w(win_d.ap()[l, 14 + ci])
            for tb in range(NB):
                pi = next_ps()
                for k in range(8):
                    mm(PS(pi), wbuf[:, slot, k, :], h[:, k, tbs(tb)], k == 0, k == 7, R=[wbt[slot], ht[tb]], W=[PT[pi]])
                evac_copy(qk[:, ci, tbs(tb)], PS(pi), R=[PT[pi]], W=qkt[ci][tb * 4:tb * 4 + 4])
        kb.dma("pool", wbuf[:].rearrange("p j k c -> p (j k c)"), wv_d.ap()[l], W=wbt)
        for tt in range(NT):
            pi = next_ps()
            for k in range(8):
                mm(PS(pi), h[:, k, tt * 128:(tt + 1) * 128], wbuf[:, :, k, :], k == 0, k == 7, R=wbt + [ht[tt // 4]], W=[PT[pi]])
            evac_copy(Vp[:, tt, :, 0:64], PS(pi).rearrange("p (h d) -> p h d", h=8), R=[PT[pi]], W=[vpt[tt]])
        items = [(m, hh) for m in range(NT) for hh in range(8)]

        def jlist(m):
            js = [j for j in range(m - 4, m + 1) if j >= 0]
            return js, 5 - len(js)

        def stageA(i):
            m, hh = items[i]
            js, s0 = jlist(m)
            hp, r0 = hh // 2, (hh % 2) * 64
            sp_ = i % 2
            Sps = PP[sp_]
            St = [PT[2 * sp_], PT[2 * sp_ + 1]]
            for idx, j in enumerate(js):
                sl = s0 + idx
                mm(Sps[:, sl * 128:(sl + 1) * 128], qk[r0:r0 + 64, 4 + hp, j * 128:(j + 1) * 128],
                   qk[r0:r0 + 64, hp, m * 128:(m + 1) * 128], True, True,
                   R=[qkt[4 + hp][j], qkt[hp][m]], W=St)
            eb = i % 2
            kb.ins("act", lambda: nc.scalar.activation(E[eb][:, s0 * 128:640], Sps[:, s0 * 128:640], AF.Exp, scale=0.125),
                   R=St, W=[et[eb]])
            kb.ins("dve", lambda: nc.vector.tensor_tensor(out=E[eb][:, s0 * 128:640], in0=E[eb][:, s0 * 128:640],
                                                          in1=Mh[:, hh, s0 * 128:640], op=ALU.mult),
                   R=[et[eb], mht], W=[et[eb]])

        def stageB(i):
            m, hh = items[i]
            js, s0 = jlist(m)
            eb = i % 2
            og = 4 + hh // 4
            for idx, j in enumerate(js):
                sl = s0 + idx
                mm(PS(og)[:, (hh % 4) * 65:(hh % 4) * 65 + 65], E[eb][:, sl * 128:(sl + 1) * 128], Vp[:, j, hh, :],
                   idx == 0, idx == len(js) - 1, R=[et[eb], vpt[j]], W=[PT[og]])

        def epilogue(m):
            for g in range(2):
                Og = PS(4 + g)[:, 0:260].rearrange("p (h d) -> p h d", h=4)
                kb.ins("dve", lambda: nc.vector.reciprocal(rec[:, g * 4:g * 4 + 4], Og[:, :, 64]), R=[PT[4 + g]], W=[rect])
                kb.ins("dve", lambda: nc.vector.tensor_tensor(out=o[:, g * 4:g * 4 + 4, :], in0=Og[:, :, 0:64],
                                                              in1=rec[:, g * 4:g * 4 + 4].unsqueeze(2).to_broadcast([128, 4, 64]),
                                                              op=ALU.mult), R=[PT[4 + g], rect], W=[ot])
            of = o[:].rearrange("p h d -> p (h d)")
            kb.ins("pool", lambda: nc.gpsimd.memset(ssq[:, 0:1], 0.0), W=[ssqt])
            kb.ins("act", lambda: nc.scalar.activation(junk[:], of, AF.Square, accum_out=ssq[:, 0:1]), R=[ot, ssqt], W=[junkt, ssqt])
            kb.ins("act", lambda: nc.scalar.activation(ssq[:, 1:2], ssq[:, 0:1], AF.Sqrt, bias=1e-6, scale=1.0 / 512), R=[ssqt], W=[ssqt])
            kb.ins("dve", lambda: nc.vector.reciprocal(ssq[:, 1:2], ssq[:, 1:2]), R=[ssqt], W=[ssqt])
            kb.ins("dve", lambda: nc.vector.scalar_tensor_tensor(out=yo[:], in0=of, scalar=ssq[:, 1:2], in1=gbc[:],
                                                                 op0=ALU.mult, op1=ALU.mult), R=[ot, ssqt, gbct], W=[yot])
            pi = 6 + (m % 2)
            for c in range(4):
                mm(PS(pi)[:, c * 128:(c + 1) * 128], yo[:, c * 128:(c + 1) * 128], ident_f[:], True, True, R=[yot, ct], W=[PT[pi]])
            evac_copy(qk[:, 0:4, m * 128:(m + 1) * 128], PS(pi).rearrange("p (c t) -> p c t", c=4), R=[PT[pi]],
                      W=[qkt[c][m] for c in range(4)])

        stageA(0)
        for i in range(len(items)):
            if i + 1 < len(items):
                stageA(i + 1)
            stageB(i)
            if items[i][1] == 7:
                epilogue(items[i][0])
        for oc in range(8):
            slot = load_w(wout_d.ap()[l, oc])
            for tb in range(NB):
                pi = next_ps(0, 4)
                for k in range(4):
                    mm(PS(pi), wbuf[:, slot, 4 + k, :], qk[:, k, tbs(tb)], k == 0, k == 3,
                       R=[wbt[slot]] + qkt[k][tb * 4:tb * 4 + 4], W=[PT[pi]])
                kb.ins("dve", lambda: nc.vector.tensor_tensor(out=x[:, oc, tbs(tb)], in0=PS(pi), in1=x[:, oc, tbs(tb)], op=ALU.add),
                       R=[PT[pi], xt[oc][tb]], W=[xt[oc][tb]])
        kb.barrier()
        st.close()

    def rwkv_phase(l):
        BS = 256
        NBLK = T // BS
        NCH = BS // 64
        NQ = BS // 128
        NSTREAM = 2
        st = ExitStack()
        lora1 = sb("lora1", [128, T], BF16, st); l1t = [Trk() for _ in range(NB)]
        lora2 = sb("lora2", [128, T], BF16, st); l2t = [Trk() for _ in range(NB)]
        loraw = sb("loraw_s", [128, 512], BF16, st); lwt = Trk()
        g2s = sb("g2_s", [128, 512], BF16, st); g2t = Trk()
        wx = sb("r_wx", [128, 2, 8, 128], BF16, st); wxt = [Trk(), Trk()]
        GNB = sb("r_GNB", [64, 1], F32, st); GNBt = Trk()
        kb.ins("pool", lambda: nc.gpsimd.memset(GNB[:], 64e-5), W=[GNBt])
        kb.dma("pool", loraw[:], loraw_d.ap()[l], W=[lwt])
        kb.dma("pool", g2s[:], g2_d.ap()[l], W=[g2t])

        ps_free = [0, 1, 2, 3, 4, 5]

        def aps():
            return ps_free.pop(0)

        def fps(i):
            ps_free.append(i)

        def bsl(b):
            return slice(b * BS, (b + 1) * BS)

        shl = sb("shbuf_l", [128, 516], F32, st); shlt = Trk()
        carl = sb("carry_l", [128, 2], F32, st); carlt = Trk()
        tl = rstd; tlt = rstdt
        for ci, cc in enumerate((12, 13)):
            slot = load_w(win_d.ap()[l, cc])
            for tb in range(NB):
                pi = aps()
                for k in range(8):
                    mm(PS(pi), wbuf[:, slot, k, :], h[:, k, tbs(tb)], k == 0, k == 7, R=[wbt[slot], ht[tb]], W=[PT[pi]])
                mu = pv[:, l, 24 + cc:25 + cc]
                omm = pvd[:, l, cc:cc + 1]
                if tb == 0:
                    kb.ins("pool", lambda: nc.gpsimd.memset(shl[:, 0:1], 0.0), W=[shlt])
                else:
                    kb.ins("act", lambda: nc.scalar.copy(shl[:, 0:1], carl[:, ci:ci + 1]), R=[carlt], W=[shlt])
                kb.ins("act", lambda: nc.scalar.activation(shl[:, 1:513], PS(pi), AF.Identity, scale=mu), R=[PT[pi], pvt, shlt], W=[shlt])
                kb.ins("act", lambda: nc.scalar.copy(carl[:, ci:ci + 1], shl[:, 512:513]), R=[shlt], W=[carlt])
                kb.ins("dve", lambda: nc.vector.scalar_tensor_tensor(out=tl[:], in0=PS(pi), scalar=omm, in1=shl[:, 0:512],
                                                                     op0=ALU.mult, op1=ALU.add), R=[PT[pi], pvdt, shlt], W=[tlt])
                fps(pi)
                if cc == 12:
                    kb.ins("act", lambda: nc.scalar.activation(lora1[0:64, tbs(tb)], tl[0:64, :], AF.Tanh), R=[tlt], W=[l1t[tb]])
                    kb.ins("act", lambda: nc.scalar.copy(lora1[64:128, tbs(tb)], tl[64:128, :]), R=[tlt], W=[l1t[tb]])
                else:
                    kb.ins("act", lambda: nc.scalar.activation(lora2[:, tbs(tb)], tl[:], AF.Sigmoid), R=[tlt], W=[l2t[tb]])

        def stream(s, hps):
            def f32t(name):
                return sb(name, [128, BS], F32, st), Trk()

            def b16t(name):
                return sb(name, [128, BS], BF16, st), Trk()
            RKV = sb("r_RKV", [128, 3, BS], F32, st)
            Rt, Rtt = RKV[:, 0, :], Trk(); Kt, Ktt = RKV[:, 1, :], Trk(); Vt, Vtt = RKV[:, 2, :], Trk()
            SA = sb("r_SA", [128, 2, BS], F32, st)
            SG, SGt = SA[:, 0, :], Trk(); Aa, Aat = SA[:, 1, :], Trk(); Gt, Gtt = b16t("r_G")
            Yraw = RKV[0:64, 0:2, :].rearrange("p a (b v) -> p (a b) v", v=64)
            Ycb = SA[0:64, :, :].rearrange("p a (b v) -> p (a b) v", v=64)
            Ynb = sb("r_Ynb", [64, 2 * NCH, 64], BF16, st); Ynbt = Trk()
            kkn, kknt = f32t("r_kkn"); t1, t1t = f32t("r_t1"); t2, t2t = f32t("r_t2")
            eG, eGt = SG, SGt; eGx, eGxt = f32t("r_eGx"); eGn, eGnt = f32t("r_eGn")
            bv, bvt = f32t("r_bv")
            kk2, kk2t = b16t("r_kk2")
            ART = sb("r_ART", [128, NCH, 128], BF16, st); ARTt = Trk()
            bT, bTt = b16t("r_bT"); kT, kTt = b16t("r_kT"); vT, vTt = b16t("r_vT")
            sgT = eGn[:].rearrange("p (q f) -> p q f", q=NQ); sgTt = eGnt
            CA = sb("r_CA", [64, NCH, 512], BF16, st); CAt = Trk()
            CB = sb("r_CB", [64, NCH, 512], BF16, st); CBt = Trk()
            NI = 2 * NCH
            Xb = [sb("r_X%d" % i, [64, NI, 64], BF16, st) for i in range(2)]; Xbt = [Trk(), Trk()]
            Nb = [sb("r_N%d" % i, [64, NI, 64], BF16, st) for i in range(2)]; Nbt = [Trk(), Trk()]
            TTf = sb("r_TT", [64, NI, 64], BF16, st); TTft = Trk()
            ST = sb("r_ST", [64, 2, 64], F32, st); STt = Trk()
            STb = sb("r_STb", [64, 2, 64], BF16, st); STbt = Trk()
            WC = sb("r_WC", [64, 2, NCH], F32, st); WCt = Trk()
            WCs = sb("r_WCs", [128, NCH], F32, st); WCst = Trk()
            P1 = sb("r_P1", [64, 2, 64], BF16, st); P1t = Trk()
            U = sb("r_U", [64, 2, 64], BF16, st); Ut = Trk()
            Yc = sb("r_Yc", [64, 2, 64], F32, st); Yct = Trk()
            Ysq = sb("r_Ysq", [64, 2, 64], F32, st); Ysqt = Trk()
            Yn = sb("r_Yn", [64, 2, 64], BF16, st); Ynt = Trk()
            stat = sb("r_stat", [64, 8 * NCH], F32, st); statt = Trk()
            sh = sb("shbuf", [128, BS + 4], F32, st); sht = Trk()
            carry = sb("carry", [128, 4], F32, st); carryt = Trk()
            yrs = sb("yrs", [128, T], BF16, st); yrst = [Trk() for _ in range(NBLK)]
            if s == 0:
                wsl = [(wbuf[:, i, :, :], wbt[i]) for i in range(3)]
            else:
                wsl = [(wbuf[:, 3, :, :], wbt[3]), (wx[:, 0, :, :], wxt[0]), (wx[:, 1, :, :], wxt[1])]
            pyo = 6 + s

            def shifted_proj(wi, cc, b, out_ap, out_trk, ci):
                wap, wtr = wsl[wi]
                pi = aps()
                for k in range(8):
                    mm(PS(pi)[:, 0:BS], wap[:, k, :], h[:, k, bsl(b)], k == 0, k == 7, R=[wtr, ht[(b * BS) // 512]], W=[PT[pi]])
                mu = pv[:, l, 24 + cc:25 + cc]
                omm = pvd[:, l, cc:cc + 1]
                if b == 0:
                    kb.ins("pool", lambda: nc.gpsimd.memset(sh[:, 0:1], 0.0), W=[sht])
                else:
                    kb.ins("act", lambda: nc.scalar.copy(sh[:, 0:1], carry[:, ci:ci + 1]), R=[carryt], W=[sht])
                kb.ins("act", lambda: nc.scalar.activation(sh[:, 1:BS + 1], PS(pi)[:, 0:BS], AF.Identity, scale=mu), R=[PT[pi], pvt, sht], W=[sht])
                kb.ins("act", lambda: nc.scalar.copy(carry[:, ci:ci + 1], sh[:, BS:BS + 1]), R=[sht], W=[carryt])
                kb.ins("dve", lambda: nc.vector.scalar_tensor_tensor(out=out_ap, in0=PS(pi)[:, 0:BS], scalar=omm, in1=sh[:, 0:BS],
                                                                     op0=ALU.mult, op1=ALU.add), R=[PT[pi], pvdt, sht], W=[out_trk])
                fps(pi)

            for hp in hps:
                for wi, cc in enumerate((hp, 4 + hp, 8 + hp)):
                    kb.dma("pool", wsl[wi][0].rearrange("p k j -> p (k j)"), win_d.ap()[l, cc], W=[wsl[wi][1]])
                kb.ins("pool", lambda: nc.gpsimd.memset(ST[:], 0.0), W=[STt])
                kb.ins("pool", lambda: nc.gpsimd.memset(STb[:], 0.0), W=[STbt])
                w0 = pv[:, l, 52 + hp:53 + hp]; a0 = pv[:, l, 56 + hp:57 + hp]; k_k = pv[:, l, 60 + hp:61 + hp]
                k_a = pv[:, l, 64 + hp:65 + hp]; r_k = pv[:, l, 68 + hp:69 + hp]
                ln_w = pv[:, l, 72 + hp:73 + hp]; ln_b = pv[:, l, 76 + hp:77 + hp]
                omka = pvd[:, l, 14 + hp:15 + hp]
                cs = slice(hp * 128, (hp + 1) * 128)
                yield
                for b in range(NBLK):
                    tb = (b * BS) // 512
                    shifted_proj(0, hp, b, Rt[:], Rtt, 0)
                    yield
                    shifted_proj(1, 4 + hp, b, Kt[:], Ktt, 1)
                    yield
                    shifted_proj(2, 8 + hp, b, Vt[:], Vtt, 2)
                    yield
                    pi = aps()
                    mm(PS(pi)[:, 0:BS], loraw[0:64, cs], lora1[0:64, bsl(b)], True, True, R=[lwt, l1t[tb]], W=[PT[pi]])
                    kb.ins("act", lambda: nc.scalar.activation(SG[:], PS(pi)[:, 0:BS], AF.Sigmoid, bias=w0, scale=1.0), R=[PT[pi], pvt], W=[SGt])
                    fps(pi)
                    pi = aps()
                    mm(PS(pi)[:, 0:BS], loraw[64:128, cs], lora1[64:128, bsl(b)], True, True, R=[lwt, l1t[tb]], W=[PT[pi]])
                    kb.ins("act", lambda: nc.scalar.activation(Aa[:], PS(pi)[:, 0:BS], AF.Sigmoid, bias=a0, scale=1.0), R=[PT[pi], pvt], W=[Aat])
                    fps(pi)
                    pi = aps()
                    mm(PS(pi)[:, 0:BS], g2s[:, cs], lora2[:, bsl(b)], True, True, R=[g2t, l2t[tb]], W=[PT[pi]])
                    evac_copy(Gt[:], PS(pi)[:, 0:BS], R=[PT[pi]], W=[Gtt], eng="act")
                    fps(pi)
                    yield
                    kb.ins("act", lambda: nc.scalar.activation(kk2[:], Kt[:], AF.Square, scale=k_k), R=[Ktt, pvt], W=[kk2t])
                    pi = aps()
                    mm(PS(pi)[:, 0:BS], blk[:], kk2[:], True, True, R=[ct, kk2t], W=[PT[pi]])
                    kb.ins("act", lambda: nc.scalar.activation(t1[:], PS(pi)[:, 0:BS], AF.Ln, bias=1e-24, scale=1.0), R=[PT[pi]], W=[t1t])
                    fps(pi)
                    kb.ins("act", lambda: nc.scalar.activation(t1[:], t1[:], AF.Exp, scale=-0.5), R=[t1t], W=[t1t])
                    kb.ins("dve", lambda: nc.vector.scalar_tensor_tensor(out=kkn[:], in0=Kt[:], scalar=k_k, in1=t1[:], op0=ALU.mult, op1=ALU.mult),
                           R=[Ktt, pvt, t1t], W=[kknt])
                    kb.ins("dve", lambda: nc.vector.tensor_scalar(t2[:], Aa[:], k_a, omka, op0=ALU.mult, op1=ALU.add), R=[Aat, pvt, pvdt], W=[t2t])
                    kb.ins("pool", lambda: nc.gpsimd.tensor_tensor(out=t2[:], in0=t2[:], in1=Kt[:], op=ALU.mult), R=[t2t, Ktt], W=[t2t])
                    kb.ins("dve", lambda: nc.vector.scalar_tensor_tensor(out=kk2[:], in0=Rt[:], scalar=r_k, in1=t2[:], op0=ALU.mult, op1=ALU.mult),
                           R=[Rtt, pvt, t2t], W=[kk2t])
                    pi = aps()
                    mm(PS(pi)[:, 0:BS], blk[:], kk2[:], True, True, R=[ct, kk2t], W=[PT[pi]])
                    kb.ins("dve", lambda: nc.vector.tensor_tensor(out=bv[:], in0=PS(pi)[:, 0:BS], in1=Vt[:], op=ALU.mult), R=[PT[pi], Vtt], W=[bvt])
                    fps(pi)
                    yield
                    pi = aps()
                    for q4 in range(NQ):
                        mm(PS(pi)[:, q4 * 128:(q4 + 1) * 128], SG[:, q4 * 128:(q4 + 1) * 128], ident_f[:], True, True, R=[SGt, ct], W=[PT[pi]])
                    evac_copy(sgT, PS(pi)[:, 0:BS].rearrange("p (q f) -> p q f", q=NQ), R=[PT[pi]], W=[sgTt], eng="act")
                    fps(pi)
                    pg = aps(); pgx = aps()
                    for q4 in range(NQ):
                        mm(PS(pg)[:, q4 * 128:(q4 + 1) * 128], sgT[:, q4, :], tri_i[:], True, True, R=[sgTt, ct], W=[PT[pg]])
                    for q4 in range(NQ):
                        mm(PS(pgx)[:, q4 * 128:(q4 + 1) * 128], sgT[:, q4, :], tri_e[:], True, True, R=[sgTt, ct], W=[PT[pgx]])
                    kb.ins("act", lambda: nc.scalar.activation(eG[:], PS(pg)[:, 0:BS], AF.Exp, scale=-DECAY_C), R=[PT[pg]], W=[eGt])
                    kb.ins("act", lambda: nc.scalar.activation(eGn[:], PS(pg)[:, 0:BS], AF.Exp, scale=DECAY_C), R=[PT[pg]], W=[eGnt])
                    kb.ins("act", lambda: nc.scalar.activation(eGx[:], PS(pgx)[:, 0:BS], AF.Exp, scale=-DECAY_C), R=[PT[pgx]], W=[eGxt])
                    fps(pg); fps(pgx)
                    yield
                    ARTv = ART[:]
                    kb.ins("dve", lambda: nc.vector.scalar_tensor_tensor(out=ARTv[:, :, 0:64], in0=kkn[:].rearrange("p (c t) -> p c t", c=NCH), scalar=-1.0,
                                                                         in1=eGx[:].rearrange("p (c t) -> p c t", c=NCH), op0=ALU.mult, op1=ALU.mult),
                           R=[kknt, eGxt], W=[ARTt])
                    kb.ins("pool", lambda: nc.gpsimd.tensor_tensor(out=ARTv[:, :, 64:128], in0=Rt[:].rearrange("p (c t) -> p c t", c=NCH),
                                                                   in1=eG[:].rearrange("p (c t) -> p c t", c=NCH), op=ALU.mult), R=[Rtt, eGt], W=[ARTt])
                    kb.ins("dve", lambda: nc.vector.tensor_tensor(out=Aa[:], in0=kkn[:], in1=Aa[:], op=ALU.mult), R=[kknt, Aat], W=[Aat])
                    kb.ins("dve", lambda: nc.vector.tensor_tensor(out=bT[:], in0=Aa[:], in1=eGn[:], op=ALU.mult), R=[Aat, eGnt], W=[bTt])
                    kb.ins("pool", lambda: nc.gpsimd.tensor_tensor(out=kT[:], in0=t2[:], in1=eGn[:], op=ALU.mult), R=[t2t, eGnt], W=[kTt])
                    kb.ins("act", lambda: nc.scalar.copy(vT[:], Vt[:]), R=[Vtt], W=[vTt])
                    yield
                    kkb = kkn[:].bitcast(BF16)
                    t2b = t2[:].bitcast(BF16)
                    exb = eGx[:].bitcast(BF16)
                    ART1 = kkb[0:64, :].rearrange("p (c t) -> p c t", c=NCH)
                    bT1 = t2b[0:64, 0:BS]; kT1 = t2b[0:64, BS:2 * BS]; vT1 = exb[0:64, 0:BS]
                    ARTf = ART[:].rearrange("p c t -> p (c t)")
                    for (src, srct, dst, dstt, n) in ((ARTf, ARTt, kkb[0:64, :], kknt, 2 * BS), (bT[:], bTt, bT1, t2t, BS),
                                                    (kT[:], kTt, kT1, t2t, BS), (vT[:], vTt, vT1, eGxt, BS)):
                        pi = aps()
                        mm(PS(pi)[0:64, 0:n], ident_b[64:128, 64:128], src[64:128, :], True, True, R=[ct, srct], W=[PT[pi]])
                        evac_copy(dst, PS(pi)[0:64, 0:n], R=[PT[pi]], W=[dstt])
                        fps(pi)
                    pi = aps()
                    eGl = eG[:].rearrange("p (c t) -> p c t", c=NCH)[:, :, 63]
                    kb.ins("dve", lambda: nc.vector.tensor_copy(WCs[:], eGl), R=[eGt], W=[WCst])
                    mm(PS(pi)[0:64, 0:NCH], ident_f[64:128, 64:128], WCs[64:128, :], True, True, R=[ct, WCst], W=[PT[pi]])
                    kb.ins("dve", lambda: nc.vector.tensor_copy(WC[:, 0, :], WCs[0:64, :]), R=[WCst], W=[WCt])
                    kb.ins("dve", lambda: nc.vector.tensor_copy(WC[:, 1, :], PS(pi)[0:64, 0:NCH]), R=[PT[pi]], W=[WCt])
                    fps(pi)
                    yield

                    def opnd(hh):
                        if hh == 0:
                            return ART[0:64], bT[0:64, :], kT[0:64, :], vT[0:64, :], [ARTt, bTt, kTt, vTt]
                        return ART1, bT1, kT1, vT1, [kknt, t2t, t2t, eGxt]
                    for c in range(NCH):
                        pa = aps(); pb = aps()
                        cs64 = slice(c * 64, c * 64 + 64)
                        for hh in range(2):
                            ARh, bh, kh, vh, trs = opnd(hh)
                            A_ = PS(pa)[0:64, hh * 256:(hh + 1) * 256]
                            B_ = PS(pb)[0:64, hh * 256:(hh + 1) * 256]
                            mm(A_[:, 0:128], bh[:, cs64], ARh[:, c, :], True, True, R=trs, W=[PT[pa]])
                            mm(A_[:, 128:256], kh[:, cs64], ARh[:, c, :], True, True, R=trs, W=[PT[pa]])
                            mm(B_[:, 0:64], ARh[:, c, 0:64], bh[:, cs64], True, True, R=trs, W=[PT[pb]])
                            mm(B_[:, 64:128], bh[:, cs64], ident_b[0:64, 0:64], True, True, R=trs + [ct], W=[PT[pb]])
                            mm(B_[:, 128:192], kh[:, cs64], ident_b[0:64, 0:64], True, True, R=trs + [ct], W=[PT[pb]])
                            mm(B_[:, 192:256], vh[:, cs64], ident_b[0:64, 0:64], True, True, R=trs + [ct], W=[PT[pb]])
                        kb.ins("dve", lambda: nc.vector.tensor_tensor(out=CA[:, c, :], in0=PS(pa)[0:64, :], in1=maskA[:], op=ALU.mult),
                               R=[PT[pa], ct], W=[CAt])
                        kb.ins("dve", lambda: nc.vector.tensor_tensor(out=CB[:, c, :], in0=PS(pb)[0:64, :], in1=maskB[:], op=ALU.mult),
                               R=[PT[pb], ct], W=[CBt])
                        fps(pa); fps(pb)
                        yield
                    CAv = CA[:].rearrange("p c (h f) -> p (c h) f", h=2)
                    CBv = CB[:].rearrange("p c (h f) -> p (c h) f", h=2)
                    kb.ins("pool", lambda: nc.gpsimd.tensor_copy(Xb[0][:], CAv[:, :, 0:64]), R=[CAt], W=[Xbt[0]])
                    kb.ins("pool", lambda: nc.gpsimd.tensor_copy(Nb[0][:], CBv[:, :, 0:64]), R=[CBt], W=[Nbt[0]])
                    kb.ins("dve", lambda: nc.vector.tensor_tensor(out=TTf[:], in0=CAv[:, :, 0:64],
                                                                  in1=ident_b[0:64, 0:64].unsqueeze(1).to_broadcast([64, NI, 64]), op=ALU.add),
                           R=[CAt, ct], W=[TTft])
                    cur = 0
                    for lev in range(5):
                        nx = 1 - cur
                        px = aps(); pn = aps()
                        if lev < 4:
                            for i in range(NI):
                                mm(PS(px)[0:64, i * 64:(i + 1) * 64], Nb[cur][:, i, :], Xb[cur][:, i, :], True, True,
                                   R=[Nbt[cur], Xbt[cur]], W=[PT[px]])
                        for i in range(NI):
                            mm(PS(pn)[0:64, i * 64:(i + 1) * 64], Xb[cur][:, i, :], Nb[cur][:, i, :], True, True,
                               R=[Nbt[cur], Xbt[cur]], W=[PT[pn]])
                        if lev < 4:
                            evac_copy(Xb[nx][:], PS(px)[0:64, 0:NI * 64].rearrange("p (i f) -> p i f", i=NI), R=[PT[px]], W=[Xbt[nx]], eng="act")
                        evac_copy(Nb[nx][:], PS(pn)[0:64, 0:NI * 64].rearrange("p (i f) -> p i f", i=NI), R=[PT[pn]], W=[Nbt[nx]], eng="dve")
                        fps(px); fps(pn)
                        yield
                        pt = aps()
                        for i in range(NI):
                            mm(PS(pt)[0:64, i * 64:(i + 1) * 64], Nb[nx][:, i, :], TTf[:, i, :], True, True,
                               R=[Nbt[nx], TTft], W=[PT[pt]])
                        kb.ins("dve", lambda: nc.vector.tensor_tensor(out=TTf[:], in0=PS(pt)[0:64, 0:NI * 64].rearrange("p (i f) -> p i f", i=NI),
                                                                      in1=TTf[:], op=ALU.add), R=[PT[pt], TTft], W=[TTft])
                        fps(pt)
                        cur = nx
                        yield
                    W1 = Xb[0]; W1t = Xbt[0]; atok = Xb[1]; atokt = Xbt[1]; Ub = Nb[0]; Ubt = Nbt[0]; Atok = Nb[1]; Atokt = Nbt[1]
                    PhiT = W1; PhiTt = W1t; RpT = atok; RpTt = atokt
                    psA = aps(); psB = aps()
                    for c in range(NCH):
                        for hh in range(2):
                            ARh, bh, kh, vh, trs = opnd(hh)
                            i = 2 * c + hh
                            mm(PS(psA)[0:64, i * 64:(i + 1) * 64], CA[:, c, hh * 256 + 128:hh * 256 + 192], CB[:, c, hh * 256 + 192:hh * 256 + 256], True, True,
                               R=[CAt, CBt], W=[PT[psA]])
                            mm(PS(psB)[0:64, i * 64:(i + 1) * 64], ARh[:, c, 0:64], ident_b[0:64, 0:64], True, True, R=trs + [ct], W=[PT[psB]])
                    evac_copy(W1[:], PS(psA)[0:64, 0:NI * 64].rearrange("p (i f) -> p i f", i=NI), R=[PT[psA]], W=[W1t], eng="act")
                    evac_copy(atok[:], PS(psB)[0:64, 0:NI * 64].rearrange("p (i f) -> p i f", i=NI), R=[PT[psB]], W=[atokt], eng="dve")
                    fps(psA); fps(psB)
                    yield
                    psA = aps(); psB = aps()
                    for i in range(NI):
                        mm(PS(psA)[0:64, i * 64:(i + 1) * 64], TTf[:, i, :], W1[:, i, :], True, True, R=[TTft, W1t], W=[PT[psA]])
                        mm(PS(psB)[0:64, i * 64:(i + 1) * 64], TTf[:, i, :], atok[:, i, :], True, True, R=[TTft, atokt], W=[PT[psB]])
                    evac_copy(Ub[:], PS(psA)[0:64, 0:NI * 64].rearrange("p (i f) -> p i f", i=NI), R=[PT[psA]], W=[Ubt], eng="act")
                    evac_copy(Atok[:], PS(psB)[0:64, 0:NI * 64].rearrange("p (i f) -> p i f", i=NI), R=[PT[psB]], W=[Atokt], eng="dve")
                    fps(psA); fps(psB)
                    yield
                    psA = aps(); psB = aps()
                    for c in range(NCH):
                        for hh in range(2):
                            i = 2 * c + hh
                            mm(PS(psA)[0:64, i * 64:(i + 1) * 64], Atok[:, i, :], CB[:, c, hh * 256 + 64:hh * 256 + 128], True, True,
                               R=[Atokt, CBt], W=[PT[psA]])
                            mm(PS(psB)[0:64, i * 64:(i + 1) * 64], Atok[:, i, :], CA[:, c, hh * 256 + 64:hh * 256 + 128], True, True,
                               R=[Atokt, CAt], W=[PT[psB]])
                    evac_copy(PhiT[:], PS(psA)[0:64, 0:NI * 64].rearrange("p (i f) -> p i f", i=NI), R=[PT[psA]], W=[PhiTt], eng="act")
                    for hh in range(2):
                        ARh, bh, kh, vh, trs = opnd(hh)
                        kb.ins("dve", lambda: nc.vector.tensor_tensor(
                            out=RpT[:].rearrange("p (c h) f -> p c h f", h=2)[:, :, hh, :],
                            in0=PS(psB)[0:64, 0:NI * 64].rearrange("p (c h f) -> p c h f", h=2, f=64)[:, :, hh, :],
                            in1=ARh[:, :, 64:128], op=ALU.add), R=[PT[psB]] + trs, W=[RpTt])
                    fps(psA); fps(psB)
                    yield
                    for c in range(NCH):
                        pp = aps()
                        Yps = PS(pp)[0:64, 0:128]
                        pst = PS(pp)[0:64, 128:256]
                        for hh in range(2):
                            i = 2 * c + hh
                            fo = slice(hh * 64, hh * 64 + 64)
                            mm(pst[:, fo], CB[:, c, hh * 256 + 64:hh * 256 + 128], Ub[:, i, :], True, False, R=[CBt, Ubt], W=[PT[pp]])
                            mm(pst[:, fo], CB[:, c, hh * 256 + 128:hh * 256 + 192], CB[:, c, hh * 256 + 192:hh * 256 + 256], False, False,
                               R=[CBt], W=[PT[pp]])
                            mm(pst[:, fo], PhiT[:, i, :], STb[:, hh, :], False, True, R=[PhiTt, STbt], W=[PT[pp]])
                        for hh in range(2):
                            i = 2 * c + hh
                            fo = slice(hh * 64, hh * 64 + 64)
                            mm(Yps[:, fo], CA[:, c, hh * 256 + 64:hh * 256 + 128], Ub[:, i, :], True, False, R=[CAt, Ubt], W=[PT[pp]])
                            mm(Yps[:, fo], CA[:, c, hh * 256 + 192:hh * 256 + 256], CB[:, c, hh * 256 + 192:hh * 256 + 256], False, False,
                               R=[CAt, CBt], W=[PT[pp]])
                            mm(Yps[:, fo], RpT[:, i, :], STb[:, hh, :], False, True, R=[RpTt, STbt], W=[PT[pp]])
                        STf = ST[:].rearrange("p h v -> p (h v)")
                        kb.ins("dve", lambda: nc.vector.tensor_tensor(out=STf, in0=pst, in1=STf, op=ALU.add), R=[PT[pp], STt], W=[STt])
                        kb.ins("dve", lambda: nc.vector.tensor_tensor(out=ST[:], in0=ST[:], in1=WC[:, :, c:c + 1].to_broadcast([64, 2, 64]), op=ALU.mult),
                               R=[STt, WCt], W=[STt])
                        kb.ins("act", lambda: nc.scalar.copy(STb[:], ST[:]), R=[STt], W=[STbt])
                        kb.ins("act", lambda: nc.scalar.copy(Yraw[:, 2 * c:2 * c + 2, :], Yps.rearrange("p (h v) -> p h v", h=2)),
                               R=[PT[pp]], W=[Rtt, Ktt])
                        fps(pp)
                        yield
                    NI2 = 2 * NCH
                    kb.ins("dve", lambda: nc.vector.tensor_reduce(out=stat[:, 0:NI2], in_=Yraw, axis=AX.X, op=ALU.add), R=[Rtt, Ktt], W=[statt])
                    kb.ins("dve", lambda: nc.vector.tensor_scalar(stat[:, NI2:2 * NI2], stat[:, 0:NI2], -1.0 / 64, None, op0=ALU.mult), R=[statt], W=[statt])
                    kb.ins("pool", lambda: nc.gpsimd.tensor_tensor(out=Ycb, in0=Yraw, in1=stat[:, NI2:2 * NI2].unsqueeze(2).to_broadcast([64, NI2, 64]), op=ALU.add),
                           R=[Rtt, Ktt, statt], W=[SGt, Aat])
                    yield
                    kb.ins("pool", lambda: nc.gpsimd.tensor_tensor(out=Yraw, in0=Ycb, in1=Ycb, op=ALU.mult), R=[SGt, Aat], W=[Rtt, Ktt])
                    kb.ins("dve", lambda: nc.vector.tensor_reduce(out=stat[:, 2 * NI2:3 * NI2], in_=Yraw, axis=AX.X, op=ALU.add), R=[Rtt, Ktt], W=[statt])
                    kb.ins("act", lambda: nc.scalar.activation(stat[:, 3 * NI2:4 * NI2], stat[:, 2 * NI2:3 * NI2], AF.Sqrt, bias=GNB[:], scale=1.0 / 64),
                           R=[statt, GNBt], W=[statt])
                    yield
                    kb.ins("dve", lambda: nc.vector.reciprocal(stat[:, 3 * NI2:4 * NI2], stat[:, 3 * NI2:4 * NI2]), R=[statt], W=[statt])
                    kb.ins("pool", lambda: nc.gpsimd.tensor_tensor(out=Ynb[:], in0=Ycb, in1=stat[:, 3 * NI2:4 * NI2].unsqueeze(2).to_broadcast([64, NI2, 64]), op=ALU.mult),
                           R=[SGt, Aat, statt], W=[Ynbt])
                    for c in range(NCH):
                        mm(PS(pyo)[:, c * 64:(c + 1) * 64], Ynb[:, 2 * c:2 * c + 2, :].rearrange("p h v -> p (h v)"), ident_b[0:64, 0:64], True, True,
                           R=[Ynbt, ct], W=[PT[pyo]])
                    yield
                    kb.ins("act", lambda: nc.scalar.activation(t1[:], PS(pyo)[:, 0:BS], AF.Identity, bias=ln_b, scale=ln_w), R=[PT[pyo], pvt], W=[t1t])
                    kb.ins("dve", lambda: nc.vector.tensor_tensor(out=t1[:], in0=t1[:], in1=bv[:], op=ALU.add), R=[t1t, bvt], W=[t1t])
                    kb.ins("dve", lambda: nc.vector.tensor_tensor(out=yrs[:, bsl(b)], in0=t1[:], in1=Gt[:], op=ALU.mult), R=[t1t, Gtt], W=[yrst[b]])
                    yield
                wap, wtr = wsl[0]
                kb.dma("pool", wap, wout_d.ap()[l].rearrange("o p (k j) -> p o k j", k=8)[:, :, hp, :], W=[wtr])
                for oc in range(8):
                    for tb in range(NB):
                        pi = aps()
                        mm(PS(pi), wap[:, oc, :], yrs[:, tbs(tb)], True, True, R=[wtr] + yrst[tb * 2:tb * 2 + 2], W=[PT[pi]])
                        kb.ins("dve", lambda: nc.vector.tensor_tensor(out=x[:, oc, tbs(tb)], in0=PS(pi), in1=x[:, oc, tbs(tb)], op=ALU.add),
                               R=[PT[pi], xt[oc][tb]], W=[xt[oc][tb]])
                        fps(pi)
                    yield

        gens = [stream(0, [0, 1]), stream(1, [2, 3])]
        alive = list(gens)
        first = True
        for _ in range(0):
            next(gens[0])
        while alive:
            for g in list(alive):
                try:
                    next(g)
                except StopIteration:
                    alive.remove(g)
            if first:
                first = False
                kb.min_free = min(getattr(kb, "min_free", 1 << 30), nc.sbuf_bytes_remaining)
        kb.barrier()
        st.close()

    def ffn_phase(l, moe):
        st = ExitStack()
        ne = 8 if moe else 2
        wg_d, wu_d, wd_d = ew_d[1 if moe else 0]
        hid = sb("hid", [128, 11, T], BF16, st); hidt = [[Trk() for _ in range(NB)] for _ in range(11)]
        slu = [sb("slu%d" % i, [128, 512], F32, st) for i in range(2)]; slut = [Trk(), Trk()]
        wdn = sb("wdn", [128, 11, 8, 128], BF16, st); wdnt = [Trk() for _ in range(11)]
        if moe:
            cbc = sb("cbc", [128, T], F32, st); cbct = [Trk() for _ in range(NB)]
            wdn_dummy = None
            combT = sb("combT", [8, T], BF16, st); combTt = [Trk() for _ in range(NT)]
            rtr = sb("rtr", [128, 8, 8], F32, st); rtrt = Trk()
            lg = sb("lg", [128, 8], F32, st); lgt = Trk()
            top = sb("top8", [128, 8], F32, st); topt = Trk()
            gts = sb("gts", [128, 4], F32, st); gtst = Trk()
            eq1 = sb("eq1", [128, 8], F32, st); eq1t = Trk()
            eq2 = sb("eq2", [128, 8], F32, st); eq2t = Trk()
            comb = sb("comb", [128, 8], F32, st); combt = Trk()
            xsq = sb("xsq", [128, 128], BF16, st); xsqt = Trk()
            rs = sb("rs_tok", [128, 2], F32, st); rst = Trk()
            kb.dma("sp", rtr[:].rearrange("p k e -> p (k e)"), router_d.ap(), W=[rtrt])
            for k in range(8):
                kb.ins("dve", lambda k=k: nc.vector.tensor_scalar(rtr[:, k, :], rtr[:, k, :], pv[:, l, 8 + k:9 + k], None, op0=ALU.mult),
                       R=[rtrt, pvt], W=[rtrt])
            lg3 = sb("lg3", [128, NT, 8], F32, st); lg3t = Trk()
            lgb = sb("lgb", [128, NT, 8], F32, st); lgbt = Trk()
            e1 = sb("e1", [128, NT, 8], F32, st); e1t = Trk()
            e2 = sb("e2", [128, NT, 8], F32, st); e2t = Trk()
            tp = sb("tp", [128, 6, NT], F32, st); tpt = Trk()
            PSl = PS(7).rearrange("p (t e) -> p t e", e=16)[:, 0:NT, :]

            def router_tb(tb):
                for q in range(4):
                    tt = tb * 4 + q
                    tsl = slice(tt * 128, (tt + 1) * 128)
                    for k in range(8):
                        mm(PSl[:, tt, 0:8], x[:, k, tsl], rtr[:, k, :], k == 0, k == 7, R=[xt[k][tb], rtrt], W=[PT[7]])
                    mm(PSl[:, tt, 8:9], rstd[0:1, q * 128:(q + 1) * 128], ident_f[0:1, 0:1], True, True, R=[rstdt, ct], W=[PT[7]])
            rmsnorm_to_h(l, 8, after_tb=router_tb)
            kb.ins("act", lambda: nc.scalar.copy(tp[:, 0, :], PSl[:, :, 8]), R=[PT[7]], W=[tpt])
            kb.ins("dve", lambda: nc.vector.tensor_tensor(out=lg3[:], in0=PSl[:, :, 0:8], in1=tp[:, 0, :].unsqueeze(2).to_broadcast([128, NT, 8]), op=ALU.mult),
                   R=[PT[7], tpt], W=[lg3t])
            kb.ins("dve", lambda: nc.vector.tensor_reduce(out=tp[:, 1, :], in_=lg3[:], axis=AX.X, op=ALU.max), R=[lg3t], W=[tpt])
            kb.ins("dve", lambda: nc.vector.tensor_tensor(out=e1[:], in0=lg3[:], in1=tp[:, 1, :].unsqueeze(2).to_broadcast([128, NT, 8]), op=ALU.is_equal),
                   R=[lg3t, tpt], W=[e1t])
            kb.ins("dve", lambda: nc.vector.scalar_tensor_tensor(out=lgb[:], in0=e1[:], scalar=-1e30, in1=lg3[:], op0=ALU.mult, op1=ALU.add),
                   R=[e1t, lg3t], W=[lgbt])
            kb.ins("dve", lambda: nc.vector.tensor_reduce(out=tp[:, 2, :], in_=lgb[:], axis=AX.X, op=ALU.max), R=[lgbt], W=[tpt])
            kb.ins("dve", lambda: nc.vector.tensor_tensor(out=e2[:], in0=lgb[:], in1=tp[:, 2, :].unsqueeze(2).to_broadcast([128, NT, 8]), op=ALU.is_equal),
                   R=[lgbt, tpt], W=[e2t])
            kb.ins("dve", lambda: nc.vector.tensor_tensor(out=tp[:, 3, :], in0=tp[:, 2, :], in1=tp[:, 1, :], op=ALU.subtract), R=[tpt], W=[tpt])
            kb.ins("act", lambda: nc.scalar.activation(tp[:, 3, :], tp[:, 3, :], AF.Exp), R=[tpt], W=[tpt])
            kb.ins("dve", lambda: nc.vector.tensor_scalar(tp[:, 3, :], tp[:, 3, :], 1.0, None, op0=ALU.add), R=[tpt], W=[tpt])
            kb.ins("dve", lambda: nc.vector.reciprocal(tp[:, 4, :], tp[:, 3, :]), R=[tpt], W=[tpt])
            kb.ins("dve", lambda: nc.vector.tensor_scalar(tp[:, 5, :], tp[:, 4, :], -1.0, 1.0, op0=ALU.mult, op1=ALU.add), R=[tpt], W=[tpt])
            kb.ins("dve", lambda: nc.vector.tensor_tensor(out=e1[:], in0=e1[:], in1=tp[:, 4, :].unsqueeze(2).to_broadcast([128, NT, 8]), op=ALU.mult),
                   R=[e1t, tpt], W=[e1t])
            kb.ins("dve", lambda: nc.vector.tensor_tensor(out=e2[:], in0=e2[:], in1=tp[:, 5, :].unsqueeze(2).to_broadcast([128, NT, 8]), op=ALU.mult),
                   R=[e2t, tpt], W=[e2t])
            kb.ins("dve", lambda: nc.vector.tensor_tensor(out=e1[:], in0=e1[:], in1=e2[:], op=ALU.add), R=[e1t, e2t], W=[e1t])
            for tt in range(NT):
                bnk = tt // 4
                mm(PS(bnk)[0:8, (tt % 4) * 128:(tt % 4 + 1) * 128], e1[:, tt, :], ident_f[:], True, True, R=[e1t, ct], W=[PT[bnk]])
            for bnk in range(4):
                evac_copy(combT[:, bnk * 512:(bnk + 1) * 512], PS(bnk)[0:8, :], R=[PT[bnk]], W=combTt[bnk * 4:bnk * 4 + 4])
        else:
            rmsnorm_to_h(l, 8)
        steps = [(e, hc) for e in range(ne) for hc in range(11)]

        def issue_loads(i):
            e_, hc_ = steps[i]
            base = 2 * (i % 2)
            load_w(wg_d.ap()[e_, hc_], slot=base)
            load_w(wu_d.ap()[e_, hc_], slot=base + 1)
        issue_loads(0)
        for i, (e, hc) in enumerate(steps):
            if hc == 0 and moe:
                for tb in range(NB):
                    pi = next_ps()
                    mm(PS(pi), sel[:, e * 128:(e + 1) * 128], combT[:, tbs(tb)], True, True, R=[ct] + combTt[tb * 4:tb * 4 + 4], W=[PT[pi]])
                    evac_copy(cbc[:, tbs(tb)], PS(pi), R=[PT[pi]], W=[cbct[tb]], eng="act")
            if i + 1 < len(steps):
                issue_loads(i + 1)
            if hc >= 1:
                for hq in ((0, 1) if hc == 1 else (hc,)):
                    kb.dma("pool", wdn[:, hq, :, :].rearrange("p o j -> p (o j)"), wd_d.ap()[e, hq], W=[wdnt[hq]])
            sg_ = 2 * (i % 2); su_ = sg_ + 1
            for tb in range(NB):
                pg = next_ps(); pu = next_ps()
                while pu == pg:
                    pu = next_ps()
                for k in range(8):
                    mm(PS(pg), wbuf[:, sg_, k, :], h[:, k, tbs(tb)], k == 0, k == 7, R=[wbt[sg_], ht[tb]], W=[PT[pg]])
                for k in range(8):
                    mm(PS(pu), wbuf[:, su_, k, :], h[:, k, tbs(tb)], k == 0, k == 7, R=[wbt[su_], ht[tb]], W=[PT[pu]])
                s = (hc * NB + tb) % 2
                kb.ins("act", lambda: nc.scalar.activation(slu[s][:], PS(pg), AF.Silu), R=[PT[pg]], W=[slut[s]])
                if moe:
                    kb.ins("dve", lambda: nc.vector.tensor_tensor(out=slu[s][:], in0=slu[s][:], in1=cbc[:, tbs(tb)], op=ALU.mult),
                           R=[slut[s], cbct[tb]], W=[slut[s]])
                kb.ins("dve", lambda: nc.vector.tensor_tensor(out=hid[:, hc, tbs(tb)], in0=PS(pu), in1=slu[s][:], op=ALU.mult),
                       R=[PT[pu], slut[s]], W=[hidt[hc][tb]])
            if hc == 10:
                down_proj(e, wd_d, hid, hidt, wdn, wdnt)
        kb.barrier()
        st.close()

    def down_proj(e, wd_d, hid, hidt, wdn, wdnt):
        for ocg in range(2):
            for tbg in range(2):
                for hc in range(11):
                    for oi in range(4):
                        oc = ocg * 4 + oi
                        for ti in range(2):
                            tb = tbg * 2 + ti
                            pi = oi * 2 + ti
                            mm(PS(pi), wdn[:, hc, oc, :], hid[:, hc, tbs(tb)], hc == 0, hc == 10,
                               R=[wdnt[hc], hidt[hc][tb]], W=[PT[pi]])
                for oi in range(4):
                    oc = ocg * 4 + oi
                    for ti in range(2):
                        tb = tbg * 2 + ti
                        pi = oi * 2 + ti
                        kb.ins("dve", lambda: nc.vector.tensor_tensor(out=x[:, oc, tbs(tb)], in0=PS(pi), in1=x[:, oc, tbs(tb)], op=ALU.add),
                               R=[PT[pi], xt[oc][tb]], W=[xt[oc][tb]])

    def final_phase():
        for tb in range(NB):
            pi = next_ps()
            for c in range(8):
                s = c % 2
                kb.ins("act", lambda c=c, s=s: nc.scalar.activation(sq[s][:], x[:, c, tbs(tb)], AF.Square), R=[xt[c][tb]], W=[sqt[s]])
                mm(PS(pi), onesm[:], sq[s][:], c == 0, c == 7, R=[ct, sqt[s]], W=[PT[pi]])
            kb.ins("act", lambda: nc.scalar.activation(rstd[:], PS(pi), AF.Ln, bias=1e-6, scale=1.0), R=[PT[pi]], W=[rstdt])
            kb.ins("act", lambda: nc.scalar.activation(rstd[:], rstd[:], AF.Exp, scale=-0.5), R=[rstdt], W=[rstdt])
            for c in range(8):
                kb.ins("dve", lambda c=c: nc.vector.scalar_tensor_tensor(
                    out=x[:, c, tbs(tb)], in0=x[:, c, tbs(tb)], scalar=pv[:, 0, 16 + c:17 + c], in1=rstd[:],
                    op0=ALU.mult, op1=ALU.mult), R=[xt[c][tb], pvt, rstdt], W=[xt[c][tb]])

    def store_x():
        for c in range(8):
            kb.dma("sp", out_d.ap()[c * 128:(c + 1) * 128, :], x[:, c, :], R=xt[c])

    done = False
    for l in range(n_layers):
        rmsnorm_to_h(l, 0)
        attention_phase(l)
        if stop_after == "attn%d" % l:
            done = True
            break
        rwkv_phase(l)
        if stop_after == "mix%d" % l:
            done = True
            break
        ffn_phase(l, moe=(l % 2 == 1))
        if stop_after == "ffn%d" % l:
            done = True
            break
    if not done:
        final_phase()
    store_x()
    kb.finish()
    kb.close()
    return kb


def host_inputs(inp):
    f = lambda a: np.ascontiguousarray(a, dtype=np.float32)
    L = 2
    w_in = inp["w_in"]
    win = f(w_in.reshape(L, 8, 128, 26, 128).transpose(0, 3, 2, 1, 4).reshape(L, 26, 128, 1024))
    wv = f(win[:, 22:26].reshape(L, 4, 128, 1024).transpose(0, 2, 1, 3).reshape(L, 128, 4096))
    wout = f(inp["w_out"].reshape(L, 8, 128, 8, 128).transpose(0, 3, 2, 1, 4).reshape(L, 8, 128, 1024))
    loraw = f(np.concatenate([inp["rwkv_w2"], inp["rwkv_a2"]], axis=1))
    g2 = f(inp["rwkv_g2"])

    def experts(wg, wu, wd, ne):
        a = f(wg.reshape(ne, 8, 128, 11, 128).transpose(0, 3, 2, 1, 4).reshape(ne, 11, 128, 1024))
        b = f(wu.reshape(ne, 8, 128, 11, 128).transpose(0, 3, 2, 1, 4).reshape(ne, 11, 128, 1024))
        c = f(wd.reshape(ne, 11, 128, 1024))
        return a, b, c
    dg = inp["ffn_w_gate"][0].reshape(1024, 2, 1408).transpose(1, 0, 2)
    du = inp["ffn_w_up"][0].reshape(1024, 2, 1408).transpose(1, 0, 2)
    dd = inp["ffn_w_down"][0].reshape(2, 1408, 1024)
    wg0, wu0, wd0 = experts(dg, du, dd, 2)
    wg1, wu1, wd1 = experts(inp["moe_w_gate"][0], inp["moe_w_up"][0], inp["moe_w_down"][0], 8)
    router = f(inp["moe_router"][0].reshape(8, 128, 8).transpose(1, 0, 2).reshape(128, 64))

    pv = np.zeros((L, 128, NPV), np.float32)
    col = lambda v: v.reshape(-1, 128).T
    for l in range(L):
        pv[l, :, 0:8] = col(inp["norm_mix_g"][l])
        pv[l, :, 8:16] = col(inp["norm_ffn_g"][l])
        pv[l, :, 16:24] = col(inp["norm_final_g"])
        pv[l, :, 24:38] = col(inp["shift_mu"][l])
        pv[l, :, 52:56] = col(inp["rwkv_w0"][l])
        pv[l, :, 56:60] = col(inp["rwkv_a0"][l])
        pv[l, :, 60:64] = col(inp["rwkv_k_k"][l])
        pv[l, :, 64:68] = col(inp["rwkv_k_a"][l])
        pv[l, :, 68:72] = col(inp["rwkv_r_k"][l])
        pv[l, :, 72:76] = col(inp["rwkv_ln_w"][l])
        pv[l, :, 76:80] = col(inp["rwkv_ln_b"][l])
    attn_g_bc = f(np.broadcast_to(inp["attn_norm_g"][:, None, :], (L, 128, 512)))
    ki = np.arange(128)[:, None]
    qi = np.arange(128)[None, :]
    biasT = np.zeros((L, 8, 128, 5, 128), np.float32)
    for s in range(5):
        rel = 128 * (4 - s) + qi - ki
        idx = np.clip(rel, -128, 128) + 128
        valid = np.ones((128, 128), bool)
        if s == 4:
            valid = ~((ki >= 64) & (qi < 64))
        if s == 0:
            valid = ~((ki < 64) & (qi >= 64))
        for l in range(L):
            g = inp["attn_rel_bias"][l][idx]
            g = np.where(valid[:, :, None], g, np.float32(-1e30))
            biasT[l, :, :, s, :] = g.transpose(2, 0, 1)
    biasT = f(biasT.reshape(L, 8, 128, 640))
    ident = np.eye(128, dtype=np.float32)
    onesm = np.full((128, 128), 1.0 / 1024, np.float32)
    blk = np.kron(np.eye(2, dtype=np.float32), np.ones((64, 64), np.float32))
    s_ = np.arange(128)[:, None]; t_ = np.arange(128)[None, :]
    same = (s_ // 64) == (t_ // 64)
    tri_i = (same & (s_ <= t_)).astype(np.float32)
    tri_e = (same & (s_ < t_)).astype(np.float32)
    s6 = np.arange(64)[:, None]; t6 = np.arange(64)[None, :]
    strict = (s6 < t6).astype(np.float32); incl = (s6 <= t6).astype(np.float32)
    lower = (t6 < s6).astype(np.float32)
    mA = np.concatenate([strict, incl, strict, incl], axis=1)
    maskA = np.concatenate([mA, mA], axis=1)
    mB = np.concatenate([lower, np.ones((64, 192), np.float32)], axis=1)
    maskB = np.concatenate([mB, mB], axis=1)
    sel = np.zeros((8, 8, 128), np.float32)
    for e in range(8):
        sel[e, e, :] = 1.0
    shared = {"pv": pv, "attn_g_bc": attn_g_bc, "biasT": biasT, "win": win, "wv": wv, "wout": wout, "loraw": loraw, "g2": g2,
              "wg0": wg0, "wu0": wu0, "wd0": wd0, "wg1": wg1, "wu1": wu1, "wd1": wd1, "router": router,
              "c_ident": ident, "c_onesm": onesm, "c_blk": blk, "c_tri_i": tri_i, "c_tri_e": tri_e,
              "c_maskA": f(maskA), "c_maskB": f(maskB), "c_sel": f(sel.reshape(8, 1024))}
    return shared


_PROG = {}


def kernel(**inputs):
    inp = {k: np.asarray(v) for k, v in inputs.items()}
    shared = host_inputs(inp)
    if "kb" not in _PROG:
        _PROG["kb"] = build_program()
    kb = _PROG["kb"]
    x = inp["x"]
    in_maps = []
    for b in range(8):
        m = dict(shared)
        m["xT"] = np.ascontiguousarray(x[b].T)
        in_maps.append(m)
    res = run_bass_kernel_spmd(kb.nc, in_maps, core_ids=list(range(8)))
    out = np.stack([np.ascontiguousarray(res.results[b]["outT"].T) for b in range(8)], axis=0)
    return out.astype(np.float32)
```

```python
import numpy as np
from contextlib import ExitStack
import concourse.bass as bass
import concourse.mybir as mybir
from concourse.bass_utils import run_bass_kernel_spmd

F32 = mybir.dt.float32
BF16 = mybir.dt.bfloat16
AF = mybir.ActivationFunctionType
ALU = mybir.AluOpType
AX = mybir.AxisListType

T = 2048
NB = 4
NT = 16
SEM_LIMIT = 60000
NPV = 80
DECAY_C = 0.6065306597126334


class Trk:
    __slots__ = ("w", "r")

    def __init__(self):
        self.w = None
        self.r = {}


class KB:
    def __init__(self, n_dma_sems=12):
        self.nc = bass.Bass("TRN2", target_bir_lowering=False)
        self.es = ExitStack()
        nc = self.nc
        self.eh = {"pe": nc.tensor, "act": nc.scalar, "dve": nc.vector, "pool": nc.gpsimd, "sp": nc.sync}
        self.sem = {}
        self.cnt = {}
        self.cur = {}
        self.eng_of = {}
        self.gen = {}
        self.known = {e: {} for e in self.eh}
        for e in self.eh:
            self._newgen(e, e)
        self.dma_pools = {"sp": ["d%d" % j for j in range(6)], "act": ["d%d" % j for j in range(6)], "pool": ["g%d" % j for j in range(8)]}
        self.dma_names = self.dma_pools["sp"] + self.dma_pools["pool"]
        for d in self.dma_names:
            self._newgen(d, None)
        self.dma_rr = {"sp": 0, "act": 0, "pool": 0}
        self.nwaits = 0
        self.nins = 0
        self.total_ins = {e: 0 for e in self.eh}

    def _newgen(self, name, eng):
        g = self.gen.get(name, -1) + 1
        self.gen[name] = g
        key = "%s_%d" % (name, g)
        self.sem[key] = self.es.enter_context(self.nc.semaphore("s_" + key))
        self.cnt[key] = 0
        self.cur[name] = key
        self.eng_of[key] = eng
        return key

    def sbuf(self, name, shape, dt, stack=None):
        self.uid = getattr(self, "uid", 0) + 1
        return (stack or self.es).enter_context(self.nc.sbuf_tensor("%s_%d" % (name, self.uid), list(shape), dt))

    def psum(self, name, shape, dt=F32):
        return self.es.enter_context(self.nc.psum_tensor(name, list(shape), dt))

    def _wait(self, eng, key, val):
        if val <= 0:
            return
        kn = self.known[eng]
        if kn.get(key, 0) >= val:
            return
        self.eh[eng].wait_ge(self.sem[key], val)
        kn[key] = val
        self.nwaits += 1

    def _deps(self, eng, R, W):
        deps = {}
        cur = self.cur[eng]
        for t in R:
            if t.w is not None:
                k, v = t.w
                if k == cur:
                    if eng != "pe":
                        self._wait(eng, k, v)
                elif self.eng_of[k] != eng:
                    if deps.get(k, 0) < v:
                        deps[k] = v
        for t in W:
            if t.w is not None:
                k, v = t.w
                if self.eng_of[k] != eng and deps.get(k, 0) < v:
                    deps[k] = v
            for k, v in t.r.items():
                if self.eng_of[k] != eng and deps.get(k, 0) < v:
                    deps[k] = v
        for k, v in deps.items():
            self._wait(eng, k, v)

    def _mark(self, key, c, R, W):
        for t in W:
            t.w = (key, c)
            t.r = {}
        for t in R:
            if t.r.get(key, 0) < c:
                t.r[key] = c

    def ins(self, eng, fn, R=(), W=()):
        cur = self.cur[eng]
        if self.cnt[cur] >= SEM_LIMIT:
            self.eh[eng].wait_ge(self.sem[cur], self.cnt[cur])
            cur = self._newgen(eng, eng)
        self._deps(eng, R, W)
        inst = fn()
        inst.then_inc(self.sem[cur], 1)
        self.cnt[cur] += 1
        self._mark(cur, self.cnt[cur], R, W)
        self.nins += 1
        self.total_ins[eng] += 1
        return inst

    def dma(self, q, out, in_, R=(), W=(), **kw):
        pool_ = self.dma_pools[q]
        name = pool_[self.dma_rr[q] % len(pool_)]
        self.dma_rr[q] += 1
        key = self.cur[name]
        self._wait(q, key, self.cnt[key])
        if self.cnt[key] >= SEM_LIMIT:
            key = self._newgen(name, None)
        self._deps(q, R, W)
        for t in list(R) + list(W):
            if t.w is not None and self.eng_of[t.w[0]] == q:
                self.eh[q].wait_ge(self.sem[t.w[0]], t.w[1])
        for t in W:
            for k, v in t.r.items():
                if self.eng_of[k] == q:
                    self.eh[q].wait_ge(self.sem[k], v)
        inst = self.eh[q].dma_start(out=out, in_=in_, **kw)
        inst.then_inc(self.sem[key], 16)
        self.cnt[key] += 16
        self._mark(key, self.cnt[key], R, W)
        self.nins += 1
        return inst

    def barrier(self):
        keys = [self.cur[n] for n in self.dma_names] + [self.cur[e] for e in self.eh]
        for e in self.eh:
            for k in keys:
                if self.eng_of[k] != e:
                    self._wait(e, k, self.cnt[k])

    def finish(self, eng="sp"):
        for name in self.dma_names:
            key = self.cur[name]
            self._wait(eng, key, self.cnt[key])
        for e in self.eh:
            if e != eng:
                key = self.cur[e]
                self._wait(eng, key, self.cnt[key])

    def close(self):
        self.es.close()


def build_program(stop_after="final", n_layers=2, dump=None, dbg=None):
    kb = KB()
    nc = kb.nc
    D = {}

    def din(name, shape):
        D[name] = nc.dram_tensor(name, list(shape), F32, kind="ExternalInput")
        return D[name]

    xT_d = din("xT", [1024, T])
    pv_d = din("pv", [2, 128, NPV])
    agbc_d = din("attn_g_bc", [2, 128, 512])
    biasT_d = din("biasT", [2, 8, 128, 640])
    win_d = din("win", [2, 26, 128, 1024])
    wv_d = din("wv", [2, 128, 4096])
    wout_d = din("wout", [2, 8, 128, 1024])
    loraw_d = din("loraw", [2, 128, 512])
    g2_d = din("g2", [2, 128, 512])
    ew_d = []
    for l, ne in enumerate((2, 8)):
        ew_d.append((din("wg%d" % l, [ne, 11, 128, 1024]), din("wu%d" % l, [ne, 11, 128, 1024]),
                     din("wd%d" % l, [ne, 11, 128, 1024])))
    router_d = din("router", [128, 64])
    c_ident_d = din("c_ident", [128, 128])
    c_onesm_d = din("c_onesm", [128, 128])
    c_blk_d = din("c_blk", [128, 128])
    c_tri_i_d = din("c_tri_i", [128, 128])
    c_tri_e_d = din("c_tri_e", [128, 128])
    c_maskA_d = din("c_maskA", [64, 512])
    c_maskB_d = din("c_maskB", [64, 512])
    c_sel_d = din("c_sel", [8, 1024])
    out_d = nc.dram_tensor("outT", [1024, T], F32, kind="ExternalOutput")

    sb = kb.sbuf
    x = sb("x", [128, 8, T], F32)
    xt = [[Trk() for _ in range(NB)] for _ in range(8)]
    h = sb("h", [128, 8, T], BF16)
    ht = [Trk() for _ in range(NB)]
    pv = sb("pvs", [128, 2, NPV], F32); pvt = Trk()
    pvd = sb("pvd", [128, 2, 24], F32); pvdt = Trk()
    ident_f = sb("ident_f", [128, 128], F32)
    ident_b = sb("ident_b", [128, 128], BF16)
    onesm = sb("onesm", [128, 128], BF16)
    blk = sb("blk", [128, 128], BF16)
    tri_i = sb("tri_i", [128, 128], F32)
    tri_e = sb("tri_e", [128, 128], F32)
    maskA = sb("maskA", [64, 512], BF16)
    maskB = sb("maskB", [64, 512], BF16)
    sel = sb("sel", [8, 1024], BF16)
    ct = Trk()
    wbuf = sb("wbuf", [128, 4, 8, 128], BF16)
    wbt = [Trk() for _ in range(4)]
    wb_rr = [0]
    sq = [sb("sq%d" % i, [128, 512], BF16) for i in range(2)]
    sqt = [Trk() for _ in range(2)]
    rstd = sb("rstd", [128, 512], F32); rstdt = Trk()

    PP = [kb.psum("pp%d" % i, [128, 1024]) for i in range(4)]
    PT = [Trk() for _ in range(8)]

    def PS(i):
        return PP[i // 2][:, (i % 2) * 512:(i % 2) * 512 + 512]

    ps_rr = [0]

    def next_ps(lo=0, hi=8):
        i = lo + (ps_rr[0] % (hi - lo))
        ps_rr[0] += 1
        return i

    for tb in range(NB):
        for c in range(8):
            kb.dma("sp", x[:, c, tb * 512:(tb + 1) * 512], xT_d.ap()[c * 128:(c + 1) * 128, tb * 512:(tb + 1) * 512], W=[xt[c][tb]])
    kb.dma("sp", pv[:, 0, :], pv_d.ap()[0], W=[pvt])
    kb.dma("sp", pv[:, 1, :], pv_d.ap()[1], W=[pvt])
    kb.dma("sp", ident_f[:], c_ident_d.ap(), W=[ct])
    kb.dma("sp", tri_i[:], c_tri_i_d.ap(), W=[ct])
    kb.dma("sp", tri_e[:], c_tri_e_d.ap(), W=[ct])
    kb.dma("pool", sel[:], c_sel_d.ap(), W=[ct])
    kb.dma("pool", ident_b[:], c_ident_d.ap(), W=[ct])
    kb.dma("pool", onesm[:], c_onesm_d.ap(), W=[ct])
    kb.dma("pool", blk[:], c_blk_d.ap(), W=[ct])
    kb.dma("pool", maskA[:], c_maskA_d.ap(), W=[ct])
    kb.dma("pool", maskB[:], c_maskB_d.ap(), W=[ct])
    for l in range(2):
        kb.ins("dve", lambda l=l: nc.vector.tensor_scalar(pvd[:, l, 0:14], pv[:, l, 24:38], -1.0, 1.0, op0=ALU.mult, op1=ALU.add),
               R=[pvt], W=[pvdt])
        kb.ins("dve", lambda l=l: nc.vector.tensor_scalar(pvd[:, l, 14:18], pv[:, l, 64:68], -1.0, 1.0, op0=ALU.mult, op1=ALU.add),
               R=[pvt], W=[pvdt])

    def load_w(src_ap, slot=None, shape4=None):
        if slot is None:
            slot = wb_rr[0] % 4
            wb_rr[0] += 1
        kb.dma("pool", wbuf[:, slot, :, :].rearrange("p k j -> p (k j)"), src_ap, W=[wbt[slot]])
        return slot

    evac_rr = [0]

    def evac_copy(out_ap, in_ap, R, W, eng=None):
        if eng is None:
            eng = "act" if (evac_rr[0] % 2 == 0) else "dve"
            evac_rr[0] += 1
        if eng == "act":
            kb.ins("act", lambda: nc.scalar.copy(out_ap, in_ap), R=R, W=W)
        else:
            kb.ins("dve", lambda: nc.vector.tensor_copy(out_ap, in_ap), R=R, W=W)

    def mm(out_ap, lhsT, rhs, start, stop, R, W):
        kb.ins("pe", lambda: nc.tensor.matmul(out_ap, lhsT, rhs, start=start, stop=stop), R=R, W=W)

    def tbs(tb):
        return slice(tb * 512, (tb + 1) * 512)

    def rmsnorm_to_h(l, gcol, after_tb=None):
        for tb in range(NB):
            pi = next_ps(0, 7)
            for c in range(8):
                s = c % 2
                kb.ins("act", lambda c=c, s=s: nc.scalar.activation(sq[s][:], x[:, c, tbs(tb)], AF.Square),
                       R=[xt[c][tb]], W=[sqt[s]])
                mm(PS(pi), onesm[:], sq[s][:], c == 0, c == 7, R=[ct, sqt[s]], W=[PT[pi]])
            kb.ins("act", lambda: nc.scalar.activation(rstd[:], PS(pi), AF.Ln, bias=1e-6, scale=1.0), R=[PT[pi]], W=[rstdt])
            kb.ins("act", lambda: nc.scalar.activation(rstd[:], rstd[:], AF.Exp, scale=-0.5), R=[rstdt], W=[rstdt])
            for c in range(8):
                kb.ins("dve", lambda c=c: nc.vector.scalar_tensor_tensor(
                    out=h[:, c, tbs(tb)], in0=x[:, c, tbs(tb)], scalar=pv[:, l, gcol + c:gcol + c + 1], in1=rstd[:],
                    op0=ALU.mult, op1=ALU.mult), R=[xt[c][tb], pvt, rstdt], W=[ht[tb]])
            if after_tb is not None:
                after_tb(tb)

    def attention_phase(l):
        st = ExitStack()
        qk = sb("qk", [128, 8, T], BF16, st)
        qkt = [[Trk() for _ in range(NT)] for _ in range(8)]
        Vp = sb("Vp", [128, NT, 8, 65], BF16, st); vpt = [Trk() for _ in range(NT)]
        Mh = sb("Mh", [128, 8, 640], BF16, st); mht = Trk()
        bst = sb("bst", [128, 640], F32, st); bstt = Trk()
        E = [sb("E%d" % i, [128, 640], BF16, st) for i in range(2)]; et = [Trk() for _ in range(2)]
        o = sb("o_att", [128, 8, 64], F32, st); ot = Trk()
        yo = sb("yo_att", [128, 512], F32, st); yot = Trk()
        junk = sb("junk_att", [128, 512], BF16, st); junkt = Trk()
        gbc = sb("gbc", [128, 512], F32, st); gbct = Trk()
        rec = sb("rec", [128, 8], F32, st); rect = Trk()
        ssq = sb("ssq", [128, 2], F32, st); ssqt = Trk()

        kb.dma("sp", gbc[:], agbc_d.ap()[l], W=[gbct])
        kb.ins("pool", lambda: nc.gpsimd.memset(Vp[:, :, :, 64:65], 1.0), W=vpt)
        for hh in range(8):
            kb.dma("sp", bst[:], biasT_d.ap()[l, hh], W=[bstt])
            kb.ins("act", lambda hh=hh: nc.scalar.activation(Mh[:, hh, :], bst[:], AF.Exp), R=[bstt], W=[mht])
        for ci in range(8):
            slot = load_w(win_d.ap()[l, 14 + ci])
            for tb in range(NB):
                pi = next_ps()
                for k in range(8):
                    mm(PS(pi), wbuf[:, slot, k, :], h[:, k, tbs(tb)], k == 0, k == 7, R=[wbt[slot], ht[tb]], W=[PT[pi]])
                evac_copy(qk[:, ci, tbs(tb)], PS(pi), R=[PT[pi]], W=qkt[ci][tb * 4:tb * 4 + 4])
        kb.dma("pool", wbuf[:].rearrange("p j k c -> p (j k c)"), wv_d.ap()[l], W=wbt)
        for tt in range(NT):
            pi = next_ps()
            for k in range(8):
                mm(PS(pi), h[:, k, tt * 128:(tt + 1) * 128], wbuf[:, :, k, :], k == 0, k == 7, R=wbt + [ht[tt // 4]], W=[PT[pi]])
            evac_copy(Vp[:, tt, :, 0:64], PS(pi).rearrange("p (h d) -> p h d", h=8), R=[PT[pi]], W=[vpt[tt]])
        items = [(m, hh) for m in range(NT) for hh in range(8)]

        def jlist(m):
            js = [j for j in range(m - 4, m + 1) if j >= 0]
            return js, 5 - len(js)

        def stageA(i):
            m, hh = items[i]
            js, s0 = jlist(m)
            hp, r0 = hh // 2, (hh % 2) * 64
            sp_ = i % 2
            Sps = PP[sp_]
            St = [PT[2 * sp_], PT[2 * sp_ + 1]]
            for idx, j in enumerate(js):
                sl = s0 + idx
                mm(Sps[:, sl * 128:(sl + 1) * 128], qk[r0:r0 + 64, 4 + hp, j * 128:(j + 1) * 128],
                   qk[r0:r0 + 64, hp, m * 128:(m + 1) * 128], True, True,
                   R=[qkt[4 + hp][j], qkt[hp][m]], W=St)
            eb = i % 2
            kb.ins("act", lambda: nc.scalar.activation(E[eb][:, s0 * 128:640], Sps[:, s0 * 128:640], AF.Exp, scale=0.125),
                   R=St, W=[et[eb]])
            kb.ins("dve", lambda: nc.vector.tensor_tensor(out=E[eb][:, s0 * 128:640], in0=E[eb][:, s0 * 128:640],
                                                          in1=Mh[:, hh, s0 * 128:640], op=ALU.mult),
                   R=[et[eb], mht], W=[et[eb]])

        def stageB(i):
            m, hh = items[i]
            js, s0 = jlist(m)
            eb = i % 2
            og = 4 + hh // 4
            for idx, j in enumerate(js):
                sl = s0 + idx
                mm(PS(og)[:, (hh % 4) * 65:(hh % 4) * 65 + 65], E[eb][:, sl * 128:(sl + 1) * 128], Vp[:, j, hh, :],
                   idx == 0, idx == len(js) - 1, R=[et[eb], vpt[j]], W=[PT[og]])

        def epilogue(m):
            for g in range(2):
                Og = PS(4 + g)[:, 0:260].rearrange("p (h d) -> p h d", h=4)
                kb.ins("dve", lambda: nc.vector.reciprocal(rec[:, g * 4:g * 4 + 4], Og[:, :, 64]), R=[PT[4 + g]], W=[rect])
                kb.ins("dve", lambda: nc.vector.tensor_tensor(out=o[:, g * 4:g * 4 + 4, :], in0=Og[:, :, 0:64],
                                                              in1=rec[:, g * 4:g * 4 + 4].unsqueeze(2).to_broadcast([128, 4, 64]),
                                                              op=ALU.mult), R=[PT[4 + g], rect], W=[ot])
            of = o[:].rearrange("p h d -> p (h d)")
            kb.ins("pool", lambda: nc.gpsimd.memset(ssq[:, 0:1], 0.0), W=[ssqt])
            kb.ins("act", lambda: nc.scalar.activation(junk[:], of, AF.Square, accum_out=ssq[:, 0:1]), R=[ot, ssqt], W=[junkt, ssqt])
            kb.ins("act", lambda: nc.scalar.activation(ssq[:, 1:2], ssq[:, 0:1], AF.Ln, bias=1e-6, scale=1.0 / 512), R=[ssqt], W=[ssqt])
            kb.ins("act", lambda: nc.scalar.activation(ssq[:, 1:2], ssq[:, 1:2], AF.Exp, scale=-0.5), R=[ssqt], W=[ssqt])
            kb.ins("dve", lambda: nc.vector.scalar_tensor_tensor(out=yo[:], in0=of, scalar=ssq[:, 1:2], in1=gbc[:],
                                                                 op0=ALU.mult, op1=ALU.mult), R=[ot, ssqt, gbct], W=[yot])
            pi = 6 + (m % 2)
            for c in range(4):
                mm(PS(pi)[:, c * 128:(c + 1) * 128], yo[:, c * 128:(c + 1) * 128], ident_f[:], True, True, R=[yot, ct], W=[PT[pi]])
            evac_copy(qk[:, 0:4, m * 128:(m + 1) * 128], PS(pi).rearrange("p (c t) -> p c t", c=4), R=[PT[pi]],
                      W=[qkt[c][m] for c in range(4)])

        stageA(0)
        for i in range(len(items)):
            if i + 1 < len(items):
                stageA(i + 1)
            stageB(i)
            if items[i][1] == 7:
                epilogue(items[i][0])
        for oc in range(8):
            slot = load_w(wout_d.ap()[l, oc])
            for tb in range(NB):
                pi = next_ps(0, 4)
                for k in range(4):
                    mm(PS(pi), wbuf[:, slot, 4 + k, :], qk[:, k, tbs(tb)], k == 0, k == 3,
                       R=[wbt[slot]] + qkt[k][tb * 4:tb * 4 + 4], W=[PT[pi]])
                kb.ins("dve", lambda: nc.vector.tensor_tensor(out=x[:, oc, tbs(tb)], in0=PS(pi), in1=x[:, oc, tbs(tb)], op=ALU.add),
                       R=[PT[pi], xt[oc][tb]], W=[xt[oc][tb]])
        kb.barrier()
        st.close()

    def rwkv_phase(l):
        BS = 256
        NBLK = T // BS
        NCH = BS // 64
        NQ = BS // 128
        NSTREAM = 2
        st = ExitStack()
        lora1 = sb("lora1", [128, T], BF16, st); l1t = [Trk() for _ in range(NB)]
        lora2 = sb("lora2", [128, T], BF16, st); l2t = [Trk() for _ in range(NB)]
        loraw = sb("loraw_s", [128, 512], BF16, st); lwt = Trk()
        g2s = sb("g2_s", [128, 512], BF16, st); g2t = Trk()
        wx = sb("r_wx", [128, 2, 8, 128], BF16, st); wxt = [Trk(), Trk()]
        GNB = sb("r_GNB", [64, 1], F32, st); GNBt = Trk()
        kb.ins("pool", lambda: nc.gpsimd.memset(GNB[:], 64e-5), W=[GNBt])
        kb.dma("pool", loraw[:], loraw_d.ap()[l], W=[lwt])
        kb.dma("pool", g2s[:], g2_d.ap()[l], W=[g2t])

        ps_free = [0, 1, 2, 3, 4, 5]

        def aps():
            return ps_free.pop(0)

        def fps(i):
            ps_free.append(i)

        def bsl(b):
            return slice(b * BS, (b + 1) * BS)

        shl = sb("shbuf_l", [128, 516], F32, st); shlt = Trk()
        carl = sb("carry_l", [128, 2], F32, st); carlt = Trk()
        tl = rstd; tlt = rstdt
        for ci, cc in enumerate((12, 13)):
            slot = load_w(win_d.ap()[l, cc])
            for tb in range(NB):
                pi = aps()
                for k in range(8):
                    mm(PS(pi), wbuf[:, slot, k, :], h[:, k, tbs(tb)], k == 0, k == 7, R=[wbt[slot], ht[tb]], W=[PT[pi]])
                mu = pv[:, l, 24 + cc:25 + cc]
                omm = pvd[:, l, cc:cc + 1]
                if tb == 0:
                    kb.ins("pool", lambda: nc.gpsimd.memset(shl[:, 0:1], 0.0), W=[shlt])
                else:
                    kb.ins("act", lambda: nc.scalar.copy(shl[:, 0:1], carl[:, ci:ci + 1]), R=[carlt], W=[shlt])
                kb.ins("act", lambda: nc.scalar.activation(shl[:, 1:513], PS(pi), AF.Identity, scale=mu), R=[PT[pi], pvt, shlt], W=[shlt])
                kb.ins("act", lambda: nc.scalar.copy(carl[:, ci:ci + 1], shl[:, 512:513]), R=[shlt], W=[carlt])
                kb.ins("dve", lambda: nc.vector.scalar_tensor_tensor(out=tl[:], in0=PS(pi), scalar=omm, in1=shl[:, 0:512],
                                                                     op0=ALU.mult, op1=ALU.add), R=[PT[pi], pvdt, shlt], W=[tlt])
                fps(pi)
                if cc == 12:
                    kb.ins("act", lambda: nc.scalar.activation(lora1[0:64, tbs(tb)], tl[0:64, :], AF.Tanh), R=[tlt], W=[l1t[tb]])
                    kb.ins("act", lambda: nc.scalar.copy(lora1[64:128, tbs(tb)], tl[64:128, :]), R=[tlt], W=[l1t[tb]])
                else:
                    kb.ins("act", lambda: nc.scalar.activation(lora2[:, tbs(tb)], tl[:], AF.Sigmoid), R=[tlt], W=[l2t[tb]])

        def stream(s, hps):
            def f32t(name):
                return sb(name, [128, BS], F32, st), Trk()

            def b16t(name):
                return sb(name, [128, BS], BF16, st), Trk()
            RKV = sb("r_RKV", [128, 3, BS], F32, st)
            Rt, Rtt = RKV[:, 0, :], Trk(); Kt, Ktt = RKV[:, 1, :], Trk(); Vt, Vtt = RKV[:, 2, :], Trk()
            SA = sb("r_SA", [128, 2, BS], F32, st)
            SG, SGt = SA[:, 0, :], Trk(); Aa, Aat = SA[:, 1, :], Trk(); Gt, Gtt = b16t("r_G")
            Yraw = RKV[0:64, 0:2, :].rearrange("p a (b v) -> p (a b) v", v=64)
            Ycb = SA[0:64, :, :].rearrange("p a (b v) -> p (a b) v", v=64)
            Ynb = sb("r_Ynb", [64, 2 * NCH, 64], BF16, st); Ynbt = Trk()
            kkn, kknt = f32t("r_kkn"); t1, t1t = f32t("r_t1"); t2, t2t = f32t("r_t2")
            eG, eGt = SG, SGt; eGx, eGxt = f32t("r_eGx"); eGn, eGnt = f32t("r_eGn")
            bv, bvt = f32t("r_bv")
            kk2, kk2t = b16t("r_kk2")
            ART = sb("r_ART", [128, NCH, 128], BF16, st); ARTt = Trk()
            bT, bTt = b16t("r_bT"); kT, kTt = b16t("r_kT"); vT, vTt = b16t("r_vT")
            sgT = eGn[:].rearrange("p (q f) -> p q f", q=NQ); sgTt = eGnt
            CA = sb("r_CA", [64, NCH, 512], BF16, st); CAt = Trk()
            CB = sb("r_CB", [64, NCH, 512], BF16, st); CBt = Trk()
            NI = 2 * NCH
            Xb = [sb("r_X%d" % i, [64, NI, 64], BF16, st) for i in range(2)]; Xbt = [Trk(), Trk()]
            Nb = [sb("r_N%d" % i, [64, NI, 64], BF16, st) for i in range(2)]; Nbt = [Trk(), Trk()]
            TTf = sb("r_TT", [64, NI, 64], BF16, st); TTft = Trk()
            ST = sb("r_ST", [64, 2, 64], F32, st); STt = Trk()
            STb = sb("r_STb", [64, 2, 64], BF16, st); STbt = Trk()
            WC = sb("r_WC", [64, 2, NCH], F32, st); WCt = Trk()
            WCs = sb("r_WCs", [128, NCH], F32, st); WCst = Trk()
            P1 = sb("r_P1", [64, 2, 64], BF16, st); P1t = Trk()
            U = sb("r_U", [64, 2, 64], BF16, st); Ut = Trk()
            Yc = sb("r_Yc", [64, 2, 64], F32, st); Yct = Trk()
            Ysq = sb("r_Ysq", [64, 2, 64], F32, st); Ysqt = Trk()
            Yn = sb("r_Yn", [64, 2, 64], BF16, st); Ynt = Trk()
            stat = sb("r_stat", [64, 8 * NCH], F32, st); statt = Trk()
            sh = sb("shbuf", [128, BS + 4], F32, st); sht = Trk()
            carry = sb("carry", [128, 4], F32, st); carryt = Trk()
            yrs = sb("yrs", [128, T], BF16, st); yrst = [Trk() for _ in range(NBLK)]
            if s == 0:
                wsl = [(wbuf[:, i, :, :], wbt[i]) for i in range(3)]
            else:
                wsl = [(wbuf[:, 3, :, :], wbt[3]), (wx[:, 0, :, :], wxt[0]), (wx[:, 1, :, :], wxt[1])]
            pyo = 6 + s

            def shifted_proj(wi, cc, b, out_ap, out_trk, ci):
                wap, wtr = wsl[wi]
                pi = aps()
                for k in range(8):
                    mm(PS(pi)[:, 0:BS], wap[:, k, :], h[:, k, bsl(b)], k == 0, k == 7, R=[wtr, ht[(b * BS) // 512]], W=[PT[pi]])
                mu = pv[:, l, 24 + cc:25 + cc]
                omm = pvd[:, l, cc:cc + 1]
                if b == 0:
                    kb.ins("pool", lambda: nc.gpsimd.memset(sh[:, 0:1], 0.0), W=[sht])
                else:
                    kb.ins("act", lambda: nc.scalar.copy(sh[:, 0:1], carry[:, ci:ci + 1]), R=[carryt], W=[sht])
                kb.ins("act", lambda: nc.scalar.activation(sh[:, 1:BS + 1], PS(pi)[:, 0:BS], AF.Identity, scale=mu), R=[PT[pi], pvt, sht], W=[sht])
                kb.ins("act", lambda: nc.scalar.copy(carry[:, ci:ci + 1], sh[:, BS:BS + 1]), R=[sht], W=[carryt])
                kb.ins("dve", lambda: nc.vector.scalar_tensor_tensor(out=out_ap, in0=PS(pi)[:, 0:BS], scalar=omm, in1=sh[:, 0:BS],
                                                                     op0=ALU.mult, op1=ALU.add), R=[PT[pi], pvdt, sht], W=[out_trk])
                fps(pi)

            for hp in hps:
                for wi, cc in enumerate((hp, 4 + hp, 8 + hp)):
                    kb.dma("pool", wsl[wi][0].rearrange("p k j -> p (k j)"), win_d.ap()[l, cc], W=[wsl[wi][1]])
                kb.ins("pool", lambda: nc.gpsimd.memset(ST[:], 0.0), W=[STt])
                kb.ins("pool", lambda: nc.gpsimd.memset(STb[:], 0.0), W=[STbt])
                w0 = pv[:, l, 52 + hp:53 + hp]; a0 = pv[:, l, 56 + hp:57 + hp]; k_k = pv[:, l, 60 + hp:61 + hp]
                k_a = pv[:, l, 64 + hp:65 + hp]; r_k = pv[:, l, 68 + hp:69 + hp]
                ln_w = pv[:, l, 72 + hp:73 + hp]; ln_b = pv[:, l, 76 + hp:77 + hp]
                omka = pvd[:, l, 14 + hp:15 + hp]
                cs = slice(hp * 128, (hp + 1) * 128)
                yield
                for b in range(NBLK):
                    tb = (b * BS) // 512
                    shifted_proj(0, hp, b, Rt[:], Rtt, 0)
                    yield
                    shifted_proj(1, 4 + hp, b, Kt[:], Ktt, 1)
                    yield
                    shifted_proj(2, 8 + hp, b, Vt[:], Vtt, 2)
                    yield
                    pi = aps()
                    mm(PS(pi)[:, 0:BS], loraw[0:64, cs], lora1[0:64, bsl(b)], True, True, R=[lwt, l1t[tb]], W=[PT[pi]])
                    kb.ins("act", lambda: nc.scalar.activation(SG[:], PS(pi)[:, 0:BS], AF.Sigmoid, bias=w0, scale=1.0), R=[PT[pi], pvt], W=[SGt])
                    fps(pi)
                    pi = aps()
                    mm(PS(pi)[:, 0:BS], loraw[64:128, cs], lora1[64:128, bsl(b)], True, True, R=[lwt, l1t[tb]], W=[PT[pi]])
                    kb.ins("act", lambda: nc.scalar.activation(Aa[:], PS(pi)[:, 0:BS], AF.Sigmoid, bias=a0, scale=1.0), R=[PT[pi], pvt], W=[Aat])
                    fps(pi)
                    pi = aps()
                    mm(PS(pi)[:, 0:BS], g2s[:, cs], lora2[:, bsl(b)], True, True, R=[g2t, l2t[tb]], W=[PT[pi]])
                    evac_copy(Gt[:], PS(pi)[:, 0:BS], R=[PT[pi]], W=[Gtt], eng="act")
                    fps(pi)
                    yield
                    kb.ins("act", lambda: nc.scalar.activation(kk2[:], Kt[:], AF.Square, scale=k_k), R=[Ktt, pvt], W=[kk2t])
                    pi = aps()
                    mm(PS(pi)[:, 0:BS], blk[:], kk2[:], True, True, R=[ct, kk2t], W=[PT[pi]])
                    kb.ins("act", lambda: nc.scalar.activation(t1[:], PS(pi)[:, 0:BS], AF.Ln, bias=1e-24, scale=1.0), R=[PT[pi]], W=[t1t])
                    fps(pi)
                    kb.ins("act", lambda: nc.scalar.activation(t1[:], t1[:], AF.Exp, scale=-0.5), R=[t1t], W=[t1t])
                    kb.ins("dve", lambda: nc.vector.scalar_tensor_tensor(out=kkn[:], in0=Kt[:], scalar=k_k, in1=t1[:], op0=ALU.mult, op1=ALU.mult),
                           R=[Ktt, pvt, t1t], W=[kknt])
                    kb.ins("dve", lambda: nc.vector.tensor_scalar(t2[:], Aa[:], k_a, omka, op0=ALU.mult, op1=ALU.add), R=[Aat, pvt, pvdt], W=[t2t])
                    kb.ins("pool", lambda: nc.gpsimd.tensor_tensor(out=t2[:], in0=t2[:], in1=Kt[:], op=ALU.mult), R=[t2t, Ktt], W=[t2t])
                    kb.ins("dve", lambda: nc.vector.scalar_tensor_tensor(out=kk2[:], in0=Rt[:], scalar=r_k, in1=t2[:], op0=ALU.mult, op1=ALU.mult),
                           R=[Rtt, pvt, t2t], W=[kk2t])
                    pi = aps()
                    mm(PS(pi)[:, 0:BS], blk[:], kk2[:], True, True, R=[ct, kk2t], W=[PT[pi]])
                    kb.ins("dve", lambda: nc.vector.tensor_tensor(out=bv[:], in0=PS(pi)[:, 0:BS], in1=Vt[:], op=ALU.mult), R=[PT[pi], Vtt], W=[bvt])
                    fps(pi)
                    yield
                    pi = aps()
                    for q4 in range(NQ):
                        mm(PS(pi)[:, q4 * 128:(q4 + 1) * 128], SG[:, q4 * 128:(q4 + 1) * 128], ident_f[:], True, True, R=[SGt, ct], W=[PT[pi]])
                    evac_copy(sgT, PS(pi)[:, 0:BS].rearrange("p (q f) -> p q f", q=NQ), R=[PT[pi]], W=[sgTt], eng="act")
                    fps(pi)
                    pg = aps(); pgx = aps()
                    for q4 in range(NQ):
                        mm(PS(pg)[:, q4 * 128:(q4 + 1) * 128], sgT[:, q4, :], tri_i[:], True, True, R=[sgTt, ct], W=[PT[pg]])
                    for q4 in range(NQ):
                        mm(PS(pgx)[:, q4 * 128:(q4 + 1) * 128], sgT[:, q4, :], tri_e[:], True, True, R=[sgTt, ct], W=[PT[pgx]])
                    kb.ins("act", lambda: nc.scalar.activation(eG[:], PS(pg)[:, 0:BS], AF.Exp, scale=-DECAY_C), R=[PT[pg]], W=[eGt])
                    kb.ins("act", lambda: nc.scalar.activation(eGn[:], PS(pg)[:, 0:BS], AF.Exp, scale=DECAY_C), R=[PT[pg]], W=[eGnt])
                    kb.ins("act", lambda: nc.scalar.activation(eGx[:], PS(pgx)[:, 0:BS], AF.Exp, scale=-DECAY_C), R=[PT[pgx]], W=[eGxt])
                    fps(pg); fps(pgx)
                    yield
                    ARTv = ART[:]
                    kb.ins("dve", lambda: nc.vector.scalar_tensor_tensor(out=ARTv[:, :, 0:64], in0=kkn[:].rearrange("p (c t) -> p c t", c=NCH), scalar=-1.0,
                                                                         in1=eGx[:].rearrange("p (c t) -> p c t", c=NCH), op0=ALU.mult, op1=ALU.mult),
                           R=[kknt, eGxt], W=[ARTt])
                    kb.ins("pool", lambda: nc.gpsimd.tensor_tensor(out=ARTv[:, :, 64:128], in0=Rt[:].rearrange("p (c t) -> p c t", c=NCH),
                                                                   in1=eG[:].rearrange("p (c t) -> p c t", c=NCH), op=ALU.mult), R=[Rtt, eGt], W=[ARTt])
                    kb.ins("dve", lambda: nc.vector.tensor_tensor(out=Aa[:], in0=kkn[:], in1=Aa[:], op=ALU.mult), R=[kknt, Aat], W=[Aat])
                    kb.ins("dve", lambda: nc.vector.tensor_tensor(out=bT[:], in0=Aa[:], in1=eGn[:], op=ALU.mult), R=[Aat, eGnt], W=[bTt])
                    kb.ins("pool", lambda: nc.gpsimd.tensor_tensor(out=kT[:], in0=t2[:], in1=eGn[:], op=ALU.mult), R=[t2t, eGnt], W=[kTt])
                    kb.ins("act", lambda: nc.scalar.copy(vT[:], Vt[:]), R=[Vtt], W=[vTt])
                    yield
                    kkb = kkn[:].bitcast(BF16)
                    t2b = t2[:].bitcast(BF16)
                    exb = eGx[:].bitcast(BF16)
                    ART1 = kkb[0:64, :].rearrange("p (c t) -> p c t", c=NCH)
                    bT1 = t2b[0:64, 0:BS]; kT1 = t2b[0:64, BS:2 * BS]; vT1 = exb[0:64, 0:BS]
                    ARTf = ART[:].rearrange("p c t -> p (c t)")
                    for (src, srct, dst, dstt, n) in ((ARTf, ARTt, kkb[0:64, :], kknt, 2 * BS), (bT[:], bTt, bT1, t2t, BS),
                                                    (kT[:], kTt, kT1, t2t, BS), (vT[:], vTt, vT1, eGxt, BS)):
                        pi = aps()
                        mm(PS(pi)[0:64, 0:n], ident_b[64:128, 64:128], src[64:128, :], True, True, R=[ct, srct], W=[PT[pi]])
                        evac_copy(dst, PS(pi)[0:64, 0:n], R=[PT[pi]], W=[dstt])
                        fps(pi)
                    pi = aps()
                    eGl = eG[:].rearrange("p (c t) -> p c t", c=NCH)[:, :, 63]
                    kb.ins("dve", lambda: nc.vector.tensor_copy(WCs[:], eGl), R=[eGt], W=[WCst])
                    mm(PS(pi)[0:64, 0:NCH], ident_f[64:128, 64:128], WCs[64:128, :], True, True, R=[ct, WCst], W=[PT[pi]])
                    kb.ins("dve", lambda: nc.vector.tensor_copy(WC[:, 0, :], WCs[0:64, :]), R=[WCst], W=[WCt])
                    kb.ins("dve", lambda: nc.vector.tensor_copy(WC[:, 1, :], PS(pi)[0:64, 0:NCH]), R=[PT[pi]], W=[WCt])
                    fps(pi)
                    yield

                    def opnd(hh):
                        if hh == 0:
                            return ART[0:64], bT[0:64, :], kT[0:64, :], vT[0:64, :], [ARTt, bTt, kTt, vTt]
                        return ART1, bT1, kT1, vT1, [kknt, t2t, t2t, eGxt]
                    for c in range(NCH):
                        pa = aps(); pb = aps()
                        cs64 = slice(c * 64, c * 64 + 64)
                        for hh in range(2):
                            ARh, bh, kh, vh, trs = opnd(hh)
                            A_ = PS(pa)[0:64, hh * 256:(hh + 1) * 256]
                            B_ = PS(pb)[0:64, hh * 256:(hh + 1) * 256]
                            mm(A_[:, 0:128], bh[:, cs64], ARh[:, c, :], True, True, R=trs, W=[PT[pa]])
                            mm(A_[:, 128:256], kh[:, cs64], ARh[:, c, :], True, True, R=trs, W=[PT[pa]])
                            mm(B_[:, 0:64], ARh[:, c, 0:64], bh[:, cs64], True, True, R=trs, W=[PT[pb]])
                            mm(B_[:, 64:128], bh[:, cs64], ident_b[0:64, 0:64], True, True, R=trs + [ct], W=[PT[pb]])
                            mm(B_[:, 128:192], kh[:, cs64], ident_b[0:64, 0:64], True, True, R=trs + [ct], W=[PT[pb]])
                            mm(B_[:, 192:256], vh[:, cs64], ident_b[0:64, 0:64], True, True, R=trs + [ct], W=[PT[pb]])
                        kb.ins("dve", lambda: nc.vector.tensor_tensor(out=CA[:, c, :], in0=PS(pa)[0:64, :], in1=maskA[:], op=ALU.mult),
                               R=[PT[pa], ct], W=[CAt])
                        kb.ins("dve", lambda: nc.vector.tensor_tensor(out=CB[:, c, :], in0=PS(pb)[0:64, :], in1=maskB[:], op=ALU.mult),
                               R=[PT[pb], ct], W=[CBt])
                        fps(pa); fps(pb)
                        yield
                    CAv = CA[:].rearrange("p c (h f) -> p (c h) f", h=2)
                    CBv = CB[:].rearrange("p c (h f) -> p (c h) f", h=2)
                    kb.ins("pool", lambda: nc.gpsimd.tensor_copy(Xb[0][:], CAv[:, :, 0:64]), R=[CAt], W=[Xbt[0]])
                    kb.ins("pool", lambda: nc.gpsimd.tensor_copy(Nb[0][:], CBv[:, :, 0:64]), R=[CBt], W=[Nbt[0]])
                    kb.ins("dve", lambda: nc.vector.tensor_tensor(out=TTf[:], in0=CAv[:, :, 0:64],
                                                                  in1=ident_b[0:64, 0:64].unsqueeze(1).to_broadcast([64, NI, 64]), op=ALU.add),
                           R=[CAt, ct], W=[TTft])
                    cur = 0
                    for lev in range(5):
                        nx = 1 - cur
                        px = aps(); pn = aps()
                        if lev < 4:
                            for i in range(NI):
                                mm(PS(px)[0:64, i * 64:(i + 1) * 64], Nb[cur][:, i, :], Xb[cur][:, i, :], True, True,
                                   R=[Nbt[cur], Xbt[cur]], W=[PT[px]])
                        for i in range(NI):
                            mm(PS(pn)[0:64, i * 64:(i + 1) * 64], Xb[cur][:, i, :], Nb[cur][:, i, :], True, True,
                               R=[Nbt[cur], Xbt[cur]], W=[PT[pn]])
                        if lev < 4:
                            evac_copy(Xb[nx][:], PS(px)[0:64, 0:NI * 64].rearrange("p (i f) -> p i f", i=NI), R=[PT[px]], W=[Xbt[nx]], eng="act")
                        evac_copy(Nb[nx][:], PS(pn)[0:64, 0:NI * 64].rearrange("p (i f) -> p i f", i=NI), R=[PT[pn]], W=[Nbt[nx]], eng="dve")
                        fps(px); fps(pn)
                        yield
                        pt = aps()
                        for i in range(NI):
                            mm(PS(pt)[0:64, i * 64:(i + 1) * 64], Nb[nx][:, i, :], TTf[:, i, :], True, True,
                               R=[Nbt[nx], TTft], W=[PT[pt]])
                        kb.ins("dve", lambda: nc.vector.tensor_tensor(out=TTf[:], in0=PS(pt)[0:64, 0:NI * 64].rearrange("p (i f) -> p i f", i=NI),
                                                                      in1=TTf[:], op=ALU.add), R=[PT[pt], TTft], W=[TTft])
                        fps(pt)
                        cur = nx
                        yield
                    W1 = Xb[0]; W1t = Xbt[0]; atok = Xb[1]; atokt = Xbt[1]; Ub = Nb[0]; Ubt = Nbt[0]; Atok = Nb[1]; Atokt = Nbt[1]
                    PhiT = W1; PhiTt = W1t; RpT = atok; RpTt = atokt
                    psA = aps(); psB = aps()
                    for c in range(NCH):
                        for hh in range(2):
                            ARh, bh, kh, vh, trs = opnd(hh)
                            i = 2 * c + hh
                            mm(PS(psA)[0:64, i * 64:(i + 1) * 64], CA[:, c, hh * 256 + 128:hh * 256 + 192], CB[:, c, hh * 256 + 192:hh * 256 + 256], True, True,
                               R=[CAt, CBt], W=[PT[psA]])
                            mm(PS(psB)[0:64, i * 64:(i + 1) * 64], ARh[:, c, 0:64], ident_b[0:64, 0:64], True, True, R=trs + [ct], W=[PT[psB]])
                    evac_copy(W1[:], PS(psA)[0:64, 0:NI * 64].rearrange("p (i f) -> p i f", i=NI), R=[PT[psA]], W=[W1t], eng="act")
                    evac_copy(atok[:], PS(psB)[0:64, 0:NI * 64].rearrange("p (i f) -> p i f", i=NI), R=[PT[psB]], W=[atokt], eng="dve")
                    fps(psA); fps(psB)
                    yield
                    psA = aps(); psB = aps()
                    for i in range(NI):
                        mm(PS(psA)[0:64, i * 64:(i + 1) * 64], TTf[:, i, :], W1[:, i, :], True, True, R=[TTft, W1t], W=[PT[psA]])
                        mm(PS(psB)[0:64, i * 64:(i + 1) * 64], TTf[:, i, :], atok[:, i, :], True, True, R=[TTft, atokt], W=[PT[psB]])
                    evac_copy(Ub[:], PS(psA)[0:64, 0:NI * 64].rearrange("p (i f) -> p i f", i=NI), R=[PT[psA]], W=[Ubt], eng="act")
                    evac_copy(Atok[:], PS(psB)[0:64, 0:NI * 64].rearrange("p (i f) -> p i f", i=NI), R=[PT[psB]], W=[Atokt], eng="dve")
                    fps(psA); fps(psB)
                    yield
                    psA = aps(); psB = aps()
                    for c in range(NCH):
                        for hh in range(2):
                            i = 2 * c + hh
                            mm(PS(psA)[0:64, i * 64:(i + 1) * 64], Atok[:, i, :], CB[:, c, hh * 256 + 64:hh * 256 + 128], True, True,
                               R=[Atokt, CBt], W=[PT[psA]])
                            mm(PS(psB)[0:64, i * 64:(i + 1) * 64], Atok[:, i, :], CA[:, c, hh * 256 + 64:hh * 256 + 128], True, True,
                               R=[Atokt, CAt], W=[PT[psB]])
                    evac_copy(PhiT[:], PS(psA)[0:64, 0:NI * 64].rearrange("p (i f) -> p i f", i=NI), R=[PT[psA]], W=[PhiTt], eng="act")
                    for hh in range(2):
                        ARh, bh, kh, vh, trs = opnd(hh)
                        kb.ins("dve", lambda: nc.vector.tensor_tensor(
                            out=RpT[:].rearrange("p (c h) f -> p c h f", h=2)[:, :, hh, :],
                            in0=PS(psB)[0:64, 0:NI * 64].rearrange("p (c h f) -> p c h f", h=2, f=64)[:, :, hh, :],
                            in1=ARh[:, :, 64:128], op=ALU.add), R=[PT[psB]] + trs, W=[RpTt])
                    fps(psA); fps(psB)
                    yield
                    for c in range(NCH):
                        pp = aps()
                        Yps = PS(pp)[0:64, 0:128]
                        pst = PS(pp)[0:64, 128:256]
                        for hh in range(2):
                            i = 2 * c + hh
                            fo = slice(hh * 64, hh * 64 + 64)
                            mm(pst[:, fo], CB[:, c, hh * 256 + 64:hh * 256 + 128], Ub[:, i, :], True, False, R=[CBt, Ubt], W=[PT[pp]])
                            mm(pst[:, fo], CB[:, c, hh * 256 + 128:hh * 256 + 192], CB[:, c, hh * 256 + 192:hh * 256 + 256], False, False,
                               R=[CBt], W=[PT[pp]])
                            mm(pst[:, fo], PhiT[:, i, :], STb[:, hh, :], False, True, R=[PhiTt, STbt], W=[PT[pp]])
                        for hh in range(2):
                            i = 2 * c + hh
                            fo = slice(hh * 64, hh * 64 + 64)
                            mm(Yps[:, fo], CA[:, c, hh * 256 + 64:hh * 256 + 128], Ub[:, i, :], True, False, R=[CAt, Ubt], W=[PT[pp]])
                            mm(Yps[:, fo], CA[:, c, hh * 256 + 192:hh * 256 + 256], CB[:, c, hh * 256 + 192:hh * 256 + 256], False, False,
                               R=[CAt, CBt], W=[PT[pp]])
                            mm(Yps[:, fo], RpT[:, i, :], STb[:, hh, :], False, True, R=[RpTt, STbt], W=[PT[pp]])
                        STf = ST[:].rearrange("p h v -> p (h v)")
                        kb.ins("dve", lambda: nc.vector.tensor_tensor(out=STf, in0=pst, in1=STf, op=ALU.add), R=[PT[pp], STt], W=[STt])
                        kb.ins("dve", lambda: nc.vector.tensor_tensor(out=ST[:], in0=ST[:], in1=WC[:, :, c:c + 1].to_broadcast([64, 2, 64]), op=ALU.mult),
                               R=[STt, WCt], W=[STt])
                        kb.ins("act", lambda: nc.scalar.copy(STb[:], ST[:]), R=[STt], W=[STbt])
                        kb.ins("act", lambda: nc.scalar.copy(Yraw[:, 2 * c:2 * c + 2, :], Yps.rearrange("p (h v) -> p h v", h=2)),
                               R=[PT[pp]], W=[Rtt, Ktt])
                        fps(pp)
                        yield
                    NI2 = 2 * NCH
                    kb.ins("dve", lambda: nc.vector.tensor_reduce(out=stat[:, 0:NI2], in_=Yraw, axis=AX.X, op=ALU.add), R=[Rtt, Ktt], W=[statt])
                    kb.ins("dve", lambda: nc.vector.tensor_scalar(stat[:, NI2:2 * NI2], stat[:, 0:NI2], -1.0 / 64, None, op0=ALU.mult), R=[statt], W=[statt])
                    kb.ins("pool", lambda: nc.gpsimd.tensor_tensor(out=Ycb, in0=Yraw, in1=stat[:, NI2:2 * NI2].unsqueeze(2).to_broadcast([64, NI2, 64]), op=ALU.add),
                           R=[Rtt, Ktt, statt], W=[SGt, Aat])
                    yield
                    kb.ins("pool", lambda: nc.gpsimd.tensor_tensor(out=Yraw, in0=Ycb, in1=Ycb, op=ALU.mult), R=[SGt, Aat], W=[Rtt, Ktt])
                    kb.ins("dve", lambda: nc.vector.tensor_reduce(out=stat[:, 2 * NI2:3 * NI2], in_=Yraw, axis=AX.X, op=ALU.add), R=[Rtt, Ktt], W=[statt])
                    kb.ins("act", lambda: nc.scalar.activation(stat[:, 3 * NI2:4 * NI2], stat[:, 2 * NI2:3 * NI2], AF.Ln, bias=GNB[:], scale=1.0 / 64),
                           R=[statt, GNBt], W=[statt])
                    yield
                    kb.ins("act", lambda: nc.scalar.activation(stat[:, 3 * NI2:4 * NI2], stat[:, 3 * NI2:4 * NI2], AF.Exp, scale=-0.5), R=[statt], W=[statt])
                    kb.ins("pool", lambda: nc.gpsimd.tensor_tensor(out=Ynb[:], in0=Ycb, in1=stat[:, 3 * NI2:4 * NI2].unsqueeze(2).to_broadcast([64, NI2, 64]), op=ALU.mult),
                           R=[SGt, Aat, statt], W=[Ynbt])
                    for c in range(NCH):
                        mm(PS(pyo)[:, c * 64:(c + 1) * 64], Ynb[:, 2 * c:2 * c + 2, :].rearrange("p h v -> p (h v)"), ident_b[0:64, 0:64], True, True,
                           R=[Ynbt, ct], W=[PT[pyo]])
                    yield
                    kb.ins("act", lambda: nc.scalar.activation(t1[:], PS(pyo)[:, 0:BS], AF.Identity, bias=ln_b, scale=ln_w), R=[PT[pyo], pvt], W=[t1t])
                    kb.ins("dve", lambda: nc.vector.tensor_tensor(out=t1[:], in0=t1[:], in1=bv[:], op=ALU.add), R=[t1t, bvt], W=[t1t])
                    kb.ins("dve", lambda: nc.vector.tensor_tensor(out=yrs[:, bsl(b)], in0=t1[:], in1=Gt[:], op=ALU.mult), R=[t1t, Gtt], W=[yrst[b]])
                    yield
                wap, wtr = wsl[0]
                kb.dma("pool", wap, wout_d.ap()[l].rearrange("o p (k j) -> p o k j", k=8)[:, :, hp, :], W=[wtr])
                for oc in range(8):
                    for tb in range(NB):
                        pi = aps()
                        mm(PS(pi), wap[:, oc, :], yrs[:, tbs(tb)], True, True, R=[wtr] + yrst[tb * 2:tb * 2 + 2], W=[PT[pi]])
                        kb.ins("dve", lambda: nc.vector.tensor_tensor(out=x[:, oc, tbs(tb)], in0=PS(pi), in1=x[:, oc, tbs(tb)], op=ALU.add),
                               R=[PT[pi], xt[oc][tb]], W=[xt[oc][tb]])
                        fps(pi)
                    yield

        gens = [stream(0, [0, 1]), stream(1, [2, 3])]
        alive = list(gens)
        first = True
        for _ in range(0):
            next(gens[0])
        while alive:
            for g in list(alive):
                try:
                    next(g)
                except StopIteration:
                    alive.remove(g)
            if first:
                first = False
                kb.min_free = min(getattr(kb, "min_free", 1 << 30), nc.sbuf_bytes_remaining)
        kb.barrier()
        st.close()

    def ffn_phase(l, moe):
        st = ExitStack()
        ne = 8 if moe else 2
        wg_d, wu_d, wd_d = ew_d[1 if moe else 0]
        hid = sb("hid", [128, 11, T], BF16, st); hidt = [[Trk() for _ in range(NB)] for _ in range(11)]
        slu = [sb("slu%d" % i, [128, 512], F32, st) for i in range(2)]; slut = [Trk(), Trk()]
        wdn = sb("wdn", [128, 11, 8, 128], BF16, st); wdnt = [Trk() for _ in range(11)]
        if moe:
            cbc = sb("cbc", [128, T], F32, st); cbct = [Trk() for _ in range(NB)]
            wdn_dummy = None
            combT = sb("combT", [8, T], BF16, st); combTt = [Trk() for _ in range(NT)]
            rtr = sb("rtr", [128, 8, 8], F32, st); rtrt = Trk()
            lg = sb("lg", [128, 8], F32, st); lgt = Trk()
            top = sb("top8", [128, 8], F32, st); topt = Trk()
            gts = sb("gts", [128, 4], F32, st); gtst = Trk()
            eq1 = sb("eq1", [128, 8], F32, st); eq1t = Trk()
            eq2 = sb("eq2", [128, 8], F32, st); eq2t = Trk()
            comb = sb("comb", [128, 8], F32, st); combt = Trk()
            xsq = sb("xsq", [128, 128], BF16, st); xsqt = Trk()
            rs = sb("rs_tok", [128, 2], F32, st); rst = Trk()
            kb.dma("sp", rtr[:].rearrange("p k e -> p (k e)"), router_d.ap(), W=[rtrt])
            for k in range(8):
                kb.ins("dve", lambda k=k: nc.vector.tensor_scalar(rtr[:, k, :], rtr[:, k, :], pv[:, l, 8 + k:9 + k], None, op0=ALU.mult),
                       R=[rtrt, pvt], W=[rtrt])
            lg3 = sb("lg3", [128, NT, 8], F32, st); lg3t = Trk()
            lgb = sb("lgb", [128, NT, 8], F32, st); lgbt = Trk()
            e1 = sb("e1", [128, NT, 8], F32, st); e1t = Trk()
            e2 = sb("e2", [128, NT, 8], F32, st); e2t = Trk()
            tp = sb("tp", [128, 6, NT], F32, st); tpt = Trk()
            PSl = PS(7).rearrange("p (t e) -> p t e", e=16)[:, 0:NT, :]

            def router_tb(tb):
                for q in range(4):
                    tt = tb * 4 + q
                    tsl = slice(tt * 128, (tt + 1) * 128)
                    for k in range(8):
                        mm(PSl[:, tt, 0:8], x[:, k, tsl], rtr[:, k, :], k == 0, k == 7, R=[xt[k][tb], rtrt], W=[PT[7]])
                    mm(PSl[:, tt, 8:9], rstd[0:1, q * 128:(q + 1) * 128], ident_f[0:1, 0:1], True, True, R=[rstdt, ct], W=[PT[7]])
            rmsnorm_to_h(l, 8, after_tb=router_tb)
            kb.ins("act", lambda: nc.scalar.copy(tp[:, 0, :], PSl[:, :, 8]), R=[PT[7]], W=[tpt])
            kb.ins("dve", lambda: nc.vector.tensor_tensor(out=lg3[:], in0=PSl[:, :, 0:8], in1=tp[:, 0, :].unsqueeze(2).to_broadcast([128, NT, 8]), op=ALU.mult),
                   R=[PT[7], tpt], W=[lg3t])
            kb.ins("dve", lambda: nc.vector.tensor_reduce(out=tp[:, 1, :], in_=lg3[:], axis=AX.X, op=ALU.max), R=[lg3t], W=[tpt])
            kb.ins("dve", lambda: nc.vector.tensor_tensor(out=e1[:], in0=lg3[:], in1=tp[:, 1, :].unsqueeze(2).to_broadcast([128, NT, 8]), op=ALU.is_equal),
                   R=[lg3t, tpt], W=[e1t])
            kb.ins("dve", lambda: nc.vector.scalar_tensor_tensor(out=lgb[:], in0=e1[:], scalar=-1e30, in1=lg3[:], op0=ALU.mult, op1=ALU.add),
                   R=[e1t, lg3t], W=[lgbt])
            kb.ins("dve", lambda: nc.vector.tensor_reduce(out=tp[:, 2, :], in_=lgb[:], axis=AX.X, op=ALU.max), R=[lgbt], W=[tpt])
            kb.ins("dve", lambda: nc.vector.tensor_tensor(out=e2[:], in0=lgb[:], in1=tp[:, 2, :].unsqueeze(2).to_broadcast([128, NT, 8]), op=ALU.is_equal),
                   R=[lgbt, tpt], W=[e2t])
            kb.ins("dve", lambda: nc.vector.tensor_tensor(out=tp[:, 3, :], in0=tp[:, 2, :], in1=tp[:, 1, :], op=ALU.subtract), R=[tpt], W=[tpt])
            kb.ins("act", lambda: nc.scalar.activation(tp[:, 3, :], tp[:, 3, :], AF.Exp), R=[tpt], W=[tpt])
            kb.ins("dve", lambda: nc.vector.tensor_scalar(tp[:, 3, :], tp[:, 3, :], 1.0, None, op0=ALU.add), R=[tpt], W=[tpt])
            kb.ins("dve", lambda: nc.vector.reciprocal(tp[:, 4, :], tp[:, 3, :]), R=[tpt], W=[tpt])
            kb.ins("dve", lambda: nc.vector.tensor_scalar(tp[:, 5, :], tp[:, 4, :], -1.0, 1.0, op0=ALU.mult, op1=ALU.add), R=[tpt], W=[tpt])
            kb.ins("dve", lambda: nc.vector.tensor_tensor(out=e1[:], in0=e1[:], in1=tp[:, 4, :].unsqueeze(2).to_broadcast([128, NT, 8]), op=ALU.mult),
                   R=[e1t, tpt], W=[e1t])
            kb.ins("dve", lambda: nc.vector.tensor_tensor(out=e2[:], in0=e2[:], in1=tp[:, 5, :].unsqueeze(2).to_broadcast([128, NT, 8]), op=ALU.mult),
                   R=[e2t, tpt], W=[e2t])
            kb.ins("dve", lambda: nc.vector.tensor_tensor(out=e1[:], in0=e1[:], in1=e2[:], op=ALU.add), R=[e1t, e2t], W=[e1t])
            for tt in range(NT):
                bnk = tt // 4
                mm(PS(bnk)[0:8, (tt % 4) * 128:(tt % 4 + 1) * 128], e1[:, tt, :], ident_f[:], True, True, R=[e1t, ct], W=[PT[bnk]])
            for bnk in range(4):
                evac_copy(combT[:, bnk * 512:(bnk + 1) * 512], PS(bnk)[0:8, :], R=[PT[bnk]], W=combTt[bnk * 4:bnk * 4 + 4])
        else:
            rmsnorm_to_h(l, 8)
        steps = [(e, hc) for e in range(ne) for hc in range(11)]

        def issue_loads(i):
            e_, hc_ = steps[i]
            base = 2 * (i % 2)
            load_w(wg_d.ap()[e_, hc_], slot=base)
            load_w(wu_d.ap()[e_, hc_], slot=base + 1)
        issue_loads(0)
        for i, (e, hc) in enumerate(steps):
            if hc == 0 and moe:
                for tb in range(NB):
                    pi = next_ps()
                    mm(PS(pi), sel[:, e * 128:(e + 1) * 128], combT[:, tbs(tb)], True, True, R=[ct] + combTt[tb * 4:tb * 4 + 4], W=[PT[pi]])
                    evac_copy(cbc[:, tbs(tb)], PS(pi), R=[PT[pi]], W=[cbct[tb]], eng="act")
            if i + 1 < len(steps):
                issue_loads(i + 1)
            if hc >= 1:
                for hq in ((0, 1) if hc == 1 else (hc,)):
                    kb.dma("pool", wdn[:, hq, :, :].rearrange("p o j -> p (o j)"), wd_d.ap()[e, hq], W=[wdnt[hq]])
            sg_ = 2 * (i % 2); su_ = sg_ + 1
            for tb in range(NB):
                pg = next_ps(); pu = next_ps()
                while pu == pg:
                    pu = next_ps()
                for k in range(8):
                    mm(PS(pg), wbuf[:, sg_, k, :], h[:, k, tbs(tb)], k == 0, k == 7, R=[wbt[sg_], ht[tb]], W=[PT[pg]])
                for k in range(8):
                    mm(PS(pu), wbuf[:, su_, k, :], h[:, k, tbs(tb)], k == 0, k == 7, R=[wbt[su_], ht[tb]], W=[PT[pu]])
                s = (hc * NB + tb) % 2
                kb.ins("act", lambda: nc.scalar.activation(slu[s][:], PS(pg), AF.Silu), R=[PT[pg]], W=[slut[s]])
                if moe:
                    kb.ins("dve", lambda: nc.vector.tensor_tensor(out=slu[s][:], in0=slu[s][:], in1=cbc[:, tbs(tb)], op=ALU.mult),
                           R=[slut[s], cbct[tb]], W=[slut[s]])
                kb.ins("dve", lambda: nc.vector.tensor_tensor(out=hid[:, hc, tbs(tb)], in0=PS(pu), in1=slu[s][:], op=ALU.mult),
                       R=[PT[pu], slut[s]], W=[hidt[hc][tb]])
            if hc == 10:
                down_proj(e, wd_d, hid, hidt, wdn, wdnt)
        kb.barrier()
        st.close()

    def down_proj(e, wd_d, hid, hidt, wdn, wdnt):
        for ocg in range(2):
            for tbg in range(2):
                for hc in range(11):
                    for oi in range(4):
                        oc = ocg * 4 + oi
                        for ti in range(2):
                            tb = tbg * 2 + ti
                            pi = oi * 2 + ti
                            mm(PS(pi), wdn[:, hc, oc, :], hid[:, hc, tbs(tb)], hc == 0, hc == 10,
                               R=[wdnt[hc], hidt[hc][tb]], W=[PT[pi]])
                for oi in range(4):
                    oc = ocg * 4 + oi
                    for ti in range(2):
                        tb = tbg * 2 + ti
                        pi = oi * 2 + ti
                        kb.ins("dve", lambda: nc.vector.tensor_tensor(out=x[:, oc, tbs(tb)], in0=PS(pi), in1=x[:, oc, tbs(tb)], op=ALU.add),
                               R=[PT[pi], xt[oc][tb]], W=[xt[oc][tb]])

    def final_phase():
        for tb in range(NB):
            pi = next_ps()
            for c in range(8):
                s = c % 2
                kb.ins("act", lambda c=c, s=s: nc.scalar.activation(sq[s][:], x[:, c, tbs(tb)], AF.Square), R=[xt[c][tb]], W=[sqt[s]])
                mm(PS(pi), onesm[:], sq[s][:], c == 0, c == 7, R=[ct, sqt[s]], W=[PT[pi]])
            kb.ins("act", lambda: nc.scalar.activation(rstd[:], PS(pi), AF.Ln, bias=1e-6, scale=1.0), R=[PT[pi]], W=[rstdt])
            kb.ins("act", lambda: nc.scalar.activation(rstd[:], rstd[:], AF.Exp, scale=-0.5), R=[rstdt], W=[rstdt])
            for c in range(8):
                kb.ins("dve", lambda c=c: nc.vector.scalar_tensor_tensor(
                    out=x[:, c, tbs(tb)], in0=x[:, c, tbs(tb)], scalar=pv[:, 0, 16 + c:17 + c], in1=rstd[:],
                    op0=ALU.mult, op1=ALU.mult), R=[xt[c][tb], pvt, rstdt], W=[xt[c][tb]])

    def store_x():
        for c in range(8):
            kb.dma("sp", out_d.ap()[c * 128:(c + 1) * 128, :], x[:, c, :], R=xt[c])

    done = False
    for l in range(n_layers):
        rmsnorm_to_h(l, 0)
        attention_phase(l)
        if stop_after == "attn%d" % l:
            done = True
            break
        rwkv_phase(l)
        if stop_after == "mix%d" % l:
            done = True
            break
        ffn_phase(l, moe=(l % 2 == 1))
        if stop_after == "ffn%d" % l:
            done = True
            break
    if not done:
        final_phase()
    store_x()
    kb.finish()
    kb.close()
    return kb


def host_inputs(inp):
    f = lambda a: np.ascontiguousarray(a, dtype=np.float32)
    L = 2
    w_in = inp["w_in"]
    win = f(w_in.reshape(L, 8, 128, 26, 128).transpose(0, 3, 2, 1, 4).reshape(L, 26, 128, 1024))
    wv = f(win[:, 22:26].reshape(L, 4, 128, 1024).transpose(0, 2, 1, 3).reshape(L, 128, 4096))
    wout = f(inp["w_out"].reshape(L, 8, 128, 8, 128).transpose(0, 3, 2, 1, 4).reshape(L, 8, 128, 1024))
    loraw = f(np.concatenate([inp["rwkv_w2"], inp["rwkv_a2"]], axis=1))
    g2 = f(inp["rwkv_g2"])

    def experts(wg, wu, wd, ne):
        a = f(wg.reshape(ne, 8, 128, 11, 128).transpose(0, 3, 2, 1, 4).reshape(ne, 11, 128, 1024))
        b = f(wu.reshape(ne, 8, 128, 11, 128).transpose(0, 3, 2, 1, 4).reshape(ne, 11, 128, 1024))
        c = f(wd.reshape(ne, 11, 128, 1024))
        return a, b, c
    dg = inp["ffn_w_gate"][0].reshape(1024, 2, 1408).transpose(1, 0, 2)
    du = inp["ffn_w_up"][0].reshape(1024, 2, 1408).transpose(1, 0, 2)
    dd = inp["ffn_w_down"][0].reshape(2, 1408, 1024)
    wg0, wu0, wd0 = experts(dg, du, dd, 2)
    wg1, wu1, wd1 = experts(inp["moe_w_gate"][0], inp["moe_w_up"][0], inp["moe_w_down"][0], 8)
    router = f(inp["moe_router"][0].reshape(8, 128, 8).transpose(1, 0, 2).reshape(128, 64))

    pv = np.zeros((L, 128, NPV), np.float32)
    col = lambda v: v.reshape(-1, 128).T
    for l in range(L):
        pv[l, :, 0:8] = col(inp["norm_mix_g"][l])
        pv[l, :, 8:16] = col(inp["norm_ffn_g"][l])
        pv[l, :, 16:24] = col(inp["norm_final_g"])
        pv[l, :, 24:38] = col(inp["shift_mu"][l])
        pv[l, :, 52:56] = col(inp["rwkv_w0"][l])
        pv[l, :, 56:60] = col(inp["rwkv_a0"][l])
        pv[l, :, 60:64] = col(inp["rwkv_k_k"][l])
        pv[l, :, 64:68] = col(inp["rwkv_k_a"][l])
        pv[l, :, 68:72] = col(inp["rwkv_r_k"][l])
        pv[l, :, 72:76] = col(inp["rwkv_ln_w"][l])
        pv[l, :, 76:80] = col(inp["rwkv_ln_b"][l])
    attn_g_bc = f(np.broadcast_to(inp["attn_norm_g"][:, None, :], (L, 128, 512)))
    ki = np.arange(128)[:, None]
    qi = np.arange(128)[None, :]
    biasT = np.zeros((L, 8, 128, 5, 128), np.float32)
    for s in range(5):
        rel = 128 * (4 - s) + qi - ki
        idx = np.clip(rel, -128, 128) + 128
        valid = np.ones((128, 128), bool)
        if s == 4:
            valid = ~((ki >= 64) & (qi < 64))
        if s == 0:
            valid = ~((ki < 64) & (qi >= 64))
        for l in range(L):
            g = inp["attn_rel_bias"][l][idx]
            g = np.where(valid[:, :, None], g, np.float32(-1e30))
            biasT[l, :, :, s, :] = g.transpose(2, 0, 1)
    biasT = f(biasT.reshape(L, 8, 128, 640))
    ident = np.eye(128, dtype=np.float32)
    onesm = np.full((128, 128), 1.0 / 1024, np.float32)
    blk = np.kron(np.eye(2, dtype=np.float32), np.ones((64, 64), np.float32))
    s_ = np.arange(128)[:, None]; t_ = np.arange(128)[None, :]
    same = (s_ // 64) == (t_ // 64)
    tri_i = (same & (s_ <= t_)).astype(np.float32)
    tri_e = (same & (s_ < t_)).astype(np.float32)
    s6 = np.arange(64)[:, None]; t6 = np.arange(64)[None, :]
    strict = (s6 < t6).astype(np.float32); incl = (s6 <= t6).astype(np.float32)
    lower = (t6 < s6).astype(np.float32)
    mA = np.concatenate([strict, incl, strict, incl], axis=1)
    maskA = np.concatenate([mA, mA], axis=1)
    mB = np.concatenate([lower, np.ones((64, 192), np.float32)], axis=1)
    maskB = np.concatenate([mB, mB], axis=1)
    sel = np.zeros((8, 8, 128), np.float32)
    for e in range(8):
        sel[e, e, :] = 1.0
    shared = {"pv": pv, "attn_g_bc": attn_g_bc, "biasT": biasT, "win": win, "wv": wv, "wout": wout, "loraw": loraw, "g2": g2,
              "wg0": wg0, "wu0": wu0, "wd0": wd0, "wg1": wg1, "wu1": wu1, "wd1": wd1, "router": router,
              "c_ident": ident, "c_onesm": onesm, "c_blk": blk, "c_tri_i": tri_i, "c_tri_e": tri_e,
              "c_maskA": f(maskA), "c_maskB": f(maskB), "c_sel": f(sel.reshape(8, 1024))}
    return shared


_PROG = {}


def kernel(**inputs):
    inp = {k: np.asarray(v) for k, v in inputs.items()}
    shared = host_inputs(inp)
    if "kb" not in _PROG:
        _PROG["kb"] = build_program()
    kb = _PROG["kb"]
    x = inp["x"]
    in_maps = []
    for b in range(8):
        m = dict(shared)
        m["xT"] = np.ascontiguousarray(x[b].T)
        in_maps.append(m)
    res = run_bass_kernel_spmd(kb.nc, in_maps, core_ids=list(range(8)))
    out = np.stack([np.ascontiguousarray(res.results[b]["outT"].T) for b in range(8)], axis=0)
    return out.astype(np.float32)
```

```python
import numpy as np
from contextlib import ExitStack
import concourse.bass as bass
import concourse.mybir as mybir
from concourse.bass_utils import run_bass_kernel_spmd

F32 = mybir.dt.float32
BF16 = mybir.dt.bfloat16
AF = mybir.ActivationFunctionType
ALU = mybir.AluOpType
AX = mybir.AxisListType

T = 2048
NB = 4
NT = 16
SEM_LIMIT = 60000
NPV = 80
DECAY_C = 0.6065306597126334


class Trk:
    __slots__ = ("w", "r")

    def __init__(self):
        self.w = None
        self.r = {}


class KB:
    def __init__(self, n_dma_sems=12):
        self.nc = bass.Bass("TRN2", target_bir_lowering=False)
        self.es = ExitStack()
        nc = self.nc
        self.eh = {"pe": nc.tensor, "act": nc.scalar, "dve": nc.vector, "pool": nc.gpsimd, "sp": nc.sync}
        self.sem = {}
        self.cnt = {}
        self.cur = {}
        self.eng_of = {}
        self.gen = {}
        self.known = {e: {} for e in self.eh}
        for e in self.eh:
            self._newgen(e, e)
        self.dma_pools = {"sp": ["d%d" % j for j in range(6)], "act": ["d%d" % j for j in range(6)], "pool": ["g%d" % j for j in range(8)]}
        self.dma_names = self.dma_pools["sp"] + self.dma_pools["pool"]
        for d in self.dma_names:
            self._newgen(d, None)
        self.dma_rr = {"sp": 0, "act": 0, "pool": 0}
        self.nwaits = 0
        self.nins = 0
        self.total_ins = {e: 0 for e in self.eh}

    def _newgen(self, name, eng):
        g = self.gen.get(name, -1) + 1
        self.gen[name] = g
        key = "%s_%d" % (name, g)
        self.sem[key] = self.es.enter_context(self.nc.semaphore("s_" + key))
        self.cnt[key] = 0
        self.cur[name] = key
        self.eng_of[key] = eng
        return key

    def sbuf(self, name, shape, dt, stack=None):
        self.uid = getattr(self, "uid", 0) + 1
        return (stack or self.es).enter_context(self.nc.sbuf_tensor("%s_%d" % (name, self.uid), list(shape), dt))

    def psum(self, name, shape, dt=F32):
        return self.es.enter_context(self.nc.psum_tensor(name, list(shape), dt))

    def _wait(self, eng, key, val):
        if val <= 0:
            return
        kn = self.known[eng]
        if kn.get(key, 0) >= val:
            return
        self.eh[eng].wait_ge(self.sem[key], val)
        kn[key] = val
        self.nwaits += 1

    def _deps(self, eng, R, W):
        deps = {}
        cur = self.cur[eng]
        for t in R:
            if t.w is not None:
                k, v = t.w
                if k == cur:
                    if eng != "pe":
                        self._wait(eng, k, v)
                elif self.eng_of[k] != eng:
                    if deps.get(k, 0) < v:
                        deps[k] = v
        for t in W:
            if t.w is not None:
                k, v = t.w
                if self.eng_of[k] != eng and deps.get(k, 0) < v:
                    deps[k] = v
            for k, v in t.r.items():
                if self.eng_of[k] != eng and deps.get(k, 0) < v:
                    deps[k] = v
        for k, v in deps.items():
            self._wait(eng, k, v)

    def _mark(self, key, c, R, W):
        for t in W:
            t.w = (key, c)
            t.r = {}
        for t in R:
            if t.r.get(key, 0) < c:
                t.r[key] = c

    def ins(self, eng, fn, R=(), W=()):
        cur = self.cur[eng]
        if self.cnt[cur] >= SEM_LIMIT:
            self.eh[eng].wait_ge(self.sem[cur], self.cnt[cur])
            cur = self._newgen(eng, eng)
        self._deps(eng, R, W)
        inst = fn()
        inst.then_inc(self.sem[cur], 1)
        self.cnt[cur] += 1
        self._mark(cur, self.cnt[cur], R, W)
        self.nins += 1
        self.total_ins[eng] += 1
        return inst

    def dma(self, q, out, in_, R=(), W=(), **kw):
        pool_ = self.dma_pools[q]
        name = pool_[self.dma_rr[q] % len(pool_)]
        self.dma_rr[q] += 1
        key = self.cur[name]
        self._wait(q, key, self.cnt[key])
        if self.cnt[key] >= SEM_LIMIT:
            key = self._newgen(name, None)
        self._deps(q, R, W)
        for t in list(R) + list(W):
            if t.w is not None and self.eng_of[t.w[0]] == q:
                self.eh[q].wait_ge(self.sem[t.w[0]], t.w[1])
        for t in W:
            for k, v in t.r.items():
                if self.eng_of[k] == q:
                    self.eh[q].wait_ge(self.sem[k], v)
        inst = self.eh[q].dma_start(out=out, in_=in_, **kw)
        inst.then_inc(self.sem[key], 16)
        self.cnt[key] += 16
        self._mark(key, self.cnt[key], R, W)
        self.nins += 1
        return inst

    def barrier(self):
        keys = [self.cur[n] for n in self.dma_names] + [self.cur[e] for e in self.eh]
        for e in self.eh:
            for k in keys:
                if self.eng_of[k] != e:
                    self._wait(e, k, self.cnt[k])

    def finish(self, eng="sp"):
        for name in self.dma_names:
            key = self.cur[name]
            self._wait(eng, key, self.cnt[key])
        for e in self.eh:
            if e != eng:
                key = self.cur[e]
                self._wait(eng, key, self.cnt[key])

    def close(self):
        self.es.close()


def build_program(stop_after="final", n_layers=2, dump=None, dbg=None):
    kb = KB()
    nc = kb.nc
    D = {}

    def din(name, shape):
        D[name] = nc.dram_tensor(name, list(shape), F32, kind="ExternalInput")
        return D[name]

    xT_d = din("xT", [1024, T])
    pv_d = din("pv", [2, 128, NPV])
    agbc_d = din("attn_g_bc", [2, 128, 512])
    biasT_d = din("biasT", [2, 8, 128, 640])
    win_d = din("win", [2, 26, 128, 1024])
    wv_d = din("wv", [2, 128, 4096])
    wout_d = din("wout", [2, 8, 128, 1024])
    loraw_d = din("loraw", [2, 128, 512])
    g2_d = din("g2", [2, 128, 512])
    ew_d = []
    for l, ne in enumerate((2, 8)):
        ew_d.append((din("wg%d" % l, [ne, 11, 128, 1024]), din("wu%d" % l, [ne, 11, 128, 1024]),
                     din("wd%d" % l, [ne, 11, 128, 1024])))
    router_d = din("router", [128, 64])
    c_ident_d = din("c_ident", [128, 128])
    c_onesm_d = din("c_onesm", [128, 128])
    c_blk_d = din("c_blk", [128, 128])
    c_tri_i_d = din("c_tri_i", [128, 128])
    c_tri_e_d = din("c_tri_e", [128, 128])
    c_maskA_d = din("c_maskA", [64, 512])
    c_maskB_d = din("c_maskB", [64, 512])
    c_sel_d = din("c_sel", [8, 1024])
    out_d = nc.dram_tensor("outT", [1024, T], F32, kind="ExternalOutput")

    sb = kb.sbuf
    x = sb("x", [128, 8, T], F32)
    xt = [[Trk() for _ in range(NB)] for _ in range(8)]
    h = sb("h", [128, 8, T], BF16)
    ht = [Trk() for _ in range(NB)]
    pv = sb("pvs", [128, 2, NPV], F32); pvt = Trk()
    pvd = sb("pvd", [128, 2, 24], F32); pvdt = Trk()
    ident_f = sb("ident_f", [128, 128], F32)
    ident_b = sb("ident_b", [128, 128], BF16)
    onesm = sb("onesm", [128, 128], BF16)
    blk = sb("blk", [128, 128], BF16)
    tri_i = sb("tri_i", [128, 128], F32)
    tri_e = sb("tri_e", [128, 128], F32)
    maskA = sb("maskA", [64, 512], BF16)
    maskB = sb("maskB", [64, 512], BF16)
    sel = sb("sel", [8, 1024], BF16)
    ct = Trk()
    wbuf = sb("wbuf", [128, 4, 8, 128], BF16)
    wbt = [Trk() for _ in range(4)]
    wb_rr = [0]
    sq = [sb("sq%d" % i, [128, 512], BF16) for i in range(2)]
    sqt = [Trk() for _ in range(2)]
    rstd = sb("rstd", [128, 512], F32); rstdt = Trk()

    PP = [kb.psum("pp%d" % i, [128, 1024]) for i in range(4)]
    PT = [Trk() for _ in range(8)]

    def PS(i):
        return PP[i // 2][:, (i % 2) * 512:(i % 2) * 512 + 512]

    ps_rr = [0]

    def next_ps(lo=0, hi=8):
        i = lo + (ps_rr[0] % (hi - lo))
        ps_rr[0] += 1
        return i

    for tb in range(NB):
        for c in range(8):
            kb.dma("sp", x[:, c, tb * 512:(tb + 1) * 512], xT_d.ap()[c * 128:(c + 1) * 128, tb * 512:(tb + 1) * 512], W=[xt[c][tb]])
    kb.dma("sp", pv[:, 0, :], pv_d.ap()[0], W=[pvt])
    kb.dma("sp", pv[:, 1, :], pv_d.ap()[1], W=[pvt])
    kb.dma("sp", ident_f[:], c_ident_d.ap(), W=[ct])
    kb.dma("sp", tri_i[:], c_tri_i_d.ap(), W=[ct])
    kb.dma("sp", tri_e[:], c_tri_e_d.ap(), W=[ct])
    kb.dma("pool", sel[:], c_sel_d.ap(), W=[ct])
    kb.dma("pool", ident_b[:], c_ident_d.ap(), W=[ct])
    kb.dma("pool", onesm[:], c_onesm_d.ap(), W=[ct])
    kb.dma("pool", blk[:], c_blk_d.ap(), W=[ct])
    kb.dma("pool", maskA[:], c_maskA_d.ap(), W=[ct])
    kb.dma("pool", maskB[:], c_maskB_d.ap(), W=[ct])
    for l in range(2):
        kb.ins("dve", lambda l=l: nc.vector.tensor_scalar(pvd[:, l, 0:14], pv[:, l, 24:38], -1.0, 1.0, op0=ALU.mult, op1=ALU.add),
               R=[pvt], W=[pvdt])
        kb.ins("dve", lambda l=l: nc.vector.tensor_scalar(pvd[:, l, 14:18], pv[:, l, 64:68], -1.0, 1.0, op0=ALU.mult, op1=ALU.add),
               R=[pvt], W=[pvdt])

    def load_w(src_ap, slot=None, shape4=None):
        if slot is None:
            slot = wb_rr[0] % 4
            wb_rr[0] += 1
        kb.dma("pool", wbuf[:, slot, :, :].rearrange("p k j -> p (k j)"), src_ap, W=[wbt[slot]])
        return slot

    evac_rr = [0]

    def evac_copy(out_ap, in_ap, R, W, eng=None):
        if eng is None:
            eng = "act" if (evac_rr[0] % 2 == 0) else "dve"
            evac_rr[0] += 1
        if eng == "act":
            kb.ins("act", lambda: nc.scalar.copy(out_ap, in_ap), R=R, W=W)
        else:
            kb.ins("dve", lambda: nc.vector.tensor_copy(out_ap, in_ap), R=R, W=W)

    def mm(out_ap, lhsT, rhs, start, stop, R, W):
        kb.ins("pe", lambda: nc.tensor.matmul(out_ap, lhsT, rhs, start=start, stop=stop), R=R, W=W)

    def tbs(tb):
        return slice(tb * 512, (tb + 1) * 512)

    def rmsnorm_to_h(l, gcol, after_tb=None):
        for tb in range(NB):
            pi = next_ps(0, 7)
            for c in range(8):
                s = c % 2
                kb.ins("act", lambda c=c, s=s: nc.scalar.activation(sq[s][:], x[:, c, tbs(tb)], AF.Square),
                       R=[xt[c][tb]], W=[sqt[s]])
                mm(PS(pi), onesm[:], sq[s][:], c == 0, c == 7, R=[ct, sqt[s]], W=[PT[pi]])
            kb.ins("act", lambda: nc.scalar.activation(rstd[:], PS(pi), AF.Ln, bias=1e-6, scale=1.0), R=[PT[pi]], W=[rstdt])
            kb.ins("act", lambda: nc.scalar.activation(rstd[:], rstd[:], AF.Exp, scale=-0.5), R=[rstdt], W=[rstdt])
            for c in range(8):
                kb.ins("dve", lambda c=c: nc.vector.scalar_tensor_tensor(
                    out=h[:, c, tbs(tb)], in0=x[:, c, tbs(tb)], scalar=pv[:, l, gcol + c:gcol + c + 1], in1=rstd[:],
                    op0=ALU.mult, op1=ALU.mult), R=[xt[c][tb], pvt, rstdt], W=[ht[tb]])
            if after_tb is not None:
                after_tb(tb)

    def attention_phase(l):
        st = ExitStack()
        qk = sb("qk", [128, 8, T], BF16, st)
        qkt = [[Trk() for _ in range(NT)] for _ in range(8)]
        Vp = sb("Vp", [128, NT, 8, 65], BF16, st); vpt = [Trk() for _ in range(NT)]
        Mh = sb("Mh", [128, 8, 640], BF16, st); mht = Trk()
        bst = sb("bst", [128, 640], F32, st); bstt = Trk()
        E = [sb("E%d" % i, [128, 640], BF16, st) for i in range(2)]; et = [Trk() for _ in range(2)]
        o = sb("o_att", [128, 8, 64], F32, st); ot = Trk()
        yo = sb("yo_att", [128, 512], F32, st); yot = Trk()
        junk = sb("junk_att", [128, 512], BF16, st); junkt = Trk()
        gbc = sb("gbc", [128, 512], F32, st); gbct = Trk()
        rec = sb("rec", [128, 8], F32, st); rect = Trk()
        ssq = sb("ssq", [128, 2], F32, st); ssqt = Trk()

        kb.dma("sp", gbc[:], agbc_d.ap()[l], W=[gbct])
        kb.ins("pool", lambda: nc.gpsimd.memset(Vp[:, :, :, 64:65], 1.0), W=vpt)
        for hh in range(8):
            kb.dma("sp", bst[:], biasT_d.ap()[l, hh], W=[bstt])
            kb.ins("act", lambda hh=hh: nc.scalar.activation(Mh[:, hh, :], bst[:], AF.Exp), R=[bstt], W=[mht])
        for ci in range(8):
            slot = load_w(win_d.ap()[l, 14 + ci])
            for tb in range(NB):
                pi = next_ps()
                for k in range(8):
                    mm(PS(pi), wbuf[:, slot, k, :], h[:, k, tbs(tb)], k == 0, k == 7, R=[wbt[slot], ht[tb]], W=[PT[pi]])
                evac_copy(qk[:, ci, tbs(tb)], PS(pi), R=[PT[pi]], W=qkt[ci][tb * 4:tb * 4 + 4])
        kb.dma("pool", wbuf[:].rearrange("p j k c -> p (j k c)"), wv_d.ap()[l], W=wbt)
        for tt in range(NT):
            pi = next_ps()
            for k in range(8):
                mm(PS(pi), h[:, k, tt * 128:(tt + 1) * 128], wbuf[:, :, k, :], k == 0, k == 7, R=wbt + [ht[tt // 4]], W=[PT[pi]])
            evac_copy(Vp[:, tt, :, 0:64], PS(pi).rearrange("p (h d) -> p h d", h=8), R=[PT[pi]], W=[vpt[tt]])
        items = [(m, hh) for m in range(NT) for hh in range(8)]

        def jlist(m):
            js = [j for j in range(m - 4, m + 1) if j >= 0]
            return js, 5 - len(js)

        def stageA(i):
            m, hh = items[i]
            js, s0 = jlist(m)
            hp, r0 = hh // 2, (hh % 2) * 64
            sp_ = i % 2
            Sps = PP[sp_]
            St = [PT[2 * sp_], PT[2 * sp_ + 1]]
            for idx, j in enumerate(js):
                sl = s0 + idx
                mm(Sps[:, sl * 128:(sl + 1) * 128], qk[r0:r0 + 64, 4 + hp, j * 128:(j + 1) * 128],
                   qk[r0:r0 + 64, hp, m * 128:(m + 1) * 128], True, True,
                   R=[qkt[4 + hp][j], qkt[hp][m]], W=St)
            eb = i % 2
            kb.ins("act", lambda: nc.scalar.activation(E[eb][:, s0 * 128:640], Sps[:, s0 * 128:640], AF.Exp, scale=0.125),
                   R=St, W=[et[eb]])
            kb.ins("dve", lambda: nc.vector.tensor_tensor(out=E[eb][:, s0 * 128:640], in0=E[eb][:, s0 * 128:640],
                                                          in1=Mh[:, hh, s0 * 128:640], op=ALU.mult),
                   R=[et[eb], mht], W=[et[eb]])

        def stageB(i):
            m, hh = items[i]
            js, s0 = jlist(m)
            eb = i % 2
            og = 4 + hh // 4
            for idx, j in enumerate(js):
                sl = s0 + idx
                mm(PS(og)[:, (hh % 4) * 65:(hh % 4) * 65 + 65], E[eb][:, sl * 128:(sl + 1) * 128], Vp[:, j, hh, :],
                   idx == 0, idx == len(js) - 1, R=[et[eb], vpt[j]], W=[PT[og]])

        def epilogue(m):
            for g in range(2):
                Og = PS(4 + g)[:, 0:260].rearrange("p (h d) -> p h d", h=4)
                kb.ins("dve", lambda: nc.vector.reciprocal(rec[:, g * 4:g * 4 + 4], Og[:, :, 64]), R=[PT[4 + g]], W=[rect])
                kb.ins("dve", lambda: nc.vector.tensor_tensor(out=o[:, g * 4:g * 4 + 4, :], in0=Og[:, :, 0:64],
                                                              in1=rec[:, g * 4:g * 4 + 4].unsqueeze(2).to_broadcast([128, 4, 64]),
                                                              op=ALU.mult), R=[PT[4 + g], rect], W=[ot])
            of = o[:].rearrange("p h d -> p (h d)")
            kb.ins("pool", lambda: nc.gpsimd.memset(ssq[:, 0:1], 0.0), W=[ssqt])
            kb.ins("act", lambda: nc.scalar.activation(junk[:], of, AF.Square, accum_out=ssq[:, 0:1]), R=[ot, ssqt], W=[junkt, ssqt])
            kb.ins("act", lambda: nc.scalar.activation(ssq[:, 1:2], ssq[:, 0:1], AF.Ln, bias=1e-6, scale=1.0 / 512), R=[ssqt], W=[ssqt])
            kb.ins("act", lambda: nc.scalar.activation(ssq[:, 1:2], ssq[:, 1:2], AF.Exp, scale=-0.5), R=[ssqt], W=[ssqt])
            kb.ins("dve", lambda: nc.vector.scalar_tensor_tensor(out=yo[:], in0=of, scalar=ssq[:, 1:2], in1=gbc[:],
                                                                 op0=ALU.mult, op1=ALU.mult), R=[ot, ssqt, gbct], W=[yot])
            pi = 6 + (m % 2)
            for c in range(4):
                mm(PS(pi)[:, c * 128:(c + 1) * 128], yo[:, c * 128:(c + 1) * 128], ident_f[:], True, True, R=[yot, ct], W=[PT[pi]])
            evac_copy(qk[:, 0:4, m * 128:(m + 1) * 128], PS(pi).rearrange("p (c t) -> p c t", c=4), R=[PT[pi]],
                      W=[qkt[c][m] for c in range(4)])

        stageA(0)
        for i in range(len(items)):
            if i + 1 < len(items):
                stageA(i + 1)
            stageB(i)
            if items[i][1] == 7:
                epilogue(items[i][0])
        for oc in range(8):
            slot = load_w(wout_d.ap()[l, oc])
            for tb in range(NB):
                pi = next_ps(0, 4)
                for k in range(4):
                    mm(PS(pi), wbuf[:, slot, 4 + k, :], qk[:, k, tbs(tb)], k == 0, k == 3,
                       R=[wbt[slot]] + qkt[k][tb * 4:tb * 4 + 4], W=[PT[pi]])
                kb.ins("dve", lambda: nc.vector.tensor_tensor(out=x[:, oc, tbs(tb)], in0=PS(pi), in1=x[:, oc, tbs(tb)], op=ALU.add),
                       R=[PT[pi], xt[oc][tb]], W=[xt[oc][tb]])
        kb.barrier()
        st.close()

    def rwkv_phase(l):
        BS = 256
        NBLK = T // BS
        NCH = BS // 64
        NQ = BS // 128
        NSTREAM = 2
        st = ExitStack()
        lora1 = sb("lora1", [128, T], BF16, st); l1t = [Trk() for _ in range(NB)]
        lora2 = sb("lora2", [128, T], BF16, st); l2t = [Trk() for _ in range(NB)]
        loraw = sb("loraw_s", [128, 512], BF16, st); lwt = Trk()
        g2s = sb("g2_s", [128, 512], BF16, st); g2t = Trk()
        wx = sb("r_wx", [128, 2, 8, 128], BF16, st); wxt = [Trk(), Trk()]
        GNB = sb("r_GNB", [64, 1], F32, st); GNBt = Trk()
        kb.ins("pool", lambda: nc.gpsimd.memset(GNB[:], 64e-5), W=[GNBt])
        kb.dma("pool", loraw[:], loraw_d.ap()[l], W=[lwt])
        kb.dma("pool", g2s[:], g2_d.ap()[l], W=[g2t])

        ps_free = [0, 1, 2, 3, 4, 5]

        def aps():
            return ps_free.pop(0)

        def fps(i):
            ps_free.append(i)

        def bsl(b):
            return slice(b * BS, (b + 1) * BS)

        shl = sb("shbuf_l", [128, 516], F32, st); shlt = Trk()
        carl = sb("carry_l", [128, 2], F32, st); carlt = Trk()
        tl = rstd; tlt = rstdt
        for ci, cc in enumerate((12, 13)):
            slot = load_w(win_d.ap()[l, cc])
            for tb in range(NB):
                pi = aps()
                for k in range(8):
                    mm(PS(pi), wbuf[:, slot, k, :], h[:, k, tbs(tb)], k == 0, k == 7, R=[wbt[slot], ht[tb]], W=[PT[pi]])
                mu = pv[:, l, 24 + cc:25 + cc]
                omm = pvd[:, l, cc:cc + 1]
                if tb == 0:
                    kb.ins("pool", lambda: nc.gpsimd.memset(shl[:, 0:1], 0.0), W=[shlt])
                else:
                    kb.ins("act", lambda: nc.scalar.copy(shl[:, 0:1], carl[:, ci:ci + 1]), R=[carlt], W=[shlt])
                kb.ins("act", lambda: nc.scalar.activation(shl[:, 1:513], PS(pi), AF.Identity, scale=mu), R=[PT[pi], pvt, shlt], W=[shlt])
                kb.ins("act", lambda: nc.scalar.copy(carl[:, ci:ci + 1], shl[:, 512:513]), R=[shlt], W=[carlt])
                kb.ins("dve", lambda: nc.vector.scalar_tensor_tensor(out=tl[:], in0=PS(pi), scalar=omm, in1=shl[:, 0:512],
                                                                     op0=ALU.mult, op1=ALU.add), R=[PT[pi], pvdt, shlt], W=[tlt])
                fps(pi)
                if cc == 12:
                    kb.ins("act", lambda: nc.scalar.activation(lora1[0:64, tbs(tb)], tl[0:64, :], AF.Tanh), R=[tlt], W=[l1t[tb]])
                    kb.ins("act", lambda: nc.scalar.copy(lora1[64:128, tbs(tb)], tl[64:128, :]), R=[tlt], W=[l1t[tb]])
                else:
                    kb.ins("act", lambda: nc.scalar.activation(lora2[:, tbs(tb)], tl[:], AF.Sigmoid), R=[tlt], W=[l2t[tb]])

        def stream(s, hps):
            def f32t(name):
                return sb(name, [128, BS], F32, st), Trk()

            def b16t(name):
                return sb(name, [128, BS], BF16, st), Trk()
            RKV = sb("r_RKV", [128, 3, BS], F32, st)
            Rt, Rtt = RKV[:, 0, :], Trk(); Kt, Ktt = RKV[:, 1, :], Trk(); Vt, Vtt = RKV[:, 2, :], Trk()
            SA = sb("r_SA", [128, 2, BS], F32, st)
            SG, SGt = SA[:, 0, :], Trk(); Aa, Aat = SA[:, 1, :], Trk(); Gt, Gtt = b16t("r_G")
            Yraw = RKV[0:64, 0:2, :].rearrange("p a (b v) -> p (a b) v", v=64)
            Ycb = SA[0:64, :, :].rearrange("p a (b v) -> p (a b) v", v=64)
            Ynb = sb("r_Ynb", [64, 2 * NCH, 64], BF16, st); Ynbt = Trk()
            kkn, kknt = f32t("r_kkn"); t1, t1t = f32t("r_t1"); t2, t2t = f32t("r_t2")
            eG, eGt = SG, SGt; eGx, eGxt = f32t("r_eGx"); eGn, eGnt = f32t("r_eGn")
            bv, bvt = f32t("r_bv")
            kk2, kk2t = b16t("r_kk2")
            ART = sb("r_ART", [128, NCH, 128], BF16, st); ARTt = Trk()
            bT, bTt = b16t("r_bT"); kT, kTt = b16t("r_kT"); vT, vTt = b16t("r_vT")
            sgT = eGn[:].rearrange("p (q f) -> p q f", q=NQ); sgTt = eGnt
            CA = sb("r_CA", [64, NCH, 512], BF16, st); CAt = Trk()
            CB = sb("r_CB", [64, NCH, 512], BF16, st); CBt = Trk()
            NI = 2 * NCH
            Xb = [sb("r_X%d" % i, [64, NI, 64], BF16, st) for i in range(2)]; Xbt = [Trk(), Trk()]
            Nb = [sb("r_N%d" % i, [64, NI, 64], BF16, st) for i in range(2)]; Nbt = [Trk(), Trk()]
            TTf = sb("r_TT", [64, NI, 64], BF16, st); TTft = Trk()
            ST = sb("r_ST", [64, 2, 64], F32, st); STt = Trk()
            STb = sb("r_STb", [64, 2, 64], BF16, st); STbt = Trk()
            WC = sb("r_WC", [64, 2, NCH], F32, st); WCt = Trk()
            WCs = sb("r_WCs", [128, NCH], F32, st); WCst = Trk()
            P1 = sb("r_P1", [64, 2, 64], BF16, st); P1t = Trk()
            U = sb("r_U", [64, 2, 64], BF16, st); Ut = Trk()
            Yc = sb("r_Yc", [64, 2, 64], F32, st); Yct = Trk()
            Ysq = sb("r_Ysq", [64, 2, 64], F32, st); Ysqt = Trk()
            Yn = sb("r_Yn", [64, 2, 64], BF16, st); Ynt = Trk()
            stat = sb("r_stat", [64, 8 * NCH], F32, st); statt = Trk()
            sh = sb("shbuf", [128, BS + 4], F32, st); sht = Trk()
            carry = sb("carry", [128, 4], F32, st); carryt = Trk()
            yrs = sb("yrs", [128, T], BF16, st); yrst = [Trk() for _ in range(NBLK)]
            if s == 0:
                wsl = [(wbuf[:, i, :, :], wbt[i]) for i in range(3)]
            else:
                wsl = [(wbuf[:, 3, :, :], wbt[3]), (wx[:, 0, :, :], wxt[0]), (wx[:, 1, :, :], wxt[1])]
            pyo = 6 + s

            def shifted_proj(wi, cc, b, out_ap, out_trk, ci):
                wap, wtr = wsl[wi]
                pi = aps()
                for k in range(8):
                    mm(PS(pi)[:, 0:BS], wap[:, k, :], h[:, k, bsl(b)], k == 0, k == 7, R=[wtr, ht[(b * BS) // 512]], W=[PT[pi]])
                mu = pv[:, l, 24 + cc:25 + cc]
                omm = pvd[:, l, cc:cc + 1]
                if b == 0:
                    kb.ins("pool", lambda: nc.gpsimd.memset(sh[:, 0:1], 0.0), W=[sht])
                else:
                    kb.ins("act", lambda: nc.scalar.copy(sh[:, 0:1], carry[:, ci:ci + 1]), R=[carryt], W=[sht])
                kb.ins("act", lambda: nc.scalar.activation(sh[:, 1:BS + 1], PS(pi)[:, 0:BS], AF.Identity, scale=mu), R=[PT[pi], pvt, sht], W=[sht])
                kb.ins("act", lambda: nc.scalar.copy(carry[:, ci:ci + 1], sh[:, BS:BS + 1]), R=[sht], W=[carryt])
                kb.ins("dve", lambda: nc.vector.scalar_tensor_tensor(out=out_ap, in0=PS(pi)[:, 0:BS], scalar=omm, in1=sh[:, 0:BS],
                                                                     op0=ALU.mult, op1=ALU.add), R=[PT[pi], pvdt, sht], W=[out_trk])
                fps(pi)

            for hp in hps:
                for wi, cc in enumerate((hp, 4 + hp, 8 + hp)):
                    kb.dma("pool", wsl[wi][0].rearrange("p k j -> p (k j)"), win_d.ap()[l, cc], W=[wsl[wi][1]])
                kb.ins("pool", lambda: nc.gpsimd.memset(ST[:], 0.0), W=[STt])
                kb.ins("pool", lambda: nc.gpsimd.memset(STb[:], 0.0), W=[STbt])
                w0 = pv[:, l, 52 + hp:53 + hp]; a0 = pv[:, l, 56 + hp:57 + hp]; k_k = pv[:, l, 60 + hp:61 + hp]
                k_a = pv[:, l, 64 + hp:65 + hp]; r_k = pv[:, l, 68 + hp:69 + hp]
                ln_w = pv[:, l, 72 + hp:73 + hp]; ln_b = pv[:, l, 76 + hp:77 + hp]
                omka = pvd[:, l, 14 + hp:15 + hp]
                cs = slice(hp * 128, (hp + 1) * 128)
                yield
                for b in range(NBLK):
                    tb = (b * BS) // 512
                    shifted_proj(0, hp, b, Rt[:], Rtt, 0)
                    yield
                    shifted_proj(1, 4 + hp, b, Kt[:], Ktt, 1)
                    yield
                    shifted_proj(2, 8 + hp, b, Vt[:], Vtt, 2)
                    yield
                    pi = aps()
                    mm(PS(pi)[:, 0:BS], loraw[0:64, cs], lora1[0:64, bsl(b)], True, True, R=[lwt, l1t[tb]], W=[PT[pi]])
                    kb.ins("act", lambda: nc.scalar.activation(SG[:], PS(pi)[:, 0:BS], AF.Sigmoid, bias=w0, scale=1.0), R=[PT[pi], pvt], W=[SGt])
                    fps(pi)
                    pi = aps()
                    mm(PS(pi)[:, 0:BS], loraw[64:128, cs], lora1[64:128, bsl(b)], True, True, R=[lwt, l1t[tb]], W=[PT[pi]])
                    kb.ins("act", lambda: nc.scalar.activation(Aa[:], PS(pi)[:, 0:BS], AF.Sigmoid, bias=a0, scale=1.0), R=[PT[pi], pvt], W=[Aat])
                    fps(pi)
                    pi = aps()
                    mm(PS(pi)[:, 0:BS], g2s[:, cs], lora2[:, bsl(b)], True, True, R=[g2t, l2t[tb]], W=[PT[pi]])
                    evac_copy(Gt[:], PS(pi)[:, 0:BS], R=[PT[pi]], W=[Gtt], eng="act")
                    fps(pi)
                    yield
                    kb.ins("act", lambda: nc.scalar.activation(kk2[:], Kt[:], AF.Square, scale=k_k), R=[Ktt, pvt], W=[kk2t])
                    pi = aps()
                    mm(PS(pi)[:, 0:BS], blk[:], kk2[:], True, True, R=[ct, kk2t], W=[PT[pi]])
                    kb.ins("act", lambda: nc.scalar.activation(t1[:], PS(pi)[:, 0:BS], AF.Ln, bias=1e-24, scale=1.0), R=[PT[pi]], W=[t1t])
                    fps(pi)
                    kb.ins("act", lambda: nc.scalar.activation(t1[:], t1[:], AF.Exp, scale=-0.5), R=[t1t], W=[t1t])
                    kb.ins("dve", lambda: nc.vector.scalar_tensor_tensor(out=kkn[:], in0=Kt[:], scalar=k_k, in1=t1[:], op0=ALU.mult, op1=ALU.mult),
                           R=[Ktt, pvt, t1t], W=[kknt])
                    kb.ins("act", lambda: nc.scalar.activation(t2[:], Aa[:], AF.Identity, bias=omka, scale=k_a), R=[Aat, pvt, pvdt], W=[t2t])
                    kb.ins("pool", lambda: nc.gpsimd.tensor_tensor(out=t2[:], in0=t2[:], in1=Kt[:], op=ALU.mult), R=[t2t, Ktt], W=[t2t])
                    kb.ins("dve", lambda: nc.vector.scalar_tensor_tensor(out=kk2[:], in0=Rt[:], scalar=r_k, in1=t2[:], op0=ALU.mult, op1=ALU.mult),
                           R=[Rtt, pvt, t2t], W=[kk2t])
                    pi = aps()
                    mm(PS(pi)[:, 0:BS], blk[:], kk2[:], True, True, R=[ct, kk2t], W=[PT[pi]])
                    kb.ins("dve", lambda: nc.vector.tensor_tensor(out=bv[:], in0=PS(pi)[:, 0:BS], in1=Vt[:], op=ALU.mult), R=[PT[pi], Vtt], W=[bvt])
                    fps(pi)
                    yield
                    pi = aps()
                    for q4 in range(NQ):
                        mm(PS(pi)[:, q4 * 128:(q4 + 1) * 128], SG[:, q4 * 128:(q4 + 1) * 128], ident_f[:], True, True, R=[SGt, ct], W=[PT[pi]])
                    evac_copy(sgT, PS(pi)[:, 0:BS].rearrange("p (q f) -> p q f", q=NQ), R=[PT[pi]], W=[sgTt], eng="act")
                    fps(pi)
                    pg = aps(); pgx = aps()
                    for q4 in range(NQ):
                        mm(PS(pg)[:, q4 * 128:(q4 + 1) * 128], sgT[:, q4, :], tri_i[:], True, True, R=[sgTt, ct], W=[PT[pg]])
                    for q4 in range(NQ):
                        mm(PS(pgx)[:, q4 * 128:(q4 + 1) * 128], sgT[:, q4, :], tri_e[:], True, True, R=[sgTt, ct], W=[PT[pgx]])
                    kb.ins("act", lambda: nc.scalar.activation(eG[:], PS(pg)[:, 0:BS], AF.Exp, scale=-DECAY_C), R=[PT[pg]], W=[eGt])
                    kb.ins("act", lambda: nc.scalar.activation(eGn[:], PS(pg)[:, 0:BS], AF.Exp, scale=DECAY_C), R=[PT[pg]], W=[eGnt])
                    kb.ins("act", lambda: nc.scalar.activation(eGx[:], PS(pgx)[:, 0:BS], AF.Exp, scale=-DECAY_C), R=[PT[pgx]], W=[eGxt])
                    fps(pg); fps(pgx)
                    yield
                    ARTv = ART[:]
                    kb.ins("dve", lambda: nc.vector.scalar_tensor_tensor(out=ARTv[:, :, 0:64], in0=kkn[:].rearrange("p (c t) -> p c t", c=NCH), scalar=-1.0,
                                                                         in1=eGx[:].rearrange("p (c t) -> p c t", c=NCH), op0=ALU.mult, op1=ALU.mult),
                           R=[kknt, eGxt], W=[ARTt])
                    kb.ins("pool", lambda: nc.gpsimd.tensor_tensor(out=ARTv[:, :, 64:128], in0=Rt[:].rearrange("p (c t) -> p c t", c=NCH),
                                                                   in1=eG[:].rearrange("p (c t) -> p c t", c=NCH), op=ALU.mult), R=[Rtt, eGt], W=[ARTt])
                    kb.ins("dve", lambda: nc.vector.tensor_tensor(out=Aa[:], in0=kkn[:], in1=Aa[:], op=ALU.mult), R=[kknt, Aat], W=[Aat])
                    kb.ins("dve", lambda: nc.vector.tensor_tensor(out=bT[:], in0=Aa[:], in1=eGn[:], op=ALU.mult), R=[Aat, eGnt], W=[bTt])
                    kb.ins("pool", lambda: nc.gpsimd.tensor_tensor(out=kT[:], in0=t2[:], in1=eGn[:], op=ALU.mult), R=[t2t, eGnt], W=[kTt])
                    kb.ins("act", lambda: nc.scalar.copy(vT[:], Vt[:]), R=[Vtt], W=[vTt])
                    yield
                    kkb = kkn[:].bitcast(BF16)
                    t2b = t2[:].bitcast(BF16)
                    exb = eGx[:].bitcast(BF16)
                    ART1 = kkb[0:64, :].rearrange("p (c t) -> p c t", c=NCH)
                    bT1 = t2b[0:64, 0:BS]; kT1 = t2b[0:64, BS:2 * BS]; vT1 = exb[0:64, 0:BS]
                    ARTf = ART[:].rearrange("p c t -> p (c t)")
                    for (src, srct, dst, dstt, n) in ((ARTf, ARTt, kkb[0:64, :], kknt, 2 * BS), (bT[:], bTt, bT1, t2t, BS),
                                                    (kT[:], kTt, kT1, t2t, BS), (vT[:], vTt, vT1, eGxt, BS)):
                        pi = aps()
                        mm(PS(pi)[0:64, 0:n], ident_b[64:128, 64:128], src[64:128, :], True, True, R=[ct, srct], W=[PT[pi]])
                        evac_copy(dst, PS(pi)[0:64, 0:n], R=[PT[pi]], W=[dstt])
                        fps(pi)
                    pi = aps()
                    eGl = eG[:].rearrange("p (c t) -> p c t", c=NCH)[:, :, 63]
                    kb.ins("dve", lambda: nc.vector.tensor_copy(WCs[:], eGl), R=[eGt], W=[WCst])
                    mm(PS(pi)[0:64, 0:NCH], ident_f[64:128, 64:128], WCs[64:128, :], True, True, R=[ct, WCst], W=[PT[pi]])
                    kb.ins("dve", lambda: nc.vector.tensor_copy(WC[:, 0, :], WCs[0:64, :]), R=[WCst], W=[WCt])
                    kb.ins("dve", lambda: nc.vector.tensor_copy(WC[:, 1, :], PS(pi)[0:64, 0:NCH]), R=[PT[pi]], W=[WCt])
                    fps(pi)
                    yield

                    def opnd(hh):
                        if hh == 0:
                            return ART[0:64], bT[0:64, :], kT[0:64, :], vT[0:64, :], [ARTt, bTt, kTt, vTt]
                        return ART1, bT1, kT1, vT1, [kknt, t2t, t2t, eGxt]
                    for c in range(NCH):
                        pa = aps(); pb = aps()
                        cs64 = slice(c * 64, c * 64 + 64)
                        for hh in range(2):
                            ARh, bh, kh, vh, trs = opnd(hh)
                            A_ = PS(pa)[0:64, hh * 256:(hh + 1) * 256]
                            B_ = PS(pb)[0:64, hh * 256:(hh + 1) * 256]
                            mm(A_[:, 0:128], bh[:, cs64], ARh[:, c, :], True, True, R=trs, W=[PT[pa]])
                            mm(A_[:, 128:256], kh[:, cs64], ARh[:, c, :], True, True, R=trs, W=[PT[pa]])
                            mm(B_[:, 0:64], ARh[:, c, 0:64], bh[:, cs64], True, True, R=trs, W=[PT[pb]])
                            mm(B_[:, 64:128], bh[:, cs64], ident_b[0:64, 0:64], True, True, R=trs + [ct], W=[PT[pb]])
                            mm(B_[:, 128:192], kh[:, cs64], ident_b[0:64, 0:64], True, True, R=trs + [ct], W=[PT[pb]])
                            mm(B_[:, 192:256], vh[:, cs64], ident_b[0:64, 0:64], True, True, R=trs + [ct], W=[PT[pb]])
                        kb.ins("dve", lambda: nc.vector.tensor_tensor(out=CA[:, c, :], in0=PS(pa)[0:64, :], in1=maskA[:], op=ALU.mult),
                               R=[PT[pa], ct], W=[CAt])
                        kb.ins("dve", lambda: nc.vector.tensor_tensor(out=CB[:, c, :], in0=PS(pb)[0:64, :], in1=maskB[:], op=ALU.mult),
                               R=[PT[pb], ct], W=[CBt])
                        fps(pa); fps(pb)
                        yield
                    CAv = CA[:].rearrange("p c (h f) -> p (c h) f", h=2)
                    CBv = CB[:].rearrange("p c (h f) -> p (c h) f", h=2)
                    kb.ins("pool", lambda: nc.gpsimd.tensor_copy(Xb[0][:], CAv[:, :, 0:64]), R=[CAt], W=[Xbt[0]])
                    kb.ins("pool", lambda: nc.gpsimd.tensor_copy(Nb[0][:], CBv[:, :, 0:64]), R=[CBt], W=[Nbt[0]])
                    kb.ins("dve", lambda: nc.vector.tensor_tensor(out=TTf[:], in0=CAv[:, :, 0:64],
                                                                  in1=ident_b[0:64, 0:64].unsqueeze(1).to_broadcast([64, NI, 64]), op=ALU.add),
                           R=[CAt, ct], W=[TTft])
                    cur = 0
                    for lev in range(5):
                        nx = 1 - cur
                        px = aps(); pn = aps()
                        if lev < 4:
                            for i in range(NI):
                                mm(PS(px)[0:64, i * 64:(i + 1) * 64], Nb[cur][:, i, :], Xb[cur][:, i, :], True, True,
                                   R=[Nbt[cur], Xbt[cur]], W=[PT[px]])
                        for i in range(NI):
                            mm(PS(pn)[0:64, i * 64:(i + 1) * 64], Xb[cur][:, i, :], Nb[cur][:, i, :], True, True,
                               R=[Nbt[cur], Xbt[cur]], W=[PT[pn]])
                        if lev < 4:
                            evac_copy(Xb[nx][:], PS(px)[0:64, 0:NI * 64].rearrange("p (i f) -> p i f", i=NI), R=[PT[px]], W=[Xbt[nx]], eng="act")
                        evac_copy(Nb[nx][:], PS(pn)[0:64, 0:NI * 64].rearrange("p (i f) -> p i f", i=NI), R=[PT[pn]], W=[Nbt[nx]], eng="dve")
                        fps(px); fps(pn)
                        yield
                        pt = aps()
                        for i in range(NI):
                            mm(PS(pt)[0:64, i * 64:(i + 1) * 64], Nb[nx][:, i, :], TTf[:, i, :], True, True,
                               R=[Nbt[nx], TTft], W=[PT[pt]])
                        kb.ins("dve", lambda: nc.vector.tensor_tensor(out=TTf[:], in0=PS(pt)[0:64, 0:NI * 64].rearrange("p (i f) -> p i f", i=NI),
                                                                      in1=TTf[:], op=ALU.add), R=[PT[pt], TTft], W=[TTft])
                        fps(pt)
                        cur = nx
                        yield
                    W1 = Xb[0]; W1t = Xbt[0]; atok = Xb[1]; atokt = Xbt[1]; Ub = Nb[0]; Ubt = Nbt[0]; Atok = Nb[1]; Atokt = Nbt[1]
                    PhiT = W1; PhiTt = W1t; RpT = atok; RpTt = atokt
                    psA = aps(); psB = aps()
                    for c in range(NCH):
                        for hh in range(2):
                            ARh, bh, kh, vh, trs = opnd(hh)
                            i = 2 * c + hh
                            mm(PS(psA)[0:64, i * 64:(i + 1) * 64], CA[:, c, hh * 256 + 128:hh * 256 + 192], CB[:, c, hh * 256 + 192:hh * 256 + 256], True, True,
                               R=[CAt, CBt], W=[PT[psA]])
                            mm(PS(psB)[0:64, i * 64:(i + 1) * 64], ARh[:, c, 0:64], ident_b[0:64, 0:64], True, True, R=trs + [ct], W=[PT[psB]])
                    evac_copy(W1[:], PS(psA)[0:64, 0:NI * 64].rearrange("p (i f) -> p i f", i=NI), R=[PT[psA]], W=[W1t], eng="act")
                    evac_copy(atok[:], PS(psB)[0:64, 0:NI * 64].rearrange("p (i f) -> p i f", i=NI), R=[PT[psB]], W=[atokt], eng="dve")
                    fps(psA); fps(psB)
                    yield
                    psA = aps(); psB = aps()
                    for i in range(NI):
                        mm(PS(psA)[0:64, i * 64:(i + 1) * 64], TTf[:, i, :], W1[:, i, :], True, True, R=[TTft, W1t], W=[PT[psA]])
                        mm(PS(psB)[0:64, i * 64:(i + 1) * 64], TTf[:, i, :], atok[:, i, :], True, True, R=[TTft, atokt], W=[PT[psB]])
                    evac_copy(Ub[:], PS(psA)[0:64, 0:NI * 64].rearrange("p (i f) -> p i f", i=NI), R=[PT[psA]], W=[Ubt], eng="act")
                    evac_copy(Atok[:], PS(psB)[0:64, 0:NI * 64].rearrange("p (i f) -> p i f", i=NI), R=[PT[psB]], W=[Atokt], eng="dve")
                    fps(psA); fps(psB)
                    yield
                    psA = aps(); psB = aps()
                    for c in range(NCH):
                        for hh in range(2):
                            i = 2 * c + hh
                            mm(PS(psA)[0:64, i * 64:(i + 1) * 64], Atok[:, i, :], CB[:, c, hh * 256 + 64:hh * 256 + 128], True, True,
                               R=[Atokt, CBt], W=[PT[psA]])
                            mm(PS(psB)[0:64, i * 64:(i + 1) * 64], Atok[:, i, :], CA[:, c, hh * 256 + 64:hh * 256 + 128], True, True,
                               R=[Atokt, CAt], W=[PT[psB]])
                    evac_copy(PhiT[:], PS(psA)[0:64, 0:NI * 64].rearrange("p (i f) -> p i f", i=NI), R=[PT[psA]], W=[PhiTt], eng="act")
                    for hh in range(2):
                        ARh, bh, kh, vh, trs = opnd(hh)
                        kb.ins("dve", lambda: nc.vector.tensor_tensor(
                            out=RpT[:].rearrange("p (c h) f -> p c h f", h=2)[:, :, hh, :],
                            in0=PS(psB)[0:64, 0:NI * 64].rearrange("p (c h f) -> p c h f", h=2, f=64)[:, :, hh, :],
                            in1=ARh[:, :, 64:128], op=ALU.add), R=[PT[psB]] + trs, W=[RpTt])
                    fps(psA); fps(psB)
                    yield
                    for c in range(NCH):
                        pp = aps()
                        Yps = PS(pp)[0:64, 0:128]
                        pst = PS(pp)[0:64, 128:256]
                        for hh in range(2):
                            i = 2 * c + hh
                            fo = slice(hh * 64, hh * 64 + 64)
                            mm(pst[:, fo], CB[:, c, hh * 256 + 64:hh * 256 + 128], Ub[:, i, :], True, False, R=[CBt, Ubt], W=[PT[pp]])
                            mm(pst[:, fo], CB[:, c, hh * 256 + 128:hh * 256 + 192], CB[:, c, hh * 256 + 192:hh * 256 + 256], False, False,
                               R=[CBt], W=[PT[pp]])
                            mm(pst[:, fo], PhiT[:, i, :], STb[:, hh, :], False, True, R=[PhiTt, STbt], W=[PT[pp]])
                        for hh in range(2):
                            i = 2 * c + hh
                            fo = slice(hh * 64, hh * 64 + 64)
                            mm(Yps[:, fo], CA[:, c, hh * 256 + 64:hh * 256 + 128], Ub[:, i, :], True, False, R=[CAt, Ubt], W=[PT[pp]])
                            mm(Yps[:, fo], CA[:, c, hh * 256 + 192:hh * 256 + 256], CB[:, c, hh * 256 + 192:hh * 256 + 256], False, False,
                               R=[CAt, CBt], W=[PT[pp]])
                            mm(Yps[:, fo], RpT[:, i, :], STb[:, hh, :], False, True, R=[RpTt, STbt], W=[PT[pp]])
                        STf = ST[:].rearrange("p h v -> p (h v)")
                        kb.ins("dve", lambda: nc.vector.tensor_tensor(out=STf, in0=pst, in1=STf, op=ALU.add), R=[PT[pp], STt], W=[STt])
                        kb.ins("dve", lambda: nc.vector.tensor_tensor(out=ST[:], in0=ST[:], in1=WC[:, :, c:c + 1].to_broadcast([64, 2, 64]), op=ALU.mult),
                               R=[STt, WCt], W=[STt])
                        kb.ins("act", lambda: nc.scalar.copy(STb[:], ST[:]), R=[STt], W=[STbt])
                        kb.ins("act", lambda: nc.scalar.copy(Yraw[:, 2 * c:2 * c + 2, :], Yps.rearrange("p (h v) -> p h v", h=2)),
                               R=[PT[pp]], W=[Rtt, Ktt])
                        fps(pp)
                        yield
                    NI2 = 2 * NCH
                    kb.ins("dve", lambda: nc.vector.tensor_reduce(out=stat[:, 0:NI2], in_=Yraw, axis=AX.X, op=ALU.add), R=[Rtt, Ktt], W=[statt])
                    kb.ins("dve", lambda: nc.vector.tensor_scalar(stat[:, NI2:2 * NI2], stat[:, 0:NI2], -1.0 / 64, None, op0=ALU.mult), R=[statt], W=[statt])
                    kb.ins("pool", lambda: nc.gpsimd.tensor_tensor(out=Ycb, in0=Yraw, in1=stat[:, NI2:2 * NI2].unsqueeze(2).to_broadcast([64, NI2, 64]), op=ALU.add),
                           R=[Rtt, Ktt, statt], W=[SGt, Aat])
                    yield
                    kb.ins("pool", lambda: nc.gpsimd.tensor_tensor(out=Yraw, in0=Ycb, in1=Ycb, op=ALU.mult), R=[SGt, Aat], W=[Rtt, Ktt])
                    kb.ins("dve", lambda: nc.vector.tensor_reduce(out=stat[:, 2 * NI2:3 * NI2], in_=Yraw, axis=AX.X, op=ALU.add), R=[Rtt, Ktt], W=[statt])
                    kb.ins("act", lambda: nc.scalar.activation(stat[:, 3 * NI2:4 * NI2], stat[:, 2 * NI2:3 * NI2], AF.Ln, bias=GNB[:], scale=1.0 / 64),
                           R=[statt, GNBt], W=[statt])
                    yield
                    kb.ins("act", lambda: nc.scalar.activation(stat[:, 3 * NI2:4 * NI2], stat[:, 3 * NI2:4 * NI2], AF.Exp, scale=-0.5), R=[statt], W=[statt])
                    kb.ins("pool", lambda: nc.gpsimd.tensor_tensor(out=Ynb[:], in0=Ycb, in1=stat[:, 3 * NI2:4 * NI2].unsqueeze(2).to_broadcast([64, NI2, 64]), op=ALU.mult),
                           R=[SGt, Aat, statt], W=[Ynbt])
                    for c in range(NCH):
                        mm(PS(pyo)[:, c * 64:(c + 1) * 64], Ynb[:, 2 * c:2 * c + 2, :].rearrange("p h v -> p (h v)"), ident_b[0:64, 0:64], True, True,
                           R=[Ynbt, ct], W=[PT[pyo]])
                    yield
                    kb.ins("act", lambda: nc.scalar.activation(t1[:], PS(pyo)[:, 0:BS], AF.Identity, bias=ln_b, scale=ln_w), R=[PT[pyo], pvt], W=[t1t])
                    kb.ins("dve", lambda: nc.vector.tensor_tensor(out=t1[:], in0=t1[:], in1=bv[:], op=ALU.add), R=[t1t, bvt], W=[t1t])
                    kb.ins("dve", lambda: nc.vector.tensor_tensor(out=yrs[:, bsl(b)], in0=t1[:], in1=Gt[:], op=ALU.mult), R=[t1t, Gtt], W=[yrst[b]])
                    yield
                wap, wtr = wsl[0]
                kb.dma("pool", wap, wout_d.ap()[l].rearrange("o p (k j) -> p o k j", k=8)[:, :, hp, :], W=[wtr])
                for oc in range(8):
                    for tb in range(NB):
                        pi = aps()
                        mm(PS(pi), wap[:, oc, :], yrs[:, tbs(tb)], True, True, R=[wtr] + yrst[tb * 2:tb * 2 + 2], W=[PT[pi]])
                        kb.ins("dve", lambda: nc.vector.tensor_tensor(out=x[:, oc, tbs(tb)], in0=PS(pi), in1=x[:, oc, tbs(tb)], op=ALU.add),
                               R=[PT[pi], xt[oc][tb]], W=[xt[oc][tb]])
                        fps(pi)
                    yield

        gens = [stream(0, [0, 1]), stream(1, [2, 3])]
        alive = list(gens)
        first = True
        for _ in range(0):
            next(gens[0])
        while alive:
            for g in list(alive):
                try:
                    next(g)
                except StopIteration:
                    alive.remove(g)
            if first:
                first = False
                kb.min_free = min(getattr(kb, "min_free", 1 << 30), nc.sbuf_bytes_remaining)
        kb.barrier()
        st.close()

    def ffn_phase(l, moe):
        st = ExitStack()
        ne = 8 if moe else 2
        wg_d, wu_d, wd_d = ew_d[1 if moe else 0]
        hid = sb("hid", [128, 11, T], BF16, st); hidt = [[Trk() for _ in range(NB)] for _ in range(11)]
        slu = [sb("slu%d" % i, [128, 512], F32, st) for i in range(2)]; slut = [Trk(), Trk()]
        wdn = sb("wdn", [128, 11, 8, 128], BF16, st); wdnt = [Trk() for _ in range(11)]
        if moe:
            cbc = sb("cbc", [128, T], F32, st); cbct = [Trk() for _ in range(NB)]
            wdn_dummy = None
            combT = sb("combT", [8, T], BF16, st); combTt = [Trk() for _ in range(NT)]
            rtr = sb("rtr", [128, 8, 8], F32, st); rtrt = Trk()
            lg = sb("lg", [128, 8], F32, st); lgt = Trk()
            top = sb("top8", [128, 8], F32, st); topt = Trk()
            gts = sb("gts", [128, 4], F32, st); gtst = Trk()
            eq1 = sb("eq1", [128, 8], F32, st); eq1t = Trk()
            eq2 = sb("eq2", [128, 8], F32, st); eq2t = Trk()
            comb = sb("comb", [128, 8], F32, st); combt = Trk()
            xsq = sb("xsq", [128, 128], BF16, st); xsqt = Trk()
            rs = sb("rs_tok", [128, 2], F32, st); rst = Trk()
            kb.dma("sp", rtr[:].rearrange("p k e -> p (k e)"), router_d.ap(), W=[rtrt])
            for k in range(8):
                kb.ins("dve", lambda k=k: nc.vector.tensor_scalar(rtr[:, k, :], rtr[:, k, :], pv[:, l, 8 + k:9 + k], None, op0=ALU.mult),
                       R=[rtrt, pvt], W=[rtrt])
            lg3 = sb("lg3", [128, NT, 8], F32, st); lg3t = Trk()
            lgb = sb("lgb", [128, NT, 8], F32, st); lgbt = Trk()
            e1 = sb("e1", [128, NT, 8], F32, st); e1t = Trk()
            e2 = sb("e2", [128, NT, 8], F32, st); e2t = Trk()
            tp = sb("tp", [128, 6, NT], F32, st); tpt = Trk()
            PSl = PS(7).rearrange("p (t e) -> p t e", e=16)[:, 0:NT, :]

            def router_tb(tb):
                for q in range(4):
                    tt = tb * 4 + q
                    tsl = slice(tt * 128, (tt + 1) * 128)
                    for k in range(8):
                        mm(PSl[:, tt, 0:8], x[:, k, tsl], rtr[:, k, :], k == 0, k == 7, R=[xt[k][tb], rtrt], W=[PT[7]])
                    mm(PSl[:, tt, 8:9], rstd[0:1, q * 128:(q + 1) * 128], ident_f[0:1, 0:1], True, True, R=[rstdt, ct], W=[PT[7]])
            rmsnorm_to_h(l, 8, after_tb=router_tb)
            kb.ins("act", lambda: nc.scalar.copy(tp[:, 0, :], PSl[:, :, 8]), R=[PT[7]], W=[tpt])
            kb.ins("dve", lambda: nc.vector.tensor_tensor(out=lg3[:], in0=PSl[:, :, 0:8], in1=tp[:, 0, :].unsqueeze(2).to_broadcast([128, NT, 8]), op=ALU.mult),
                   R=[PT[7], tpt], W=[lg3t])
            kb.ins("dve", lambda: nc.vector.tensor_reduce(out=tp[:, 1, :], in_=lg3[:], axis=AX.X, op=ALU.max), R=[lg3t], W=[tpt])
            kb.ins("dve", lambda: nc.vector.tensor_tensor(out=e1[:], in0=lg3[:], in1=tp[:, 1, :].unsqueeze(2).to_broadcast([128, NT, 8]), op=ALU.is_equal),
                   R=[lg3t, tpt], W=[e1t])
            kb.ins("dve", lambda: nc.vector.scalar_tensor_tensor(out=lgb[:], in0=e1[:], scalar=-1e30, in1=lg3[:], op0=ALU.mult, op1=ALU.add),
                   R=[e1t, lg3t], W=[lgbt])
            kb.ins("dve", lambda: nc.vector.tensor_reduce(out=tp[:, 2, :], in_=lgb[:], axis=AX.X, op=ALU.max), R=[lgbt], W=[tpt])
            kb.ins("dve", lambda: nc.vector.tensor_tensor(out=e2[:], in0=lgb[:], in1=tp[:, 2, :].unsqueeze(2).to_broadcast([128, NT, 8]), op=ALU.is_equal),
                   R=[lgbt, tpt], W=[e2t])
            kb.ins("dve", lambda: nc.vector.tensor_tensor(out=tp[:, 3, :], in0=tp[:, 2, :], in1=tp[:, 1, :], op=ALU.subtract), R=[tpt], W=[tpt])
            kb.ins("act", lambda: nc.scalar.activation(tp[:, 3, :], tp[:, 3, :], AF.Exp), R=[tpt], W=[tpt])
            kb.ins("dve", lambda: nc.vector.tensor_scalar(tp[:, 3, :], tp[:, 3, :], 1.0, None, op0=ALU.add), R=[tpt], W=[tpt])
            kb.ins("dve", lambda: nc.vector.reciprocal(tp[:, 4, :], tp[:, 3, :]), R=[tpt], W=[tpt])
            kb.ins("dve", lambda: nc.vector.tensor_scalar(tp[:, 5, :], tp[:, 4, :], -1.0, 1.0, op0=ALU.mult, op1=ALU.add), R=[tpt], W=[tpt])
            kb.ins("dve", lambda: nc.vector.tensor_tensor(out=e1[:], in0=e1[:], in1=tp[:, 4, :].unsqueeze(2).to_broadcast([128, NT, 8]), op=ALU.mult),
                   R=[e1t, tpt], W=[e1t])
            kb.ins("dve", lambda: nc.vector.tensor_tensor(out=e2[:], in0=e2[:], in1=tp[:, 5, :].unsqueeze(2).to_broadcast([128, NT, 8]), op=ALU.mult),
                   R=[e2t, tpt], W=[e2t])
            kb.ins("dve", lambda: nc.vector.tensor_tensor(out=e1[:], in0=e1[:], in1=e2[:], op=ALU.add), R=[e1t, e2t], W=[e1t])
            for tt in range(NT):
                bnk = tt // 4
                mm(PS(bnk)[0:8, (tt % 4) * 128:(tt % 4 + 1) * 128], e1[:, tt, :], ident_f[:], True, True, R=[e1t, ct], W=[PT[bnk]])
            for bnk in range(4):
                evac_copy(combT[:, bnk * 512:(bnk + 1) * 512], PS(bnk)[0:8, :], R=[PT[bnk]], W=combTt[bnk * 4:bnk * 4 + 4])
        else:
            rmsnorm_to_h(l, 8)
        steps = [(e, hc) for e in range(ne) for hc in range(11)]

        def issue_loads(i):
            e_, hc_ = steps[i]
            base = 2 * (i % 2)
            load_w(wg_d.ap()[e_, hc_], slot=base)
            load_w(wu_d.ap()[e_, hc_], slot=base + 1)
        issue_loads(0)
        for i, (e, hc) in enumerate(steps):
            if hc == 0 and moe:
                for tb in range(NB):
                    pi = next_ps()
                    mm(PS(pi), sel[:, e * 128:(e + 1) * 128], combT[:, tbs(tb)], True, True, R=[ct] + combTt[tb * 4:tb * 4 + 4], W=[PT[pi]])
                    evac_copy(cbc[:, tbs(tb)], PS(pi), R=[PT[pi]], W=[cbct[tb]], eng="act")
            if i + 1 < len(steps):
                issue_loads(i + 1)
            if hc >= 1:
                for hq in ((0, 1) if hc == 1 else (hc,)):
                    kb.dma("pool", wdn[:, hq, :, :].rearrange("p o j -> p (o j)"), wd_d.ap()[e, hq], W=[wdnt[hq]])
            sg_ = 2 * (i % 2); su_ = sg_ + 1
            for tb in range(NB):
                pg = next_ps(); pu = next_ps()
                while pu == pg:
                    pu = next_ps()
                for k in range(8):
                    mm(PS(pg), wbuf[:, sg_, k, :], h[:, k, tbs(tb)], k == 0, k == 7, R=[wbt[sg_], ht[tb]], W=[PT[pg]])
                for k in range(8):
                    mm(PS(pu), wbuf[:, su_, k, :], h[:, k, tbs(tb)], k == 0, k == 7, R=[wbt[su_], ht[tb]], W=[PT[pu]])
                s = (hc * NB + tb) % 2
                kb.ins("act", lambda: nc.scalar.activation(slu[s][:], PS(pg), AF.Silu), R=[PT[pg]], W=[slut[s]])
                if moe:
                    kb.ins("dve", lambda: nc.vector.tensor_tensor(out=slu[s][:], in0=slu[s][:], in1=cbc[:, tbs(tb)], op=ALU.mult),
                           R=[slut[s], cbct[tb]], W=[slut[s]])
                kb.ins("dve", lambda: nc.vector.tensor_tensor(out=hid[:, hc, tbs(tb)], in0=PS(pu), in1=slu[s][:], op=ALU.mult),
                       R=[PT[pu], slut[s]], W=[hidt[hc][tb]])
            if hc == 10:
                down_proj(e, wd_d, hid, hidt, wdn, wdnt)
        kb.barrier()
        st.close()

    def down_proj(e, wd_d, hid, hidt, wdn, wdnt):
        for ocg in range(2):
            for tbg in range(2):
                for hc in range(11):
                    for oi in range(4):
                        oc = ocg * 4 + oi
                        for ti in range(2):
                            tb = tbg * 2 + ti
                            pi = oi * 2 + ti
                            mm(PS(pi), wdn[:, hc, oc, :], hid[:, hc, tbs(tb)], hc == 0, hc == 10,
                               R=[wdnt[hc], hidt[hc][tb]], W=[PT[pi]])
                for oi in range(4):
                    oc = ocg * 4 + oi
                    for ti in range(2):
                        tb = tbg * 2 + ti
                        pi = oi * 2 + ti
                        kb.ins("dve", lambda: nc.vector.tensor_tensor(out=x[:, oc, tbs(tb)], in0=PS(pi), in1=x[:, oc, tbs(tb)], op=ALU.add),
                               R=[PT[pi], xt[oc][tb]], W=[xt[oc][tb]])

    def final_phase():
        for tb in range(NB):
            pi = next_ps()
            for c in range(8):
                s = c % 2
                kb.ins("act", lambda c=c, s=s: nc.scalar.activation(sq[s][:], x[:, c, tbs(tb)], AF.Square), R=[xt[c][tb]], W=[sqt[s]])
                mm(PS(pi), onesm[:], sq[s][:], c == 0, c == 7, R=[ct, sqt[s]], W=[PT[pi]])
            kb.ins("act", lambda: nc.scalar.activation(rstd[:], PS(pi), AF.Ln, bias=1e-6, scale=1.0), R=[PT[pi]], W=[rstdt])
            kb.ins("act", lambda: nc.scalar.activation(rstd[:], rstd[:], AF.Exp, scale=-0.5), R=[rstdt], W=[rstdt])
            for c in range(8):
                kb.ins("dve", lambda c=c: nc.vector.scalar_tensor_tensor(
                    out=x[:, c, tbs(tb)], in0=x[:, c, tbs(tb)], scalar=pv[:, 0, 16 + c:17 + c], in1=rstd[:],
                    op0=ALU.mult, op1=ALU.mult), R=[xt[c][tb], pvt, rstdt], W=[xt[c][tb]])

    def store_x():
        for c in range(8):
            kb.dma("sp", out_d.ap()[c * 128:(c + 1) * 128, :], x[:, c, :], R=xt[c])

    done = False
    for l in range(n_layers):
        rmsnorm_to_h(l, 0)
        attention_phase(l)
        if stop_after == "attn%d" % l:
            done = True
            break
        rwkv_phase(l)
        if stop_after == "mix%d" % l:
            done = True
            break
        ffn_phase(l, moe=(l % 2 == 1))
        if stop_after == "ffn%d" % l:
            done = True
            break
    if not done:
        final_phase()
    store_x()
    kb.finish()
    kb.close()
    return kb


def host_inputs(inp):
    f = lambda a: np.ascontiguousarray(a, dtype=np.float32)
    L = 2
    w_in = inp["w_in"]
    win = f(w_in.reshape(L, 8, 128, 26, 128).transpose(0, 3, 2, 1, 4).reshape(L, 26, 128, 1024))
    wv = f(win[:, 22:26].reshape(L, 4, 128, 1024).transpose(0, 2, 1, 3).reshape(L, 128, 4096))
    wout = f(inp["w_out"].reshape(L, 8, 128, 8, 128).transpose(0, 3, 2, 1, 4).reshape(L, 8, 128, 1024))
    loraw = f(np.concatenate([inp["rwkv_w2"], inp["rwkv_a2"]], axis=1))
    g2 = f(inp["rwkv_g2"])

    def experts(wg, wu, wd, ne):
        a = f(wg.reshape(ne, 8, 128, 11, 128).transpose(0, 3, 2, 1, 4).reshape(ne, 11, 128, 1024))
        b = f(wu.reshape(ne, 8, 128, 11, 128).transpose(0, 3, 2, 1, 4).reshape(ne, 11, 128, 1024))
        c = f(wd.reshape(ne, 11, 128, 1024))
        return a, b, c
    dg = inp["ffn_w_gate"][0].reshape(1024, 2, 1408).transpose(1, 0, 2)
    du = inp["ffn_w_up"][0].reshape(1024, 2, 1408).transpose(1, 0, 2)
    dd = inp["ffn_w_down"][0].reshape(2, 1408, 1024)
    wg0, wu0, wd0 = experts(dg, du, dd, 2)
    wg1, wu1, wd1 = experts(inp["moe_w_gate"][0], inp["moe_w_up"][0], inp["moe_w_down"][0], 8)
    router = f(inp["moe_router"][0].reshape(8, 128, 8).transpose(1, 0, 2).reshape(128, 64))

    pv = np.zeros((L, 128, NPV), np.float32)
    col = lambda v: v.reshape(-1, 128).T
    for l in range(L):
        pv[l, :, 0:8] = col(inp["norm_mix_g"][l])
        pv[l, :, 8:16] = col(inp["norm_ffn_g"][l])
        pv[l, :, 16:24] = col(inp["norm_final_g"])
        pv[l, :, 24:38] = col(inp["shift_mu"][l])
        pv[l, :, 52:56] = col(inp["rwkv_w0"][l])
        pv[l, :, 56:60] = col(inp["rwkv_a0"][l])
        pv[l, :, 60:64] = col(inp["rwkv_k_k"][l])
        pv[l, :, 64:68] = col(inp["rwkv_k_a"][l])
        pv[l, :, 68:72] = col(inp["rwkv_r_k"][l])
        pv[l, :, 72:76] = col(inp["rwkv_ln_w"][l])
        pv[l, :, 76:80] = col(inp["rwkv_ln_b"][l])
    attn_g_bc = f(np.broadcast_to(inp["attn_norm_g"][:, None, :], (L, 128, 512)))
    ki = np.arange(128)[:, None]
    qi = np.arange(128)[None, :]
    biasT = np.zeros((L, 8, 128, 5, 128), np.float32)
    for s in range(5):
        rel = 128 * (4 - s) + qi - ki
        idx = np.clip(rel, -128, 128) + 128
        valid = np.ones((128, 128), bool)
        if s == 4:
            valid = ~((ki >= 64) & (qi < 64))
        if s == 0:
            valid = ~((ki < 64) & (qi >= 64))
        for l in range(L):
            g = inp["attn_rel_bias"][l][idx]
            g = np.where(valid[:, :, None], g, np.float32(-1e30))
            biasT[l, :, :, s, :] = g.transpose(2, 0, 1)
    biasT = f(biasT.reshape(L, 8, 128, 640))
    ident = np.eye(128, dtype=np.float32)
    onesm = np.full((128, 128), 1.0 / 1024, np.float32)
    blk = np.kron(np.eye(2, dtype=np.float32), np.ones((64, 64), np.float32))
    s_ = np.arange(128)[:, None]; t_ = np.arange(128)[None, :]
    same = (s_ // 64) == (t_ // 64)
    tri_i = (same & (s_ <= t_)).astype(np.float32)
    tri_e = (same & (s_ < t_)).astype(np.float32)
    s6 = np.arange(64)[:, None]; t6 = np.arange(64)[None, :]
    strict = (s6 < t6).astype(np.float32); incl = (s6 <= t6).astype(np.float32)
    lower = (t6 < s6).astype(np.float32)
    mA = np.concatenate([strict, incl, strict, incl], axis=1)
    maskA = np.concatenate([mA, mA], axis=1)
    mB = np.concatenate([lower, np.ones((64, 192), np.float32)], axis=1)
    maskB = np.concatenate([mB, mB], axis=1)
    sel = np.zeros((8, 8, 128), np.float32)
    for e in range(8):
        sel[e, e, :] = 1.0
    shared = {"pv": pv, "attn_g_bc": attn_g_bc, "biasT": biasT, "win": win, "wv": wv, "wout": wout, "loraw": loraw, "g2": g2,
              "wg0": wg0, "wu0": wu0, "wd0": wd0, "wg1": wg1, "wu1": wu1, "wd1": wd1, "router": router,
              "c_ident": ident, "c_onesm": onesm, "c_blk": blk, "c_tri_i": tri_i, "c_tri_e": tri_e,
              "c_maskA": f(maskA), "c_maskB": f(maskB), "c_sel": f(sel.reshape(8, 1024))}
    return shared


_PROG = {}


def kernel(**inputs):
    inp = {k: np.asarray(v) for k, v in inputs.items()}
    shared = host_inputs(inp)
    if "kb" not in _PROG:
        _PROG["kb"] = build_program()
    kb = _PROG["kb"]
    x = inp["x"]
    in_maps = []
    for b in range(8):
        m = dict(shared)
        m["xT"] = np.ascontiguousarray(x[b].T)
        in_maps.append(m)
    res = run_bass_kernel_spmd(kb.nc, in_maps, core_ids=list(range(8)))
    out = np.stack([np.ascontiguousarray(res.results[b]["outT"].T) for b in range(8)], axis=0)
    return out.astype(np.float32)
```
